# Optimizing a Trainium2 kernel written in Bass

```python
import math
import jax, jax.numpy as jnp
from jax import lax
import numpy as np

D_MODEL = 1024
BATCH = 16
SEQ = 2048
DEPTH = 1

CHUNK = 64
D_MIX = D_MODEL
A_HEADS = 8
A_HEAD_DIM = 64
A_WIDTH = A_HEADS * A_HEAD_DIM
IDX_HEADS = 8
IDX_DIM = 32
TOPK_MAX = 256
B_HEADS = 4
B_KEY_DIM = 64
B_VAL_DIM = 128
B_WIDTH = B_HEADS * B_VAL_DIM
B_KEY_WIDTH = B_HEADS * B_KEY_DIM
GATE_RANK = 16
GATE_NORMALIZER = 16.0
N_BUCKETS = 32
MAX_DISTANCE = 128
D_FF = ((8 * D_MODEL + 3 * 256 - 1) // (3 * 256)) * 256
EPS = 1e-6
NEG = -1e30

SPLIT_SIZES = (
    A_WIDTH, A_WIDTH, A_WIDTH,
    IDX_HEADS * IDX_DIM, IDX_DIM, IDX_HEADS,
    B_KEY_WIDTH, B_KEY_WIDTH, B_WIDTH,
    GATE_RANK, B_WIDTH,
)
D_IN = sum(SPLIT_SIZES)

kernel_name = "hymba_dsa_gla_streaming_block"


def rmsnorm(x, g):
    xf = x.astype(jnp.float32)
    y = xf * lax.rsqrt(jnp.mean(xf * xf, axis=-1, keepdims=True) + EPS)
    return (y * g.astype(jnp.float32)).astype(x.dtype)


def t5_bucket(rel):
    half = N_BUCKETS // 2
    max_exact = half // 2
    ret = jnp.where(rel > 0, half, 0)
    n = jnp.abs(rel)
    nf = jnp.maximum(n, 1).astype(jnp.float32)
    large = max_exact + (jnp.log(nf / max_exact) / math.log(MAX_DISTANCE / max_exact)
                         * (half - max_exact)).astype(jnp.int32)
    large = jnp.minimum(large, half - 1)
    return ret + jnp.where(n < max_exact, n, large)


def dsa_mixer(q, k, v, q_idx, k_idx, w_idx, rel_bias):
    bsz, seq = q.shape[0], q.shape[1]
    topk = min(TOPK_MAX, seq // 4)
    n_blk = seq // CHUNK
    key_pos = jnp.arange(seq)
    k_idx_f = k_idx.astype(jnp.float32)

    def block(args):
        n, qb, qib, wb = args
        limit = (n + 1) * CHUNK
        qpos = n * CHUNK + jnp.arange(CHUNK)
        dots = jax.nn.relu(jnp.einsum('bqhd,bsd->bqhs', qib.astype(jnp.float32), k_idx_f)
                           * IDX_DIM ** -0.5)
        score = jnp.einsum('bqhs,bqh->bqs', dots, wb.astype(jnp.float32) * IDX_HEADS ** -0.5)
        score = jnp.where(key_pos[None, None, :] < limit, score, NEG)
        _, sel = lax.top_k(score, topk)
        valid = sel < limit
        kg = jax.vmap(lambda kb, ib: kb[ib])(k, sel)
        vg = jax.vmap(lambda vb, ib: vb[ib])(v, sel)
        bias = rel_bias[t5_bucket(sel - qpos[None, :, None])]
        logits = (jnp.einsum('bqhd,bqkhd->bqhk', qb, kg).astype(jnp.float32) * A_HEAD_DIM ** -0.5
                  + jnp.swapaxes(bias, 2, 3).astype(jnp.float32))
        logits = jnp.where(valid[:, :, None, :], logits, NEG)
        p = jax.nn.softmax(logits, axis=-1).astype(v.dtype)
        return jnp.einsum('bqhk,bqkhd->bqhd', p, vg)

    def to_blocks(t):
        return jnp.swapaxes(t.reshape(bsz, n_blk, CHUNK, *t.shape[2:]), 0, 1)

    out = lax.map(block, (jnp.arange(n_blk), to_blocks(q), to_blocks(q_idx), to_blocks(w_idx)))
    return jnp.swapaxes(out, 0, 1).reshape(bsz, seq, A_WIDTH)


def gla_mixer(q, k, v, log_a):
    bsz, seq, nh, dk = q.shape
    nc = seq // CHUNK

    def chunks(t):
        return t.astype(jnp.float32).reshape(bsz, nc, CHUNK, nh, -1).transpose(1, 0, 3, 2, 4)

    qc = chunks(q) * dk ** -0.5
    kc, vc, gc = chunks(k), chunks(v), chunks(log_a)
    bcum = jnp.cumsum(gc, axis=3)
    blast = bcum[..., -1:, :]
    q_in = qc * jnp.exp(bcum)
    k_in = kc * jnp.exp(-bcum)
    k_dec = kc * jnp.exp(blast - bcum)
    causal = jnp.tril(jnp.ones((CHUNK, CHUNK), dtype=bool))
    att = jnp.where(causal, jnp.einsum('nbhtd,nbhsd->nbhts', q_in, k_in), 0.0)
    o_intra = jnp.einsum('nbhts,nbhsv->nbhtv', att, vc)

    def step(state, inp):
        qi, kd, vv, bl = inp
        o = jnp.einsum('bhtd,bhdv->bhtv', qi, state)
        state = state * jnp.exp(bl)[:, :, 0, :, None] + jnp.einsum('bhsd,bhsv->bhdv', kd, vv)
        return state, o

    s0 = jnp.zeros((bsz, nh, dk, vc.shape[-1]), jnp.float32)
    _, o_inter = lax.scan(step, s0, (q_in, k_dec, vc, blast))
    o = o_intra + o_inter
    return o.transpose(1, 0, 3, 2, 4).reshape(bsz, seq, nh, -1)


def setup_inputs(seed: int = 0) -> dict:
    key = jax.random.key(seed)
    ks = jax.random.split(key, 16)
    f32 = jnp.float32
    nrm = lambda k, shape, s: jax.random.normal(k, shape, f32) * s
    return {
        "x": nrm(ks[0], (BATCH, SEQ, D_MODEL), 1.0),
        "norm_mix": 1.0 + nrm(ks[1], (DEPTH, D_MODEL), 0.02),
        "w_in": nrm(ks[2], (DEPTH, D_MODEL, D_IN), D_MODEL ** -0.5),
        "w_gate_up": nrm(ks[3], (DEPTH, GATE_RANK, B_KEY_WIDTH), GATE_RANK ** -0.5),
        "b_gate": nrm(ks[4], (DEPTH, B_KEY_WIDTH), 0.01),
        "gla_norm": 1.0 + nrm(ks[5], (DEPTH, B_WIDTH), 0.02),
        "w_out": nrm(ks[6], (DEPTH, D_MIX, D_MODEL), D_MIX ** -0.5),
        "rel_bias": nrm(ks[7], (N_BUCKETS, A_HEADS), 0.2),
        "norm_ffn": 1.0 + nrm(ks[8], (DEPTH, D_MODEL), 0.02),
        "w_ffn_gate": nrm(ks[9], (DEPTH, D_MODEL, D_FF), D_MODEL ** -0.5),
        "w_ffn_up": nrm(ks[10], (DEPTH, D_MODEL, D_FF), D_MODEL ** -0.5),
        "w_ffn_down": nrm(ks[11], (DEPTH, D_FF, D_MODEL), D_FF ** -0.5),
        "norm_final": 1.0 + nrm(ks[12], (D_MODEL,), 0.02),
    }


def reference(x, norm_mix, w_in, w_gate_up, b_gate, gla_norm, w_out, rel_bias,
              norm_ffn, w_ffn_gate, w_ffn_up, w_ffn_down, norm_final):
    bsz, seq, _ = x.shape
    offsets = [int(o) for o in np.cumsum(SPLIT_SIZES)[:-1]]
    for l in range(DEPTH):
        h = rmsnorm(x, norm_mix[l])
        proj = h @ w_in[l]
        (qa, ka, va, qi, ki, wi, qb, kb, vb, gdown, ogate) = jnp.split(proj, offsets, axis=-1)
        o_a = dsa_mixer(qa.reshape(bsz, seq, A_HEADS, A_HEAD_DIM),
                        ka.reshape(bsz, seq, A_HEADS, A_HEAD_DIM),
                        va.reshape(bsz, seq, A_HEADS, A_HEAD_DIM),
                        qi.reshape(bsz, seq, IDX_HEADS, IDX_DIM), ki, wi, rel_bias)
        log_a = jax.nn.log_sigmoid((gdown @ w_gate_up[l] + b_gate[l]).astype(jnp.float32)) / GATE_NORMALIZER
        o_b = gla_mixer(qb.reshape(bsz, seq, B_HEADS, B_KEY_DIM),
                        kb.reshape(bsz, seq, B_HEADS, B_KEY_DIM),
                        vb.reshape(bsz, seq, B_HEADS, B_VAL_DIM),
                        log_a.reshape(bsz, seq, B_HEADS, B_KEY_DIM))
        o_b = rmsnorm(o_b, gla_norm[l].reshape(B_HEADS, B_VAL_DIM)).reshape(bsz, seq, B_WIDTH)
        o_b = (o_b * jax.nn.silu(ogate.astype(jnp.float32))).astype(x.dtype)
        mix = jnp.concatenate([o_a.astype(x.dtype), o_b], axis=-1) @ w_out[l]
        x = x + mix
        h = rmsnorm(x, norm_ffn[l])
        x = x + (jax.nn.silu(h @ w_ffn_gate[l]) * (h @ w_ffn_up[l])) @ w_ffn_down[l]
    return rmsnorm(x, norm_final)
```

```python
import math
from contextlib import ExitStack

import numpy as np
import concourse.bass as bass
import concourse.mybir as mybir
from concourse.bass_utils import run_bass_kernel_spmd

F32 = mybir.dt.float32
BF16 = mybir.dt.bfloat16
ALU = mybir.AluOpType
AF = mybir.ActivationFunctionType
AX = mybir.AxisListType

S = 2048
D = 1024
NT = S // 128
DFF = 2816
NJ = DFF // 128
NEG = -1.0e30
EPS = 1e-6

O_QA, O_KA, O_VA, O_QI, O_KI, O_WI, O_QB, O_KB, O_VB, O_GD, O_OG = (
    0, 512, 1024, 1536, 1792, 1824, 1832, 2088, 2344, 2856, 2872)

FM_TILES = ([("qa%d" % j, 128) for j in range(4)] + [("ka%d" % j, 128) for j in range(4)]
            + [("qi%d" % j, 96) for j in range(3)] + [("ki", 96)]
            + [("qb%d" % j, 128) for j in range(2)] + [("kb%d" % j, 128) for j in range(2)]
            + [("gd", 16)])
NFM = sum(m for _, m in FM_TILES)
NTM = 512 + 264 + 512 + 512

C_ID, C_U01, C_UNEG, C_LNEG, C_DMASK, C_GMIX, C_GFFN, C_CFAR = 0, 128, 256, 384, 512, 640, 648, 656
C_MIN = 672
C_GLAN = 672
C_GFIN = C_GLAN + 512
C_BIAS = C_GFIN + 1024
NCONST = C_BIAS + 2048

R_BIS = 128.0
N_BIS = 17


class Buf:
    __slots__ = ("ap", "name", "last_w", "readers")

    def __init__(self, ap, name=""):
        self.ap = ap
        self.name = name
        self.last_w = None
        self.readers = []


class Op:
    __slots__ = ("eng", "fn", "deps", "needs_inc", "semval", "is_dma", "dsem", "dval", "phase")

    def __init__(self, eng, fn):
        self.eng = eng
        self.fn = fn
        self.deps = []
        self.needs_inc = False
        self.semval = None
        self.is_dma = False
        self.dsem = None
        self.dval = None


class Prog:
    ENGS = ("tensor", "vector", "scalar", "gpsimd", "sync")

    def __init__(self, nc):
        self.nc = nc
        self.ops = {e: [] for e in self.ENGS}
        self.es = ExitStack()
        self.dma_counts = {}
        self.dma_last = {}
        self.phase = 0

    def sbuf(self, name, shape, dt):
        return self.es.enter_context(self.nc.sbuf_tensor("sb_" + name, list(shape), dt))

    def psum(self, name, shape, dt):
        return self.es.enter_context(self.nc.psum_tensor("ps_" + name, list(shape), dt))

    def _dep(self, op, src):
        if src is None or src is op:
            return
        if src.eng == "tensor" and op.eng == "tensor" and not src.is_dma and not op.is_dma:
            return
        op.deps.append(src)
        if not src.is_dma:
            src.needs_inc = True

    def op(self, eng, fn, reads=(), writes=(), dma_key=None, extra=()):
        o = Op(eng, fn)
        o.phase = self.phase
        if dma_key is not None:
            o.is_dma = True
            o.dsem = dma_key
            self.dma_counts[dma_key] = self.dma_counts.get(dma_key, 0) + 1
            o.dval = 16 * self.dma_counts[dma_key]
            self.dma_last[dma_key] = o
        for s in extra:
            self._dep(o, s)
        for b in reads:
            self._dep(o, b.last_w)
        for b in writes:
            lw = b.last_w
            if lw is not None and not (lw.eng == eng and not lw.is_dma and not o.is_dma):
                self._dep(o, lw)
            for r in b.readers:
                if r.eng == eng and not r.is_dma and not o.is_dma:
                    continue
                self._dep(o, r)
        for b in reads:
            b.readers.append(o)
        for b in writes:
            b.last_w = o
            b.readers = []
        self.ops[eng].append(o)
        return o

    def barrier(self):
        lasts = []
        for e in self.ENGS:
            for o in reversed(self.ops[e]):
                if not o.is_dma:
                    lasts.append(o)
                    break
        lasts += list(self.dma_last.values())
        for e in self.ENGS:
            self.op(e, lambda eng: eng.nop(), extra=[l for l in lasts])
        self.phase += 1

    def dma(self, eng, out_ap, in_ap, key, reads=(), writes=()):
        return self.op(eng, lambda e: e.dma_start(out=out_ap, in_=in_ap), reads, writes, dma_key=key)

    def matmul(self, out, lhsT, rhs, start, stop, reads=(), writes=()):
        return self.op("tensor", lambda e: e.matmul(out, lhsT=lhsT, rhs=rhs, start=start, stop=stop), reads, writes)

    def transpose(self, out, in_, ident, reads=(), writes=()):
        return self.op("tensor", lambda e: e.transpose(out, in_, ident), reads, writes)

    def act(self, out, in_, func, reads=(), writes=(), bias=None, scale=None, accum=None):
        kw = {}
        if bias is not None:
            kw["bias"] = bias
        if scale is not None:
            kw["scale"] = scale
        if accum is not None:
            kw["accum_out"] = accum
        return self.op("scalar", lambda e: e.activation(out=out, in_=in_, func=func, **kw), reads, writes)

    def tt(self, eng, out, in0, in1, op, reads=(), writes=()):
        return self.op(eng, lambda e: e.tensor_tensor(out=out, in0=in0, in1=in1, op=op), reads, writes)

    def ts(self, eng, out, in0, s1, s2, op0, op1=None, reads=(), writes=(), accum=None):
        kw = {}
        if op1 is not None:
            kw["op1"] = op1
        if accum is not None:
            kw["accum_out"] = accum
        return self.op(eng, lambda e: e.tensor_scalar(out=out, in0=in0, scalar1=s1, scalar2=s2, op0=op0, **kw),
                       reads, writes)

    def stt(self, out, in0, scalar, in1, op0, op1, reads=(), writes=()):
        return self.op("vector", lambda e: e.scalar_tensor_tensor(out=out, in0=in0, scalar=scalar, in1=in1,
                                                                  op0=op0, op1=op1), reads, writes)

    def copy(self, eng, out, in_, reads=(), writes=()):
        if eng == "scalar":
            return self.op(eng, lambda e: e.copy(out=out, in_=in_), reads, writes)
        return self.op(eng, lambda e: e.tensor_copy(out=out, in_=in_), reads, writes)

    def memset(self, eng, ap, val, writes=()):
        return self.op(eng, lambda e: e.memset(ap, val), (), writes)

    def recip(self, out, in_, reads=(), writes=()):
        return self.op("vector", lambda e: e.reciprocal(out=out, in_=in_), reads, writes)

    def reduce(self, out, in_, op, reads=(), writes=()):
        return self.op("vector", lambda e: e.tensor_reduce(out=out, in_=in_, axis=AX.X, op=op), reads, writes)

    def emit(self, final_waits=()):
        nc = self.nc
        es = self.es
        esem = {(e, ph): es.enter_context(nc.semaphore("s_%s_%d" % (e, ph)))
                for e in self.ENGS for ph in range(self.phase + 1)}
        dsem = {k: es.enter_context(nc.semaphore("d_" + str(k))) for k in self.dma_counts}
        for e in self.ENGS:
            c = {}
            for o in self.ops[e]:
                if o.is_dma:
                    continue
                if o.needs_inc:
                    c[o.phase] = c.get(o.phase, 0) + 1
                    o.semval = c[o.phase]
        block = es.enter_context(nc.Block())

        def run(ename):
            def body(eng):
                known = {}
                for o in self.ops[ename]:
                    for d in o.deps:
                        if d.is_dma:
                            key, val, sem = ("d", d.dsem), d.dval, dsem[d.dsem]
                        else:
                            key, val, sem = ("e", d.eng, d.phase), d.semval, esem[(d.eng, d.phase)]
                        if known.get(key, 0) >= val:
                            continue
                        known[key] = val
                        eng.wait_ge(sem, val)
                    ins = o.fn(eng)
                    if o.is_dma:
                        ins.then_inc(dsem[o.dsem], 16)
                    elif o.needs_inc:
                        ins.then_inc(esem[(ename, o.phase)], 1)
                if ename == "sync":
                    for k in final_waits:
                        eng.wait_ge(dsem[k], 16 * self.dma_counts[k])
            return body

        block.tensor(run("tensor"))
        block.vector(run("vector"))
        block.scalar(run("scalar"))
        block.gpsimd(run("gpsimd"))
        block.sync(run("sync"))
        es.close()


class Arena:
    def __init__(self, P, nf32):
        self.t = P.sbuf("arena", [128, nf32], F32)
        self.n = nf32
        self.off = 0

    def reset(self, to=0):
        self.off = to

    def alloc(self, shape, dt, at=None):
        per = 1
        for s in shape[1:]:
            per *= s
        nf = (per + 1) // 2 if dt == BF16 else per
        off = self.off if at is None else at
        assert off + nf <= self.n, ("arena overflow", off, nf, self.n)
        v = self.t[0:shape[0], off:off + nf]
        if dt == BF16:
            v = v.bitcast(BF16)
            if per % 2:
                v = v[:, 0:per]
        if len(shape) == 3:
            v = v.rearrange("p (a b) -> p a b", b=shape[2])
        elif len(shape) == 4:
            v = v.rearrange("p (a b c) -> p a b c", b=shape[2], c=shape[3])
        if at is None:
            self.off = off + nf
        return v


def _t5_bucket(rel):
    half, max_exact = 16, 8
    ret = np.where(rel > 0, half, 0)
    n = np.abs(rel)
    nf = np.maximum(n, 1).astype(np.float32)
    large = max_exact + (np.log(nf / np.float32(max_exact)) / np.float32(math.log(128 / max_exact))
                         * np.float32(half - max_exact)).astype(np.int32)
    large = np.minimum(large, half - 1)
    return ret + np.where(n < max_exact, n, large)


def _host_layout(inp):
    w_in = np.asarray(inp["w_in"], np.float32)[0]
    cols = []
    for j in range(4):
        cols.append(w_in[:, O_QA + j * 128:O_QA + (j + 1) * 128])
    for j in range(4):
        cols.append(w_in[:, O_KA + j * 128:O_KA + (j + 1) * 128])
    qi = w_in[:, O_QI:O_QI + 256]
    cols.append(qi[:, 0:96])
    cols.append(qi[:, 96:192])
    cols.append(np.concatenate([qi[:, 192:256], np.zeros((D, 32), np.float32)], axis=1))
    ki = w_in[:, O_KI:O_KI + 32]
    cols.append(np.concatenate([ki, ki, ki], axis=1))
    for j in range(2):
        cols.append(w_in[:, O_QB + j * 128:O_QB + (j + 1) * 128])
    for j in range(2):
        cols.append(w_in[:, O_KB + j * 128:O_KB + (j + 1) * 128])
    cols.append(w_in[:, O_GD:O_GD + 16])
    w_fm = np.ascontiguousarray(np.concatenate(cols, axis=1))
    assert w_fm.shape[1] == NFM
    w_tm = np.ascontiguousarray(np.concatenate([
        w_in[:, O_VA:O_VA + 512], w_in[:, O_KB:O_KB + 256], w_in[:, O_WI:O_WI + 8],
        w_in[:, O_VB:O_VB + 512], w_in[:, O_OG:O_OG + 512]], axis=1))
    assert w_tm.shape[1] == NTM

    c = np.zeros((128, NCONST), np.float32)
    ii = np.arange(128)
    c[:, C_ID:C_ID + 128] = np.eye(128, dtype=np.float32)
    u01 = (ii[:, None] <= ii[None, :]).astype(np.float32)
    c[:, C_U01:C_U01 + 128] = u01
    c[:, C_UNEG:C_UNEG + 128] = -u01 / 16.0
    c[:, C_LNEG:C_LNEG + 128] = -(ii[:, None] > ii[None, :]).astype(np.float32) / 16.0
    dm = np.zeros((128, 128), np.float32)
    dm[:64, 64:] = NEG
    c[:, C_DMASK:C_DMASK + 128] = dm
    c[:, C_GMIX:C_GMIX + 8] = np.asarray(inp["norm_mix"], np.float32)[0].reshape(8, 128).T
    c[:, C_GFFN:C_GFFN + 8] = np.asarray(inp["norm_ffn"], np.float32)[0].reshape(8, 128).T
    rb = np.asarray(inp["rel_bias"], np.float32)
    c[:, C_CFAR:C_CFAR + 8] = rb[15][None, :]
    c[:, C_GLAN:C_GLAN + 512] = np.asarray(inp["gla_norm"], np.float32)[0][None, :]
    c[:, C_GFIN:C_GFIN + 1024] = np.asarray(inp["norm_final"], np.float32)[None, :]
    bt = np.zeros((128, 2, 8, 128), np.float32)
    for which in range(2):
        rel = (ii[:, None] - 128 * which) - ii[None, :]
        bk = _t5_bucket(rel)
        bt[:, which] = rb[bk].transpose(0, 2, 1)
    c[:, C_BIAS:C_BIAS + 2048] = bt.reshape(128, 2048)
    wgu = np.concatenate([np.asarray(inp["w_gate_up"], np.float32)[0],
                          np.asarray(inp["b_gate"], np.float32)[0][None, :]], axis=0)
    return {
        "w_fm": w_fm, "w_tm": w_tm,
        "w_out": np.ascontiguousarray(np.asarray(inp["w_out"], np.float32)[0]),
        "w_g": np.ascontiguousarray(np.asarray(inp["w_ffn_gate"], np.float32)[0]),
        "w_u": np.ascontiguousarray(np.asarray(inp["w_ffn_up"], np.float32)[0]),
        "w_d": np.ascontiguousarray(np.asarray(inp["w_ffn_down"], np.float32)[0]),
        "consts": c, "wgu": np.ascontiguousarray(wgu),
    }


def build_nc(debug=False, nseq=2):
    nc = bass.Bass("TRN2", target_bir_lowering=False)
    x_d = nc.dram_tensor("x", [2, S, D], F32, kind="ExternalInput").ap()
    wfm_d = nc.dram_tensor("w_fm", [D, NFM], F32, kind="ExternalInput").ap()
    wtm_d = nc.dram_tensor("w_tm", [D, NTM], F32, kind="ExternalInput").ap()
    wout_d = nc.dram_tensor("w_out", [D, D], F32, kind="ExternalInput").ap()
    wg_d = nc.dram_tensor("w_g", [D, DFF], F32, kind="ExternalInput").ap()
    wu_d = nc.dram_tensor("w_u", [D, DFF], F32, kind="ExternalInput").ap()
    wd_d = nc.dram_tensor("w_d", [DFF, D], F32, kind="ExternalInput").ap()
    c_d = nc.dram_tensor("consts", [128, NCONST], F32, kind="ExternalInput").ap()
    wgu_d = nc.dram_tensor("wgu", [17, 256], F32, kind="ExternalInput").ap()
    y_d = nc.dram_tensor("y", [2, S, D], F32, kind="ExternalOutput").ap()
    x1_d = nc.dram_tensor("x1_scratch", [S, D], F32, kind="Internal").ap()
    dbg = {}
    if debug:
        for nm, shp in (("d_qaT", [128, 4, S]), ("d_oa", [S, 512]), ("d_ob", [S, 512]), ("d_sc", [128, S]),
                        ("d_x1", [S, D])):
            dbg[nm] = nc.dram_tensor(nm, shp, F32 if nm in ("d_sc", "d_x1") else BF16, kind="ExternalOutput").ap()

    P = Prog(nc)
    cmin_t = P.sbuf("cmin", [128, C_MIN], F32)
    cmin = Buf(cmin_t[:], "cmin")
    identb_t = P.sbuf("identb", [128, 128], BF16)
    identb = Buf(identb_t[:], "identb")
    wgu_t = P.sbuf("wgu", [17, 256], F32)
    wgu = Buf(wgu_t[:], "wgu")
    obT_t = P.sbuf("obT", [128, 4, S], BF16)
    stat_t = P.sbuf("stat", [128, 64], F32)
    ARENA_F = 47800
    ar = Arena(P, ARENA_F)
    OA_OFF = ARENA_F - 4096

    pf_t = [P.psum("pf%d" % i, [128, 512], F32) for i in range(6)]
    pb_t = [P.psum("pb%d" % i, [128, 1024], BF16) for i in range(2)]
    pf = [Buf(t[:], "pf") for t in pf_t]
    pb = [Buf(t[:], "pb") for t in pb_t]

    ident_f = cmin_t[:, C_ID:C_ID + 128]
    u01 = cmin_t[:, C_U01:C_U01 + 128]
    uneg = cmin_t[:, C_UNEG:C_UNEG + 128]
    lneg = cmin_t[:, C_LNEG:C_LNEG + 128]
    dmask = cmin_t[:, C_DMASK:C_DMASK + 128]
    gmixT = cmin_t[:, C_GMIX:C_GMIX + 8]
    gffnT = cmin_t[:, C_GFFN:C_GFFN + 8]
    cfar = cmin_t[:, C_CFAR:C_CFAR + 8]

    P.dma("sync", cmin.ap, c_d[:, 0:C_MIN], "cmin", writes=[cmin])
    P.dma("sync", wgu.ap, wgu_d, "wgu", writes=[wgu])
    P.copy("vector", identb.ap, ident_f, reads=[cmin], writes=[identb])

    stat_bufs = [Buf(stat_t[:, 4 * i:4 * i + 4], "stat%d" % i) for i in range(8)]
    stat_ctr = [0]

    def next_stat():
        b = stat_bufs[stat_ctr[0] % 8]
        stat_ctr[0] += 1
        return b

    evac_ctr = [0]

    def evac(out, in_, reads, writes=()):
        evac_ctr[0] += 1
        eng = "scalar" if evac_ctr[0] % 2 else "vector"
        return P.copy(eng, out, in_, reads=reads, writes=writes)

    def rms_to_T(src_buf, src_ap, xs_buf, dstT_ap, gT, pbuf):
        st = next_stat()
        P.act(xs_buf.ap, src_ap, AF.Square, reads=[src_buf], writes=[xs_buf, st], accum=st.ap[:, 0:1])
        P.act(st.ap[:, 1:2], st.ap[:, 0:1], AF.Ln, reads=[st], writes=[st], scale=1.0 / D, bias=EPS)
        P.act(st.ap[:, 2:3], st.ap[:, 1:2], AF.Exp, reads=[st], writes=[st], scale=-0.5)
        P.ts("vector", xs_buf.ap, src_ap, st.ap[:, 2:3], None, ALU.mult, reads=[src_buf, st], writes=[xs_buf])
        pv = pbuf.ap.rearrange("p (a b) -> p a b", b=128)
        for kc in range(8):
            P.transpose(pv[:, kc, :], xs_buf.ap[:, kc * 128:(kc + 1) * 128], identb.ap,
                        reads=[xs_buf, identb], writes=[pbuf])
        P.tt("vector", dstT_ap, pv, gT.unsqueeze(2).to_broadcast([128, 8, 128]), ALU.mult,
             reads=[pbuf, cmin], writes=[])
        return st

    for seq in range(nseq):
        P.barrier()
        ar.reset(0)
        qaT = ar.alloc([128, 4, S], BF16)
        kaT = ar.alloc([128, 4, S], BF16)
        va = ar.alloc([128, NT, 8, 66], BF16)
        qiT = ar.alloc([96, 3, S], BF16)
        kiT = ar.alloc([96, S], BF16)
        wi = ar.alloc([128, NT, 8], F32)
        AB_END = ar.off
        wfm = ar.alloc([128, 8, NFM], BF16)
        wtm = ar.alloc([128, 8, NTM], BF16)
        wfm_b, wtm_b = Buf(wfm, "wfm"), Buf(wtm, "wtm")
        glan = ar.alloc([128, 512], F32)
        glan_b = Buf(glan, "glan")
        hTs = [ar.alloc([128, 8, 512], BF16) for _ in range(2)]
        hT_bs = [Buf(hTs[i], "hT%d" % i) for i in range(2)]
        xin = [Buf(ar.alloc([128, D], F32), "xin%d" % i) for i in range(2)]
        xs = [Buf(ar.alloc([128, D], BF16), "xs%d" % i) for i in range(2)]
        qbT = Buf(ar.alloc([128, 2, 512], F32), "qbT")
        kbT = Buf(ar.alloc([128, 2, 512], F32), "kbT")
        gdT = Buf(ar.alloc([17, 512], F32), "gdT")
        kbtm = [Buf(ar.alloc([128, 256], F32), "kbtm") for _ in range(2)]
        vbtm = [Buf(ar.alloc([128, 512], BF16), "vbtm") for _ in range(2)]
        sg = [Buf(ar.alloc([128, 512], F32), "sg") for _ in range(2)]
        etmp = Buf(ar.alloc([128, 256], F32), "etmp")
        lsb = Buf(ar.alloc([128, 256], F32), "lsb")
        e1Ts = [Buf(ar.alloc([128, 2, 128], F32), "e1T") for _ in range(2)]
        e2T = Buf(ar.alloc([128, 2, 128], F32), "e2T")
        e3 = Buf(ar.alloc([128, 256], F32), "e3")
        qinTs = [Buf(ar.alloc([128, 2, 128], BF16), "qinT") for _ in range(2)]
        kinTs = [Buf(ar.alloc([128, 2, 128], BF16), "kinT") for _ in range(2)]
        kdecs = [Buf(ar.alloc([128, 256], BF16), "kdec") for _ in range(2)]
        attS = [Buf(ar.alloc([128, 128], BF16), "attS") for _ in range(2)]
        on = Buf(ar.alloc([128, 512], F32), "on")
        obss = [Buf(ar.alloc([128, 512], BF16), "obs%d" % i) for i in range(2)]
        stf = Buf(ar.alloc([128, 2, 128], F32), "stf")
        stb = Buf(ar.alloc([128, 2, 128], BF16), "stb")

        P.dma("gpsimd", wfm, wfm_d.rearrange("(kc p) n -> p kc n", p=128), "wfm", writes=[wfm_b])
        P.dma("gpsimd", wtm, wtm_d.rearrange("(kc p) n -> p kc n", p=128), "wtm", writes=[wtm_b])
        P.dma("sync", glan, c_d[:, C_GLAN:C_GLAN + 512], "glan", writes=[glan_b])
        P.memset("gpsimd", va[:, :, :, 64:66], 1.0)
        P.memset("gpsimd", gdT.ap, 1.0, writes=[gdT])
        P.memset("vector", stf.ap, 0.0, writes=[stf])
        P.memset("vector", stb.ap, 0.0, writes=[stb])

        fm_off = {}
        o = 0
        for nm, m in FM_TILES:
            fm_off[nm] = (o, m)
            o += m
        mm_ctr = [0]
        zps = Buf(pf_t[2][:, 0:256], "zps")
        rps = Buf(pf_t[2][:, 256:512], "rps")
        cps = Buf(pf_t[3][:, 0:256], "cps")
        ops_ = pf[4]
        aps2 = [Buf(pf_t[5][:, 0:128], "aps0"), Buf(pf_t[5][:, 128:256], "aps1")]
        kvp = Buf(pf_t[5][:, 256:512], "kvp")

        def prep(tb):
            hT, hT_b = hTs[tb % 2], hT_bs[tb % 2]
            for i in range(4):
                t = tb * 4 + i
                xb = xin[t % 2]
                P.dma("sync", xb.ap, x_d[seq, t * 128:(t + 1) * 128, :], "xin%d" % (t % 2), writes=[xb])
                st = next_stat()
                xsb = xs[t % 2]
                P.act(xsb.ap, xb.ap, AF.Square, reads=[xb], writes=[xsb, st], accum=st.ap[:, 0:1])
                P.act(st.ap[:, 1:2], st.ap[:, 0:1], AF.Ln, reads=[st], writes=[st], scale=1.0 / D, bias=EPS)
                P.act(st.ap[:, 2:3], st.ap[:, 1:2], AF.Exp, reads=[st], writes=[st], scale=-0.5)
                P.ts("vector", xsb.ap, xb.ap, st.ap[:, 2:3], None, ALU.mult, reads=[xb, st], writes=[xsb])
                pbuf = pb[t % 2]
                pv = pbuf.ap.rearrange("p (a b) -> p a b", b=128)
                for kc in range(8):
                    P.transpose(pv[:, kc, :], xsb.ap[:, kc * 128:(kc + 1) * 128], identb.ap,
                                reads=[xsb, identb], writes=[pbuf])
                P.tt("vector", hT[:, :, i * 128:(i + 1) * 128], pv,
                     gmixT.unsqueeze(2).to_broadcast([128, 8, 128]), ALU.mult,
                     reads=[pbuf, cmin], writes=[hT_b])

        def fm_block(tb):
            hT, hT_b = hTs[tb % 2], hT_bs[tb % 2]
            cs = slice(tb * 512, (tb + 1) * 512)
            for nm, m in FM_TILES:
                off, _ = fm_off[nm]
                ps = pf[mm_ctr[0] % 2]
                mm_ctr[0] += 1
                for kc in range(8):
                    P.matmul(ps.ap[0:m, :], wfm[:, kc, off:off + m], hT[:, kc, :], kc == 0, kc == 7,
                             reads=[wfm_b, hT_b], writes=[ps])
                if nm.startswith("qa"):
                    evac(qaT[:, int(nm[2]), cs], ps.ap, [ps])
                elif nm.startswith("ka"):
                    evac(kaT[:, int(nm[2]), cs], ps.ap, [ps])
                elif nm.startswith("qi"):
                    evac(qiT[:, int(nm[2]), cs], ps.ap[0:96, :], [ps])
                elif nm == "ki":
                    evac(kiT[:, cs], ps.ap[0:96, :], [ps])
                elif nm.startswith("qb"):
                    evac(qbT.ap[:, int(nm[2]), :], ps.ap, [ps], [qbT])
                elif nm.startswith("kb"):
                    evac(kbT.ap[:, int(nm[2]), :], ps.ap, [ps], [kbT])
                else:
                    evac(gdT.ap[0:16, :], ps.ap[0:16, :], [ps], [gdT])

        def gla_s1(t):
            tb, i = t // 4, t % 4
            hT, hT_b = hTs[tb % 2], hT_bs[tb % 2]
            tcs = slice(i * 128, (i + 1) * 128)
            kb_s, vb_s, sg_s = kbtm[t % 2], vbtm[t % 2], sg[t % 2]
            e1T, qinT, kinT, kdec = e1Ts[t % 2], qinTs[t % 2], kinTs[t % 2], kdecs[t % 2]
            P.matmul(zps.ap, gdT.ap[0:17, tcs], wgu.ap, True, True, reads=[gdT, wgu], writes=[zps])
            P.act(etmp.ap, zps.ap, AF.Exp, reads=[zps], writes=[etmp], scale=-1.0)
            P.act(lsb.ap, etmp.ap, AF.Ln, reads=[etmp], writes=[lsb], bias=1.0)
            for g, (goff, gn) in enumerate(((0, 512), (512, 264), (776, 512), (1288, 512))):
                ps = pf[mm_ctr[0] % 2]
                mm_ctr[0] += 1
                for kc in range(8):
                    P.matmul(ps.ap[:, 0:gn], hT[:, kc, tcs], wtm[:, kc, goff:goff + gn], kc == 0, kc == 7,
                             reads=[wtm_b, hT_b], writes=[ps])
                if g == 0:
                    evac(va[:, t, :, 0:64], ps.ap.rearrange("p (h d) -> p h d", d=64), [ps])
                elif g == 1:
                    P.copy("vector", kb_s.ap, ps.ap[:, 0:256], reads=[ps], writes=[kb_s])
                    P.copy("vector", wi[:, t, :], ps.ap[:, 256:264], reads=[ps])
                elif g == 2:
                    P.copy("scalar", vb_s.ap, ps.ap, reads=[ps], writes=[vb_s])
                else:
                    P.act(sg_s.ap, ps.ap, AF.Silu, reads=[ps], writes=[sg_s])
            cv = cps.ap.rearrange("p (a b) -> p a b", b=128)
            for pr in range(2):
                P.matmul(cv[:, pr, :], lsb.ap[:, pr * 128:(pr + 1) * 128], uneg, True, True,
                         reads=[lsb, cmin], writes=[cps])
            P.matmul(rps.ap, lneg, lsb.ap, True, True, reads=[lsb, cmin], writes=[rps])
            P.act(e1T.ap, cv, AF.Exp, reads=[cps], writes=[e1T])
            P.act(e2T.ap, cv, AF.Exp, reads=[cps], writes=[e2T], scale=-1.0)
            P.act(e3.ap, rps.ap, AF.Exp, reads=[rps], writes=[e3])
            P.stt(qinT.ap, qbT.ap[:, :, tcs], 0.125, e1T.ap, ALU.mult, ALU.mult,
                  reads=[qbT, e1T], writes=[qinT])
            P.tt("vector", kinT.ap, kbT.ap[:, :, tcs], e2T.ap, ALU.mult, reads=[kbT, e2T], writes=[kinT])
            P.tt("gpsimd", kdec.ap, kb_s.ap, e3.ap, ALU.mult, reads=[kb_s, e3], writes=[kdec])

        def gla_s2(t):
            vb_s, sg_s = vbtm[t % 2], sg[t % 2]
            e1T, qinT, kinT, kdec = e1Ts[t % 2], qinTs[t % 2], kinTs[t % 2], kdecs[t % 2]
            for pr in range(2):
                for hh in range(2):
                    h = 2 * pr + hh
                    hp = slice(hh * 64, hh * 64 + 64)
                    aps = aps2[hh]
                    P.matmul(aps.ap, kinT.ap[hp, pr, :], qinT.ap[hp, pr, :], True, True,
                             reads=[kinT, qinT], writes=[aps])
                    asb = attS[hh]
                    P.tt("vector", asb.ap, aps.ap, u01, ALU.mult, reads=[aps, cmin], writes=[asb])
                    P.matmul(ops_.ap[:, h * 128:(h + 1) * 128], asb.ap, vb_s.ap[:, h * 128:(h + 1) * 128],
                             True, False, reads=[asb, vb_s], writes=[ops_])
                    P.matmul(ops_.ap[:, h * 128:(h + 1) * 128], qinT.ap[hp, pr, :], stb.ap[hp, pr, :],
                             False, True, reads=[qinT, stb], writes=[ops_])
                P.matmul(kvp.ap, kdec.ap[:, pr * 128:(pr + 1) * 128],
                         vb_s.ap[:, pr * 256:(pr + 1) * 256], True, True, reads=[kdec, vb_s], writes=[kvp])
                for hh in range(2):
                    hp = slice(hh * 64, hh * 64 + 64)
                    P.stt(stf.ap[hp, pr, :], stf.ap[hp, pr, :], e1T.ap[hp, pr, 127:128],
                          kvp.ap[hp, hh * 128:(hh + 1) * 128], ALU.mult, ALU.add,
                          reads=[stf, e1T, kvp], writes=[stf])
            P.copy("gpsimd", stb.ap, stf.ap, reads=[stf], writes=[stb])
            st = next_stat()
            P.act(on.ap, ops_.ap, AF.Square, reads=[ops_], writes=[on])
            P.reduce(st.ap[:, 0:4], on.ap.rearrange("p (h v) -> p h v", v=128), ALU.add, reads=[on], writes=[st])
            st2 = next_stat()
            P.act(st2.ap[:, 0:4], st.ap[:, 0:4], AF.Ln, reads=[st], writes=[st2], scale=1.0 / 128, bias=EPS)
            st3 = next_stat()
            P.act(st3.ap[:, 0:4], st2.ap[:, 0:4], AF.Exp, reads=[st2], writes=[st3], scale=-0.5)
            P.tt("vector", on.ap.rearrange("p (h v) -> p h v", v=128),
                 ops_.ap.rearrange("p (h v) -> p h v", v=128),
                 st3.ap[:, 0:4].unsqueeze(2).to_broadcast([128, 4, 128]), ALU.mult,
                 reads=[ops_, st3], writes=[on])
            P.tt("gpsimd", on.ap, on.ap, glan, ALU.mult, reads=[on, glan_b], writes=[on])
            obs = obss[t % 2]
            P.tt("vector", obs.ap, on.ap, sg_s.ap, ALU.mult, reads=[on, sg_s], writes=[obs])
            if debug and seq == 0:
                P.dma("sync", dbg["d_ob"][t * 128:(t + 1) * 128, :], obs.ap, "dbg_ob", reads=[obs])

        def gla_s3(t):
            obs = obss[t % 2]
            pbuf = pb[t % 2]
            pv = pbuf.ap.rearrange("p (a b) -> p a b", b=128)
            for j in range(4):
                P.transpose(pv[:, j, :], obs.ap[:, j * 128:(j + 1) * 128], identb.ap,
                            reads=[obs, identb], writes=[pbuf])
            P.copy("scalar", obT_t[:, :, t * 128:(t + 1) * 128], pv[:, 0:4, :], reads=[pbuf])

        prep(0)
        for t in range(NT):
            if t % 4 == 0:
                fm_block(t // 4)
                if t // 4 + 1 < 4:
                    prep(t // 4 + 1)
            gla_s1(t)
            if t >= 1:
                gla_s2(t - 1)
            if t >= 2:
                gla_s3(t - 2)
        gla_s2(NT - 1)
        gla_s3(NT - 2)
        gla_s3(NT - 1)
        if debug and seq == 0:
            P.barrier()
            P.dma("sync", dbg["d_qaT"], qaT, "dbg_qaT")

        P.barrier()
        ar.reset(AB_END)
        bias8 = ar.alloc([128, 2, 8, 128], BF16)
        bias8_b = Buf(bias8, "bias8")
        biasT = ar.alloc([128, 2, 8, 128], F32)
        biasT_b = Buf(biasT, "biasT")
        P.dma("sync", biasT.rearrange("p a b c -> p (a b c)"), c_d[:, C_BIAS:C_BIAS + 2048], "biasT", writes=[biasT_b])
        P.ts("gpsimd", bias8, biasT, 8.0, None, ALU.mult, reads=[biasT_b], writes=[bias8_b])
        sc = [Buf(ar.alloc([128, S], F32), "sc%d" % i) for i in range(4)]
        junkB = ar.alloc([128, S], BF16)
        msk = [Buf(ar.alloc([128, S], BF16), "msk%d" % i) for i in range(4)]
        mskT = [Buf(ar.alloc([128, NT, 128], BF16), "mskT%d" % i) for i in range(4)]
        rbf = [Buf(ar.alloc([128, 256], BF16), "rbf%d" % i) for i in range(3)]
        diagw = [Buf(ar.alloc([128, 8, 128], BF16), "diagw%d" % i) for i in range(2)]
        dmask_bf = Buf(ar.alloc([128, 128], BF16), "dmaskbf")
        P.copy("vector", dmask_bf.ap, dmask, reads=[cmin], writes=[dmask_bf])
        NPMAX = 5
        ebuf = [Buf(ar.alloc([128, 4, 128], BF16), "e%d" % i) for i in range(NPMAX)]
        pbuf_ = [Buf(ar.alloc([128, 4, 128], BF16), "p%d" % i) for i in range(NPMAX)]
        oasb = Buf(ar.alloc([128, 512], BF16), "oasb")
        bis_t = [ar.alloc([128, 8], F32) for i in range(2)]
        midb = [Buf(b_[:, 0:2], "mid") for b_ in bis_t]
        cntb = [[Buf(b_[:, 2:3], "cnt0"), Buf(b_[:, 3:4], "cnt1")] for b_ in bis_t]
        tbb = [Buf(b_[:, 4:6], "tb") for b_ in bis_t]
        thrb = [Buf(b_[:, 6:8], "thr") for b_ in bis_t]
        nrm = Buf(ar.alloc([128, 16], F32), "nrm")
        actb_t = ar.alloc([128, 8], F32)
        nmid = Buf(actb_t[:, 0:1], "nmid")
        ssum = Buf(actb_t[:, 1:2], "ssum")
        sgnb = Buf(actb_t[:, 2:3], "sgnb")
        thrA = Buf(actb_t[:, 3:4], "thrA")
        junkB2 = ar.alloc([128, S], BF16)
        assert ar.off <= OA_OFF, ("phase B arena", ar.off, OA_OFF)
        oaT = ar.alloc([128, 4, S], BF16, at=OA_OFF)
        r_ctr = [0]
        lgb = [pf[2], pf[3], pb[1], pf[0], pf[1]]
        lgv = [pf_t[2][:], pf_t[3][:], pb_t[1][:].bitcast(F32), pf_t[0][:], pf_t[1][:]]
        pbm = Buf(pb_t[0][:, 0:512], "pbm")
        pbo = pbm
        sps = Buf(pb_t[0][:, 512:1024].bitcast(F32), "sps")
        ops2 = [pf[4], pf[5]]
        ov = [b_.ap[:, 0:264].rearrange("p (h d) -> p h d", d=66) for b_ in ops2]

        def idx_group(g4):
            qts = [4 * g4 + k for k in range(4)]
            for k, qt in enumerate(qts):
                sk = (qt + 1) * 128
                scb = sc[k]
                qcs = slice(qt * 128, (qt + 1) * 128)
                dg = diagw[qt % 2]
                for h in range(8):
                    P.ts("gpsimd", dg.ap[:, h, :], ident_f, wi[:, qt, h:h + 1], None, ALU.mult,
                         reads=[cmin], writes=[dg])
                for c0 in range(0, sk, 256):
                    w = min(256, sk - c0)
                    last_chunk = (c0 + w == sk)

                    def emit_d(h, c0=c0, w=w):
                        hp = slice((h % 3) * 32, (h % 3) * 32 + 32)
                        ps = pf[h % 2]
                        P.matmul(ps.ap[:, 0:w], qiT[hp, h // 3, qcs], kiT[hp, c0:c0 + w], True, True, writes=[ps])
                        rb_ = rbf[r_ctr[0] % 3]
                        r_ctr[0] += 1
                        P.act(rb_.ap[:, 0:w], ps.ap[:, 0:w], AF.Relu, reads=[ps], writes=[rb_])
                        return rb_

                    rbs = {0: emit_d(0)}
                    for h in range(8):
                        if h + 1 < 8:
                            rbs[h + 1] = emit_d(h + 1)
                        P.matmul(sps.ap[:, 0:w], dg.ap[:, h, :], rbs[h].ap[:, 0:w], h == 0,
                                 h == 7 and not last_chunk, reads=[dg, rbs[h]], writes=[sps])
                    if last_chunk:
                        P.matmul(sps.ap[:, w - 128:w], identb.ap, dmask_bf.ap, False, True,
                                 reads=[identb, dmask_bf], writes=[sps])
                    P.copy("vector", scb.ap[:, c0:c0 + w], sps.ap[:, 0:w], reads=[sps], writes=[scb])
                if debug and seq == 0 and qt == 5:
                    P.dma("sync", dbg["d_sc"], scb.ap, "dbg_sc", reads=[scb])

        def bis_group(g4):
            qts = [4 * g4 + k for k in range(4)]
            if g4 == 0:
                pairs, act_k = [(), (2, 3)], None
            else:
                pairs, act_k = [(0, 1), (2, 3)], None
            act_pairs = [pi for pi in range(2) if len(pairs[pi])]
            for pi in act_pairs:
                P.memset("vector", midb[pi].ap, 0.0, writes=[midb[pi]])
            if act_k is not None:
                P.memset("gpsimd", nmid.ap, 0.0, writes=[nmid])
                sk_a = (qts[act_k] + 1) * 128
            for it in range(1, N_BIS + 1):
                step = R_BIS / (2.0 ** it)
                for pi in act_pairs:
                    for a, k in enumerate(pairs[pi]):
                        sk = (qts[k] + 1) * 128
                        P.ts("vector", junkB[:, 0:sk], sc[k].ap[:, 0:sk], midb[pi].ap[:, a:a + 1], None,
                             ALU.is_ge, ALU.add, reads=[sc[k], midb[pi]], writes=[cntb[pi][a]],
                             accum=cntb[pi][a].ap)
                if act_k is not None:
                    P.act(junkB2[:, 0:sk_a], sc[act_k].ap[:, 0:sk_a], AF.Sign, reads=[sc[act_k], nmid],
                          writes=[ssum], bias=nmid.ap, accum=ssum.ap)
                    P.act(sgnb.ap, ssum.ap, AF.Sign, reads=[ssum], writes=[sgnb], bias=float(sk_a) - 511.5)
                    P.act(nmid.ap, sgnb.ap, AF.Identity, reads=[sgnb, nmid], writes=[nmid], scale=-step, bias=nmid.ap)
                for pi in act_pairs:
                    n_ = len(pairs[pi])
                    P.ts("vector", tbb[pi].ap[:, 0:n_], bis_t[pi][:, 2:2 + n_], 255.5, 2.0 * step, ALU.is_ge, ALU.mult,
                         reads=[cntb[pi][a] for a in range(n_)], writes=[tbb[pi]])
                for pi in act_pairs:
                    n_ = len(pairs[pi])
                    P.stt(midb[pi].ap[:, 0:n_], tbb[pi].ap[:, 0:n_], -step, midb[pi].ap[:, 0:n_], ALU.add, ALU.add,
                          reads=[tbb[pi], midb[pi]], writes=[midb[pi]])
                yield
            last_step = R_BIS / (2.0 ** N_BIS)
            for pi in act_pairs:
                n_ = len(pairs[pi])
                P.ts("vector", thrb[pi].ap[:, 0:n_], midb[pi].ap[:, 0:n_], -last_step, None, ALU.add,
                     reads=[midb[pi]], writes=[thrb[pi]])
            if act_k is not None:
                P.act(thrA.ap, nmid.ap, AF.Identity, reads=[nmid], writes=[thrA], scale=-1.0, bias=-last_step)
            thr_of = {}
            for pi in act_pairs:
                for a, k in enumerate(pairs[pi]):
                    thr_of[k] = (thrb[pi], thrb[pi].ap[:, a:a + 1])
            if act_k is not None:
                thr_of[act_k] = (thrA, thrA.ap)
            for k, qt in enumerate(qts):
                sk = (qt + 1) * 128
                if qt >= 2:
                    P.ts("vector", msk[k].ap[:, 0:sk], sc[k].ap[:, 0:sk], thr_of[k][1], None, ALU.is_ge,
                         reads=[sc[k], thr_of[k][0]], writes=[msk[k]])
                else:
                    P.ts("vector", msk[k].ap[:, 0:sk], sc[k].ap[:, 0:sk], -1.0e29, None, ALU.is_ge,
                         reads=[sc[k]], writes=[msk[k]])
            for k, qt in enumerate(qts):
                nkb = qt + 1
                mT = mskT[k]
                pv = pbm.ap.rearrange("p (a b) -> p a b", b=128)
                for c0 in range(0, nkb, 4):
                    n = min(4, nkb - c0)
                    for kk in range(n):
                        P.transpose(pv[:, kk, :], msk[k].ap[:, (c0 + kk) * 128:(c0 + kk + 1) * 128], identb.ap,
                                    reads=[msk[k], identb], writes=[pbm])
                    P.copy("scalar", mT.ap[:, c0:c0 + n, :], pv[:, 0:n, :], reads=[pbm], writes=[mT])

        def attn_group(g4):
            NPIPE = 5 if g4 == 3 else 3
            qts = [4 * g4 + k for k in range(4)]
            items = []
            for k, qt in enumerate(qts):
                near = [kb for kb in (qt - 1, qt) if kb >= 0]
                far = list(range(0, max(qt - 1, 0)))
                groups = [("far", far[c0:c0 + 4]) for c0 in range(0, len(far), 4)] + [("near", near)]
                for h in range(8):
                    for gi, (kind, kbs) in enumerate(groups):
                        items.append((k, qt, h, kind, kbs, gi == 0, gi == len(groups) - 1))

            def stage1(item, i):
                k, qt, h, kind, kbs, _, _ = item
                qcs = slice(qt * 128, (qt + 1) * 128)
                j, hp = h // 2, slice((h % 2) * 64, (h % 2) * 64 + 64)
                n = len(kbs)
                lps = lgb[i % NPIPE]
                lv = lgv[i % NPIPE].rearrange("p (a b) -> p a b", b=128)
                for kk, kb in enumerate(kbs):
                    if kind == "far":
                        P.matmul(lv[:, kk, :], kaT[hp, j, kb * 128:(kb + 1) * 128], qaT[hp, j, qcs], True, True,
                                 writes=[lps])
                    else:
                        which = 0 if kb == qt else 1
                        P.matmul(lv[:, kk, :], kaT[hp, j, kb * 128:(kb + 1) * 128], qaT[hp, j, qcs], True, False,
                                 writes=[lps])
                        P.matmul(lv[:, kk, :], identb.ap, bias8[:, which, h, :], False, True,
                                 reads=[identb, bias8_b], writes=[lps])
                eb = ebuf[i % NPIPE]
                pbf = pbuf_[i % NPIPE]
                if kind == "far":
                    P.act(eb.ap[:, 0:n, :], lv[:, 0:n, :], AF.Exp, reads=[lps, cmin], writes=[eb],
                          scale=0.125, bias=cfar[:, h:h + 1])
                else:
                    P.act(eb.ap[:, 0:n, :], lv[:, 0:n, :], AF.Exp, reads=[lps], writes=[eb], scale=0.125)
                meng = "vector" if (g4 == 3 and i % 2 == 1) else "gpsimd"
                P.tt(meng, pbf.ap[:, 0:n, :], eb.ap[:, 0:n, :], mskT[k].ap[:, kbs[0]:kbs[0] + n, :], ALU.mult,
                     reads=[eb, mskT[k]], writes=[pbf])

            def stage2(item, i):
                k, qt, h, kind, kbs, first_g, last_g = item
                qcs = slice(qt * 128, (qt + 1) * 128)
                n = len(kbs)
                pbf = pbuf_[i % NPIPE]
                for kk, kb in enumerate(kbs):
                    P.matmul(ov[h // 4][:, h % 4, 0:65], pbf.ap[:, kk, :], va[:, kb, h, 0:65],
                             first_g and kk == 0, last_g and kk == n - 1, reads=[pbf], writes=[ops2[h // 4]])
                if h == 7 and last_g:
                    for hh in range(2):
                        P.act(nrm.ap[:, hh * 4:(hh + 1) * 4].unsqueeze(2), ov[hh][:, :, 64:65], AF.Ln,
                              reads=[ops2[hh]], writes=[nrm])
                    P.act(nrm.ap[:, 8:16], nrm.ap[:, 0:8], AF.Exp, reads=[nrm], writes=[nrm], scale=-1.0)
                    for h2 in range(8):
                        P.act(oasb.ap[:, h2 * 64:(h2 + 1) * 64], ov[h2 // 4][:, h2 % 4, 0:64], AF.Copy,
                              reads=[ops2[h2 // 4], nrm], writes=[oasb], scale=nrm.ap[:, 8 + h2:9 + h2])
                    if debug and seq == 0:
                        P.dma("sync", dbg["d_oa"][qt * 128:(qt + 1) * 128, :], oasb.ap, "dbg_oa", reads=[oasb])
                    pv = pbo.ap.rearrange("p (a b) -> p a b", b=128)
                    for j2 in range(4):
                        P.transpose(pv[:, j2, :], oasb.ap[:, j2 * 128:(j2 + 1) * 128], identb.ap,
                                    reads=[oasb, identb], writes=[pbo])
                    P.copy("scalar", oaT[:, :, qcs], pv[:, 0:4, :], reads=[pbo])

            LA = NPIPE - 1
            for i in range(len(items) + LA):
                if i < len(items):
                    stage1(items[i], i)
                if i >= LA:
                    stage2(items[i - LA], i - LA)
                yield

        N_ITEMS = [48, 80, 112, 144]
        idx_group(0)
        for _ in bis_group(0):
            pass
        for g4 in range(4):
            if g4 + 1 < 4:
                idx_group(g4 + 1)
                ga = attn_group(g4)
                per = (N_ITEMS[g4] + 2 + N_BIS - 1) // N_BIS
                for _ in bis_group(g4 + 1):
                    for _i in range(per):
                        next(ga, None)
                assert next(ga, "done") == "done", "attention items not fully emitted before masks"
            else:
                for _ in attn_group(g4):
                    pass

        P.barrier()
        ar.reset(0)
        h2T = ar.alloc([128, 8, S], BF16)
        wd = ar.alloc([128, NJ, D], BF16)
        wd_b = Buf(wd, "wd")
        NR = 3
        wgr = [Buf(ar.alloc([128, 8, 256], BF16), "wg%d" % i) for i in range(NR)]
        wur = [Buf(ar.alloc([128, 8, 256], BF16), "wu%d" % i) for i in range(NR)]
        gfin = ar.alloc([128, D], F32)
        gfin_b = Buf(gfin, "gfin")
        C2_START = ar.off
        wout = ar.alloc([128, 8, D], BF16)
        wout_b = Buf(wout, "wout")
        wgv = wg_d.rearrange("(kc p) n -> p kc n", p=128)
        wuv = wu_d.rearrange("(kc p) n -> p kc n", p=128)
        P.dma("gpsimd", wout, wout_d.rearrange("(kc p) n -> p kc n", p=128), "wout", writes=[wout_b])
        for q4 in range(2):
            P.dma("gpsimd", wd[:, q4 * 11:(q4 + 1) * 11, :],
                  wd_d[q4 * 1408:(q4 + 1) * 1408, :].rearrange("(j p) n -> p j n", p=128), "wd", writes=[wd_b])
        for r in range(NR):
            P.dma("gpsimd", wgr[r].ap, wgv[:, :, r * 256:(r + 1) * 256], "wg%d" % r, writes=[wgr[r]])
            P.dma("gpsimd", wur[r].ap, wuv[:, :, r * 256:(r + 1) * 256], "wu%d" % r, writes=[wur[r]])
        P.dma("sync", gfin, c_d[:, C_GFIN:C_GFIN + 1024], "gfin", writes=[gfin_b])
        xin = [Buf(ar.alloc([128, D], F32), "xinC%d" % i) for i in range(2)]
        x1s = [Buf(ar.alloc([128, D], F32), "x1s%d" % i) for i in range(2)]
        xs = [Buf(ar.alloc([128, D], BF16), "xsC%d" % i) for i in range(2)]
        assert ar.off <= OA_OFF
        x1d_b = Buf(x1_d, "x1d")
        for t in range(NT):
            tcs = slice(t * 128, (t + 1) * 128)
            xb, x1b, xsb = xin[t % 2], x1s[t % 2], xs[t % 2]
            P.dma("sync", xb.ap, x_d[seq, tcs, :], "xinC%d" % (t % 2), writes=[xb])
            for half in range(2):
                ps = pf[(2 * t + half) % 4]
                for kc in range(8):
                    lhs = oaT[:, kc, tcs] if kc < 4 else obT_t[:, kc - 4, tcs]
                    P.matmul(ps.ap, lhs, wout[:, kc, half * 512:(half + 1) * 512], kc == 0, kc == 7,
                             reads=[wout_b], writes=[ps])
                P.tt("vector", x1b.ap[:, half * 512:(half + 1) * 512], xb.ap[:, half * 512:(half + 1) * 512], ps.ap,
                     ALU.add, reads=[xb, ps], writes=[x1b])
            P.dma("sync", x1_d[tcs, :], x1b.ap, "x1st", reads=[x1b], writes=[x1d_b])
            if debug and seq == 0:
                P.dma("sync", dbg["d_x1"][tcs, :], x1b.ap, "dbg_x1", reads=[x1b])
            rms_to_T(x1b, x1b.ap, xsb, h2T[:, :, tcs], gffnT, pb[t % 2])

        P.barrier()
        ar.reset(C2_START)
        actT = ar.alloc([128, NJ, 1024], BF16)
        actT_b = Buf(actT, "actT")
        x1r = [Buf(ar.alloc([128, D], F32), "x1r%d" % i) for i in range(2)]
        ysb = [Buf(ar.alloc([128, D], F32), "ysb%d" % i) for i in range(2)]
        osb = [Buf(ar.alloc([128, D], F32), "osb%d" % i) for i in range(2)]
        sgc = [Buf(ar.alloc([128, 512], F32), "sgc%d" % i) for i in range(2)]
        junkC = ar.alloc([128, D], BF16)
        ring = [0]
        cctr = [0]
        for cb in range(2):
            for gj in range(NJ // 2):
                r = ring[0] % NR
                ring[0] += 1
                if ring[0] > NR:
                    P.dma("gpsimd", wgr[r].ap, wgv[:, :, gj * 256:(gj + 1) * 256], "wg%d" % r, writes=[wgr[r]])
                    P.dma("gpsimd", wur[r].ap, wuv[:, :, gj * 256:(gj + 1) * 256], "wu%d" % r, writes=[wur[r]])
                for jj in range(2):
                    j = 2 * gj + jj
                    for hb in range(2):
                        tok = slice(cb * 1024 + hb * 512, cb * 1024 + (hb + 1) * 512)
                        gps = pf[(cctr[0] % 2) * 2]
                        ups = pf[(cctr[0] % 2) * 2 + 1]
                        sgb = sgc[cctr[0] % 2]
                        cctr[0] += 1
                        for kc in range(8):
                            P.matmul(gps.ap, wgr[r].ap[:, kc, jj * 128:(jj + 1) * 128], h2T[:, kc, tok], kc == 0, kc == 7,
                                     reads=[wgr[r]], writes=[gps])
                        for kc in range(8):
                            P.matmul(ups.ap, wur[r].ap[:, kc, jj * 128:(jj + 1) * 128], h2T[:, kc, tok], kc == 0, kc == 7,
                                     reads=[wur[r]], writes=[ups])
                        P.act(sgb.ap, gps.ap, AF.Silu, reads=[gps], writes=[sgb])
                        P.tt("vector", actT[:, j, hb * 512:(hb + 1) * 512], sgb.ap, ups.ap, ALU.mult,
                             reads=[sgb, ups], writes=[actT_b])
            for ti in range(8):
                t = cb * 8 + ti
                tcs = slice(t * 128, (t + 1) * 128)
                xr, yb, ob_ = x1r[t % 2], ysb[t % 2], osb[t % 2]
                P.dma("sync", xr.ap, x1_d[tcs, :], "x1r%d" % (t % 2), reads=[x1d_b], writes=[xr])
                for half in range(2):
                    ps = pf[4 + half]
                    for j in range(NJ):
                        P.matmul(ps.ap, actT[:, j, ti * 128:(ti + 1) * 128], wd[:, j, half * 512:(half + 1) * 512],
                                 j == 0, j == NJ - 1, reads=[actT_b, wd_b], writes=[ps])
                    P.tt("vector", yb.ap[:, half * 512:(half + 1) * 512], xr.ap[:, half * 512:(half + 1) * 512], ps.ap,
                         ALU.add, reads=[xr, ps], writes=[yb])
                st = next_stat()
                P.act(junkC, yb.ap, AF.Square, reads=[yb], writes=[st], accum=st.ap[:, 0:1])
                P.act(st.ap[:, 1:2], st.ap[:, 0:1], AF.Ln, reads=[st], writes=[st], scale=1.0 / D, bias=EPS)
                P.act(st.ap[:, 2:3], st.ap[:, 1:2], AF.Exp, reads=[st], writes=[st], scale=-0.5)
                P.stt(ob_.ap, yb.ap, st.ap[:, 2:3], gfin, ALU.mult, ALU.mult, reads=[yb, st, gfin_b], writes=[ob_])
                P.dma("sync", y_d[seq, tcs, :], ob_.ap, "yout%d" % (t % 2), reads=[ob_])

    P.emit(final_waits=["yout0", "yout1"])
    return nc


_NC_CACHE = {}


def kernel(**inputs):
    x = np.ascontiguousarray(np.asarray(inputs["x"], np.float32))
    lay = _host_layout(inputs)
    if "nc" not in _NC_CACHE:
        _NC_CACHE["nc"] = build_nc()
    nc = _NC_CACHE["nc"]
    in_maps = []
    for c in range(8):
        m = {"x": np.ascontiguousarray(x[2 * c:2 * c + 2])}
        m.update(lay)
        in_maps.append(m)
    res = run_bass_kernel_spmd(nc, in_maps, core_ids=list(range(8)))
    out = np.concatenate([np.asarray(r["y"], np.float32).reshape(2, S, D) for r in res.results], axis=0)
    return out
```

```python
import math
from contextlib import ExitStack

import numpy as np
import concourse.bass as bass
import concourse.mybir as mybir
from concourse.bass_utils import run_bass_kernel_spmd

F32 = mybir.dt.float32
BF16 = mybir.dt.bfloat16
ALU = mybir.AluOpType
AF = mybir.ActivationFunctionType
AX = mybir.AxisListType

S = 2048
D = 1024
NT = S // 128
DFF = 2816
NJ = DFF // 128
NEG = -1.0e30
EPS = 1e-6

O_QA, O_KA, O_VA, O_QI, O_KI, O_WI, O_QB, O_KB, O_VB, O_GD, O_OG = (
    0, 512, 1024, 1536, 1792, 1824, 1832, 2088, 2344, 2856, 2872)

FM_TILES = ([("qa%d" % j, 128) for j in range(4)] + [("ka%d" % j, 128) for j in range(4)]
            + [("qi%d" % j, 96) for j in range(3)] + [("ki", 96)]
            + [("qb%d" % j, 128) for j in range(2)] + [("kb%d" % j, 128) for j in range(2)]
            + [("gd", 16)])
NFM = sum(m for _, m in FM_TILES)
NTM = 512 + 264 + 512 + 512

C_ID, C_U01, C_UNEG, C_LNEG, C_DMASK, C_GMIX, C_GFFN, C_CFAR = 0, 128, 256, 384, 512, 640, 648, 656
C_MIN = 672
C_GLAN = 672
C_GFIN = C_GLAN + 512
C_BIAS = C_GFIN + 1024
NCONST = C_BIAS + 2048

R_BIS = 128.0
N_BIS = 17


class Buf:
    __slots__ = ("ap", "name", "last_w", "readers")

    def __init__(self, ap, name=""):
        self.ap = ap
        self.name = name
        self.last_w = None
        self.readers = []


class Op:
    __slots__ = ("eng", "fn", "deps", "needs_inc", "semval", "is_dma", "dsem", "dval", "phase")

    def __init__(self, eng, fn):
        self.eng = eng
        self.fn = fn
        self.deps = []
        self.needs_inc = False
        self.semval = None
        self.is_dma = False
        self.dsem = None
        self.dval = None


class Prog:
    ENGS = ("tensor", "vector", "scalar", "gpsimd", "sync")

    def __init__(self, nc):
        self.nc = nc
        self.ops = {e: [] for e in self.ENGS}
        self.es = ExitStack()
        self.dma_counts = {}
        self.dma_last = {}
        self.phase = 0

    def sbuf(self, name, shape, dt):
        return self.es.enter_context(self.nc.sbuf_tensor("sb_" + name, list(shape), dt))

    def psum(self, name, shape, dt):
        return self.es.enter_context(self.nc.psum_tensor("ps_" + name, list(shape), dt))

    def _dep(self, op, src):
        if src is None or src is op:
            return
        if src.eng == "tensor" and op.eng == "tensor" and not src.is_dma and not op.is_dma:
            return
        op.deps.append(src)
        if not src.is_dma:
            src.needs_inc = True

    def op(self, eng, fn, reads=(), writes=(), dma_key=None, extra=()):
        o = Op(eng, fn)
        o.phase = self.phase
        if dma_key is not None:
            o.is_dma = True
            o.dsem = dma_key
            self.dma_counts[dma_key] = self.dma_counts.get(dma_key, 0) + 1
            o.dval = 16 * self.dma_counts[dma_key]
            self.dma_last[dma_key] = o
        for s in extra:
            self._dep(o, s)
        for b in reads:
            self._dep(o, b.last_w)
        for b in writes:
            lw = b.last_w
            if lw is not None and not (lw.eng == eng and not lw.is_dma and not o.is_dma):
                self._dep(o, lw)
            for r in b.readers:
                if r.eng == eng and not r.is_dma and not o.is_dma:
                    continue
                self._dep(o, r)
        for b in reads:
            b.readers.append(o)
        for b in writes:
            b.last_w = o
            b.readers = []
        self.ops[eng].append(o)
        return o

    def barrier(self):
        lasts = []
        for e in self.ENGS:
            for o in reversed(self.ops[e]):
                if not o.is_dma:
                    lasts.append(o)
                    break
        lasts += list(self.dma_last.values())
        for e in self.ENGS:
            self.op(e, lambda eng: eng.nop(), extra=[l for l in lasts])
        self.phase += 1

    def dma(self, eng, out_ap, in_ap, key, reads=(), writes=()):
        return self.op(eng, lambda e: e.dma_start(out=out_ap, in_=in_ap), reads, writes, dma_key=key)

    def matmul(self, out, lhsT, rhs, start, stop, reads=(), writes=()):
        return self.op("tensor", lambda e: e.matmul(out, lhsT=lhsT, rhs=rhs, start=start, stop=stop), reads, writes)

    def transpose(self, out, in_, ident, reads=(), writes=()):
        return self.op("tensor", lambda e: e.transpose(out, in_, ident), reads, writes)

    def act(self, out, in_, func, reads=(), writes=(), bias=None, scale=None, accum=None):
        kw = {}
        if bias is not None:
            kw["bias"] = bias
        if scale is not None:
            kw["scale"] = scale
        if accum is not None:
            kw["accum_out"] = accum
        return self.op("scalar", lambda e: e.activation(out=out, in_=in_, func=func, **kw), reads, writes)

    def tt(self, eng, out, in0, in1, op, reads=(), writes=()):
        return self.op(eng, lambda e: e.tensor_tensor(out=out, in0=in0, in1=in1, op=op), reads, writes)

    def ts(self, eng, out, in0, s1, s2, op0, op1=None, reads=(), writes=(), accum=None):
        kw = {}
        if op1 is not None:
            kw["op1"] = op1
        if accum is not None:
            kw["accum_out"] = accum
        return self.op(eng, lambda e: e.tensor_scalar(out=out, in0=in0, scalar1=s1, scalar2=s2, op0=op0, **kw),
                       reads, writes)

    def stt(self, out, in0, scalar, in1, op0, op1, reads=(), writes=()):
        return self.op("vector", lambda e: e.scalar_tensor_tensor(out=out, in0=in0, scalar=scalar, in1=in1,
                                                                  op0=op0, op1=op1), reads, writes)

    def copy(self, eng, out, in_, reads=(), writes=()):
        if eng == "scalar":
            return self.op(eng, lambda e: e.copy(out=out, in_=in_), reads, writes)
        return self.op(eng, lambda e: e.tensor_copy(out=out, in_=in_), reads, writes)

    def memset(self, eng, ap, val, writes=()):
        return self.op(eng, lambda e: e.memset(ap, val), (), writes)

    def recip(self, out, in_, reads=(), writes=()):
        return self.op("vector", lambda e: e.reciprocal(out=out, in_=in_), reads, writes)

    def reduce(self, out, in_, op, reads=(), writes=()):
        return self.op("vector", lambda e: e.tensor_reduce(out=out, in_=in_, axis=AX.X, op=op), reads, writes)

    def emit(self, final_waits=()):
        nc = self.nc
        es = self.es
        esem = {(e, ph): es.enter_context(nc.semaphore("s_%s_%d" % (e, ph)))
                for e in self.ENGS for ph in range(self.phase + 1)}
        dsem = {k: es.enter_context(nc.semaphore("d_" + str(k))) for k in self.dma_counts}
        for e in self.ENGS:
            c = {}
            for o in self.ops[e]:
                if o.is_dma:
                    continue
                if o.needs_inc:
                    c[o.phase] = c.get(o.phase, 0) + 1
                    o.semval = c[o.phase]
        block = es.enter_context(nc.Block())

        def run(ename):
            def body(eng):
                known = {}
                for o in self.ops[ename]:
                    for d in o.deps:
                        if d.is_dma:
                            key, val, sem = ("d", d.dsem), d.dval, dsem[d.dsem]
                        else:
                            key, val, sem = ("e", d.eng, d.phase), d.semval, esem[(d.eng, d.phase)]
                        if known.get(key, 0) >= val:
                            continue
                        known[key] = val
                        eng.wait_ge(sem, val)
                    ins = o.fn(eng)
                    if o.is_dma:
                        ins.then_inc(dsem[o.dsem], 16)
                    elif o.needs_inc:
                        ins.then_inc(esem[(ename, o.phase)], 1)
                if ename == "sync":
                    for k in final_waits:
                        eng.wait_ge(dsem[k], 16 * self.dma_counts[k])
            return body

        block.tensor(run("tensor"))
        block.vector(run("vector"))
        block.scalar(run("scalar"))
        block.gpsimd(run("gpsimd"))
        block.sync(run("sync"))
        es.close()


class Arena:
    def __init__(self, P, nf32):
        self.t = P.sbuf("arena", [128, nf32], F32)
        self.n = nf32
        self.off = 0

    def reset(self, to=0):
        self.off = to

    def alloc(self, shape, dt, at=None):
        per = 1
        for s in shape[1:]:
            per *= s
        nf = (per + 1) // 2 if dt == BF16 else per
        off = self.off if at is None else at
        assert off + nf <= self.n, ("arena overflow", off, nf, self.n)
        v = self.t[0:shape[0], off:off + nf]
        if dt == BF16:
            v = v.bitcast(BF16)
            if per % 2:
                v = v[:, 0:per]
        if len(shape) == 3:
            v = v.rearrange("p (a b) -> p a b", b=shape[2])
        elif len(shape) == 4:
            v = v.rearrange("p (a b c) -> p a b c", b=shape[2], c=shape[3])
        if at is None:
            self.off = off + nf
        return v


def _t5_bucket(rel):
    half, max_exact = 16, 8
    ret = np.where(rel > 0, half, 0)
    n = np.abs(rel)
    nf = np.maximum(n, 1).astype(np.float32)
    large = max_exact + (np.log(nf / np.float32(max_exact)) / np.float32(math.log(128 / max_exact))
                         * np.float32(half - max_exact)).astype(np.int32)
    large = np.minimum(large, half - 1)
    return ret + np.where(n < max_exact, n, large)


def _host_layout(inp):
    w_in = np.asarray(inp["w_in"], np.float32)[0]
    cols = []
    for j in range(4):
        cols.append(w_in[:, O_QA + j * 128:O_QA + (j + 1) * 128])
    for j in range(4):
        cols.append(w_in[:, O_KA + j * 128:O_KA + (j + 1) * 128])
    qi = w_in[:, O_QI:O_QI + 256]
    cols.append(qi[:, 0:96])
    cols.append(qi[:, 96:192])
    cols.append(np.concatenate([qi[:, 192:256], np.zeros((D, 32), np.float32)], axis=1))
    ki = w_in[:, O_KI:O_KI + 32]
    cols.append(np.concatenate([ki, ki, ki], axis=1))
    for j in range(2):
        cols.append(w_in[:, O_QB + j * 128:O_QB + (j + 1) * 128])
    for j in range(2):
        cols.append(w_in[:, O_KB + j * 128:O_KB + (j + 1) * 128])
    cols.append(w_in[:, O_GD:O_GD + 16])
    w_fm = np.ascontiguousarray(np.concatenate(cols, axis=1))
    assert w_fm.shape[1] == NFM
    w_tm = np.ascontiguousarray(np.concatenate([
        w_in[:, O_VA:O_VA + 512], w_in[:, O_KB:O_KB + 256], w_in[:, O_WI:O_WI + 8],
        w_in[:, O_VB:O_VB + 512], w_in[:, O_OG:O_OG + 512]], axis=1))
    assert w_tm.shape[1] == NTM

    c = np.zeros((128, NCONST), np.float32)
    ii = np.arange(128)
    c[:, C_ID:C_ID + 128] = np.eye(128, dtype=np.float32)
    u01 = (ii[:, None] <= ii[None, :]).astype(np.float32)
    c[:, C_U01:C_U01 + 128] = u01
    c[:, C_UNEG:C_UNEG + 128] = -u01 / 16.0
    c[:, C_LNEG:C_LNEG + 128] = -(ii[:, None] > ii[None, :]).astype(np.float32) / 16.0
    dm = np.zeros((128, 128), np.float32)
    dm[:64, 64:] = NEG
    c[:, C_DMASK:C_DMASK + 128] = dm
    c[:, C_GMIX:C_GMIX + 8] = np.asarray(inp["norm_mix"], np.float32)[0].reshape(8, 128).T
    c[:, C_GFFN:C_GFFN + 8] = np.asarray(inp["norm_ffn"], np.float32)[0].reshape(8, 128).T
    rb = np.asarray(inp["rel_bias"], np.float32)
    c[:, C_CFAR:C_CFAR + 8] = rb[15][None, :]
    c[:, C_GLAN:C_GLAN + 512] = np.asarray(inp["gla_norm"], np.float32)[0][None, :]
    c[:, C_GFIN:C_GFIN + 1024] = np.asarray(inp["norm_final"], np.float32)[None, :]
    bt = np.zeros((128, 2, 8, 128), np.float32)
    for which in range(2):
        rel = (ii[:, None] - 128 * which) - ii[None, :]
        bk = _t5_bucket(rel)
        bt[:, which] = rb[bk].transpose(0, 2, 1)
    c[:, C_BIAS:C_BIAS + 2048] = bt.reshape(128, 2048)
    wgu = np.concatenate([np.asarray(inp["w_gate_up"], np.float32)[0],
                          np.asarray(inp["b_gate"], np.float32)[0][None, :]], axis=0)
    return {
        "w_fm": w_fm, "w_tm": w_tm,
        "w_out": np.ascontiguousarray(np.asarray(inp["w_out"], np.float32)[0]),
        "w_g": np.ascontiguousarray(np.asarray(inp["w_ffn_gate"], np.float32)[0]),
        "w_u": np.ascontiguousarray(np.asarray(inp["w_ffn_up"], np.float32)[0]),
        "w_d": np.ascontiguousarray(np.asarray(inp["w_ffn_down"], np.float32)[0]),
        "consts": c, "wgu": np.ascontiguousarray(wgu),
    }


def build_nc(debug=False, nseq=2):
    nc = bass.Bass("TRN2", target_bir_lowering=False)
    x_d = nc.dram_tensor("x", [2, S, D], F32, kind="ExternalInput").ap()
    wfm_d = nc.dram_tensor("w_fm", [D, NFM], F32, kind="ExternalInput").ap()
    wtm_d = nc.dram_tensor("w_tm", [D, NTM], F32, kind="ExternalInput").ap()
    wout_d = nc.dram_tensor("w_out", [D, D], F32, kind="ExternalInput").ap()
    wg_d = nc.dram_tensor("w_g", [D, DFF], F32, kind="ExternalInput").ap()
    wu_d = nc.dram_tensor("w_u", [D, DFF], F32, kind="ExternalInput").ap()
    wd_d = nc.dram_tensor("w_d", [DFF, D], F32, kind="ExternalInput").ap()
    c_d = nc.dram_tensor("consts", [128, NCONST], F32, kind="ExternalInput").ap()
    wgu_d = nc.dram_tensor("wgu", [17, 256], F32, kind="ExternalInput").ap()
    y_d = nc.dram_tensor("y", [2, S, D], F32, kind="ExternalOutput").ap()
    x1_d = nc.dram_tensor("x1_scratch", [S, D], F32, kind="Internal").ap()
    dbg = {}
    if debug:
        for nm, shp in (("d_qaT", [128, 4, S]), ("d_oa", [S, 512]), ("d_ob", [S, 512]), ("d_sc", [128, S]),
                        ("d_x1", [S, D])):
            dbg[nm] = nc.dram_tensor(nm, shp, F32 if nm in ("d_sc", "d_x1") else BF16, kind="ExternalOutput").ap()

    P = Prog(nc)
    cmin_t = P.sbuf("cmin", [128, C_MIN], F32)
    cmin = Buf(cmin_t[:], "cmin")
    identb_t = P.sbuf("identb", [128, 128], BF16)
    identb = Buf(identb_t[:], "identb")
    wgu_t = P.sbuf("wgu", [17, 256], F32)
    wgu = Buf(wgu_t[:], "wgu")
    obT_t = P.sbuf("obT", [128, 4, S], BF16)
    stat_t = P.sbuf("stat", [128, 64], F32)
    ARENA_F = 47800
    ar = Arena(P, ARENA_F)
    OA_OFF = ARENA_F - 4096

    pf_t = [P.psum("pf%d" % i, [128, 512], F32) for i in range(6)]
    pb_t = [P.psum("pb%d" % i, [128, 1024], BF16) for i in range(2)]
    pf = [Buf(t[:], "pf") for t in pf_t]
    pb = [Buf(t[:], "pb") for t in pb_t]

    ident_f = cmin_t[:, C_ID:C_ID + 128]
    u01 = cmin_t[:, C_U01:C_U01 + 128]
    uneg = cmin_t[:, C_UNEG:C_UNEG + 128]
    lneg = cmin_t[:, C_LNEG:C_LNEG + 128]
    dmask = cmin_t[:, C_DMASK:C_DMASK + 128]
    gmixT = cmin_t[:, C_GMIX:C_GMIX + 8]
    gffnT = cmin_t[:, C_GFFN:C_GFFN + 8]
    cfar = cmin_t[:, C_CFAR:C_CFAR + 8]

    P.dma("sync", cmin.ap, c_d[:, 0:C_MIN], "cmin", writes=[cmin])
    P.dma("sync", wgu.ap, wgu_d, "wgu", writes=[wgu])
    P.copy("vector", identb.ap, ident_f, reads=[cmin], writes=[identb])

    stat_bufs = [Buf(stat_t[:, 4 * i:4 * i + 4], "stat%d" % i) for i in range(8)]
    stat_ctr = [0]

    def next_stat():
        b = stat_bufs[stat_ctr[0] % 8]
        stat_ctr[0] += 1
        return b

    evac_ctr = [0]

    def evac(out, in_, reads, writes=()):
        evac_ctr[0] += 1
        eng = "scalar" if evac_ctr[0] % 2 else "vector"
        return P.copy(eng, out, in_, reads=reads, writes=writes)

    def rms_to_T(src_buf, src_ap, xs_buf, dstT_ap, gT, pbuf):
        st = next_stat()
        P.act(xs_buf.ap, src_ap, AF.Square, reads=[src_buf], writes=[xs_buf, st], accum=st.ap[:, 0:1])
        P.act(st.ap[:, 1:2], st.ap[:, 0:1], AF.Ln, reads=[st], writes=[st], scale=1.0 / D, bias=EPS)
        P.act(st.ap[:, 2:3], st.ap[:, 1:2], AF.Exp, reads=[st], writes=[st], scale=-0.5)
        P.ts("vector", xs_buf.ap, src_ap, st.ap[:, 2:3], None, ALU.mult, reads=[src_buf, st], writes=[xs_buf])
        pv = pbuf.ap.rearrange("p (a b) -> p a b", b=128)
        for kc in range(8):
            P.transpose(pv[:, kc, :], xs_buf.ap[:, kc * 128:(kc + 1) * 128], identb.ap,
                        reads=[xs_buf, identb], writes=[pbuf])
        P.tt("vector", dstT_ap, pv, gT.unsqueeze(2).to_broadcast([128, 8, 128]), ALU.mult,
             reads=[pbuf, cmin], writes=[])
        return st

    for seq in range(nseq):
        P.barrier()
        ar.reset(0)
        qaT = ar.alloc([128, 4, S], BF16)
        kaT = ar.alloc([128, 4, S], BF16)
        va = ar.alloc([128, NT, 8, 66], BF16)
        qiT = ar.alloc([96, 3, S], BF16)
        kiT = ar.alloc([96, S], BF16)
        wi = ar.alloc([128, NT, 8], F32)
        AB_END = ar.off
        wfm = ar.alloc([128, 8, NFM], BF16)
        wtm = ar.alloc([128, 8, NTM], BF16)
        wfm_b, wtm_b = Buf(wfm, "wfm"), Buf(wtm, "wtm")
        glan = ar.alloc([128, 512], F32)
        glan_b = Buf(glan, "glan")
        hTs = [ar.alloc([128, 8, 512], BF16) for _ in range(2)]
        hT_bs = [Buf(hTs[i], "hT%d" % i) for i in range(2)]
        xin = [Buf(ar.alloc([128, D], F32), "xin%d" % i) for i in range(2)]
        xs = [Buf(ar.alloc([128, D], BF16), "xs%d" % i) for i in range(2)]
        qbT = Buf(ar.alloc([128, 2, 512], F32), "qbT")
        kbT = Buf(ar.alloc([128, 2, 512], F32), "kbT")
        gdT = Buf(ar.alloc([17, 512], F32), "gdT")
        kbtm = [Buf(ar.alloc([128, 256], F32), "kbtm") for _ in range(2)]
        vbtm = [Buf(ar.alloc([128, 512], BF16), "vbtm") for _ in range(2)]
        sg = [Buf(ar.alloc([128, 512], F32), "sg") for _ in range(2)]
        etmp = Buf(ar.alloc([128, 256], F32), "etmp")
        lsb = Buf(ar.alloc([128, 256], F32), "lsb")
        e1Ts = [Buf(ar.alloc([128, 2, 128], F32), "e1T") for _ in range(2)]
        e2T = Buf(ar.alloc([128, 2, 128], F32), "e2T")
        e3 = Buf(ar.alloc([128, 256], F32), "e3")
        qinTs = [Buf(ar.alloc([128, 2, 128], BF16), "qinT") for _ in range(2)]
        kinTs = [Buf(ar.alloc([128, 2, 128], BF16), "kinT") for _ in range(2)]
        kdecs = [Buf(ar.alloc([128, 256], BF16), "kdec") for _ in range(2)]
        attS = [Buf(ar.alloc([128, 128], BF16), "attS") for _ in range(2)]
        on = Buf(ar.alloc([128, 512], F32), "on")
        obss = [Buf(ar.alloc([128, 512], BF16), "obs%d" % i) for i in range(2)]
        stf = Buf(ar.alloc([128, 2, 128], F32), "stf")
        stb = Buf(ar.alloc([128, 2, 128], BF16), "stb")

        P.dma("gpsimd", wfm, wfm_d.rearrange("(kc p) n -> p kc n", p=128), "wfm", writes=[wfm_b])
        P.dma("gpsimd", wtm, wtm_d.rearrange("(kc p) n -> p kc n", p=128), "wtm", writes=[wtm_b])
        P.dma("sync", glan, c_d[:, C_GLAN:C_GLAN + 512], "glan", writes=[glan_b])
        P.memset("gpsimd", va[:, :, :, 64:66], 1.0)
        P.memset("gpsimd", gdT.ap, 1.0, writes=[gdT])
        P.memset("vector", stf.ap, 0.0, writes=[stf])
        P.memset("vector", stb.ap, 0.0, writes=[stb])

        fm_off = {}
        o = 0
        for nm, m in FM_TILES:
            fm_off[nm] = (o, m)
            o += m
        mm_ctr = [0]
        zps = Buf(pf_t[2][:, 0:256], "zps")
        rps = Buf(pf_t[2][:, 256:512], "rps")
        cps = Buf(pf_t[3][:, 0:256], "cps")
        ops_ = pf[4]
        aps2 = [Buf(pf_t[5][:, 0:128], "aps0"), Buf(pf_t[5][:, 128:256], "aps1")]
        kvp = Buf(pf_t[5][:, 256:512], "kvp")

        def prep(tb):
            hT, hT_b = hTs[tb % 2], hT_bs[tb % 2]
            for i in range(4):
                t = tb * 4 + i
                xb = xin[t % 2]
                P.dma("sync", xb.ap, x_d[seq, t * 128:(t + 1) * 128, :], "xin%d" % (t % 2), writes=[xb])
                st = next_stat()
                xsb = xs[t % 2]
                P.act(xsb.ap, xb.ap, AF.Square, reads=[xb], writes=[xsb, st], accum=st.ap[:, 0:1])
                P.act(st.ap[:, 1:2], st.ap[:, 0:1], AF.Ln, reads=[st], writes=[st], scale=1.0 / D, bias=EPS)
                P.act(st.ap[:, 2:3], st.ap[:, 1:2], AF.Exp, reads=[st], writes=[st], scale=-0.5)
                P.ts("vector", xsb.ap, xb.ap, st.ap[:, 2:3], None, ALU.mult, reads=[xb, st], writes=[xsb])
                pbuf = pb[t % 2]
                pv = pbuf.ap.rearrange("p (a b) -> p a b", b=128)
                for kc in range(8):
                    P.transpose(pv[:, kc, :], xsb.ap[:, kc * 128:(kc + 1) * 128], identb.ap,
                                reads=[xsb, identb], writes=[pbuf])
                P.tt("vector", hT[:, :, i * 128:(i + 1) * 128], pv,
                     gmixT.unsqueeze(2).to_broadcast([128, 8, 128]), ALU.mult,
                     reads=[pbuf, cmin], writes=[hT_b])

        def fm_block(tb):
            hT, hT_b = hTs[tb % 2], hT_bs[tb % 2]
            cs = slice(tb * 512, (tb + 1) * 512)
            for nm, m in FM_TILES:
                off, _ = fm_off[nm]
                ps = pf[mm_ctr[0] % 2]
                mm_ctr[0] += 1
                for kc in range(8):
                    P.matmul(ps.ap[0:m, :], wfm[:, kc, off:off + m], hT[:, kc, :], kc == 0, kc == 7,
                             reads=[wfm_b, hT_b], writes=[ps])
                if nm.startswith("qa"):
                    evac(qaT[:, int(nm[2]), cs], ps.ap, [ps])
                elif nm.startswith("ka"):
                    evac(kaT[:, int(nm[2]), cs], ps.ap, [ps])
                elif nm.startswith("qi"):
                    evac(qiT[:, int(nm[2]), cs], ps.ap[0:96, :], [ps])
                elif nm == "ki":
                    evac(kiT[:, cs], ps.ap[0:96, :], [ps])
                elif nm.startswith("qb"):
                    evac(qbT.ap[:, int(nm[2]), :], ps.ap, [ps], [qbT])
                elif nm.startswith("kb"):
                    evac(kbT.ap[:, int(nm[2]), :], ps.ap, [ps], [kbT])
                else:
                    evac(gdT.ap[0:16, :], ps.ap[0:16, :], [ps], [gdT])

        def gla_s1(t):
            tb, i = t // 4, t % 4
            hT, hT_b = hTs[tb % 2], hT_bs[tb % 2]
            tcs = slice(i * 128, (i + 1) * 128)
            kb_s, vb_s, sg_s = kbtm[t % 2], vbtm[t % 2], sg[t % 2]
            e1T, qinT, kinT, kdec = e1Ts[t % 2], qinTs[t % 2], kinTs[t % 2], kdecs[t % 2]
            P.matmul(zps.ap, gdT.ap[0:17, tcs], wgu.ap, True, True, reads=[gdT, wgu], writes=[zps])
            P.act(etmp.ap, zps.ap, AF.Exp, reads=[zps], writes=[etmp], scale=-1.0)
            P.act(lsb.ap, etmp.ap, AF.Ln, reads=[etmp], writes=[lsb], bias=1.0)
            for g, (goff, gn) in enumerate(((0, 512), (512, 264), (776, 512), (1288, 512))):
                ps = pf[mm_ctr[0] % 2]
                mm_ctr[0] += 1
                for kc in range(8):
                    P.matmul(ps.ap[:, 0:gn], hT[:, kc, tcs], wtm[:, kc, goff:goff + gn], kc == 0, kc == 7,
                             reads=[wtm_b, hT_b], writes=[ps])
                if g == 0:
                    evac(va[:, t, :, 0:64], ps.ap.rearrange("p (h d) -> p h d", d=64), [ps])
                elif g == 1:
                    P.copy("vector", kb_s.ap, ps.ap[:, 0:256], reads=[ps], writes=[kb_s])
                    P.copy("vector", wi[:, t, :], ps.ap[:, 256:264], reads=[ps])
                elif g == 2:
                    P.copy("scalar", vb_s.ap, ps.ap, reads=[ps], writes=[vb_s])
                else:
                    P.act(sg_s.ap, ps.ap, AF.Silu, reads=[ps], writes=[sg_s])
            cv = cps.ap.rearrange("p (a b) -> p a b", b=128)
            for pr in range(2):
                P.matmul(cv[:, pr, :], lsb.ap[:, pr * 128:(pr + 1) * 128], uneg, True, True,
                         reads=[lsb, cmin], writes=[cps])
            P.matmul(rps.ap, lneg, lsb.ap, True, True, reads=[lsb, cmin], writes=[rps])
            P.act(e1T.ap, cv, AF.Exp, reads=[cps], writes=[e1T])
            P.act(e2T.ap, cv, AF.Exp, reads=[cps], writes=[e2T], scale=-1.0)
            P.act(e3.ap, rps.ap, AF.Exp, reads=[rps], writes=[e3])
            P.stt(qinT.ap, qbT.ap[:, :, tcs], 0.125, e1T.ap, ALU.mult, ALU.mult,
                  reads=[qbT, e1T], writes=[qinT])
            P.tt("vector", kinT.ap, kbT.ap[:, :, tcs], e2T.ap, ALU.mult, reads=[kbT, e2T], writes=[kinT])
            P.tt("gpsimd", kdec.ap, kb_s.ap, e3.ap, ALU.mult, reads=[kb_s, e3], writes=[kdec])

        def gla_s2(t):
            vb_s, sg_s = vbtm[t % 2], sg[t % 2]
            e1T, qinT, kinT, kdec = e1Ts[t % 2], qinTs[t % 2], kinTs[t % 2], kdecs[t % 2]
            for pr in range(2):
                for hh in range(2):
                    h = 2 * pr + hh
                    hp = slice(hh * 64, hh * 64 + 64)
                    aps = aps2[hh]
                    P.matmul(aps.ap, kinT.ap[hp, pr, :], qinT.ap[hp, pr, :], True, True,
                             reads=[kinT, qinT], writes=[aps])
                    asb = attS[hh]
                    P.tt("vector", asb.ap, aps.ap, u01, ALU.mult, reads=[aps, cmin], writes=[asb])
                    P.matmul(ops_.ap[:, h * 128:(h + 1) * 128], asb.ap, vb_s.ap[:, h * 128:(h + 1) * 128],
                             True, False, reads=[asb, vb_s], writes=[ops_])
                    P.matmul(ops_.ap[:, h * 128:(h + 1) * 128], qinT.ap[hp, pr, :], stb.ap[hp, pr, :],
                             False, True, reads=[qinT, stb], writes=[ops_])
                P.matmul(kvp.ap, kdec.ap[:, pr * 128:(pr + 1) * 128],
                         vb_s.ap[:, pr * 256:(pr + 1) * 256], True, True, reads=[kdec, vb_s], writes=[kvp])
                for hh in range(2):
                    hp = slice(hh * 64, hh * 64 + 64)
                    P.stt(stf.ap[hp, pr, :], stf.ap[hp, pr, :], e1T.ap[hp, pr, 127:128],
                          kvp.ap[hp, hh * 128:(hh + 1) * 128], ALU.mult, ALU.add,
                          reads=[stf, e1T, kvp], writes=[stf])
            P.copy("gpsimd", stb.ap, stf.ap, reads=[stf], writes=[stb])
            st = next_stat()
            P.act(on.ap, ops_.ap, AF.Square, reads=[ops_], writes=[on])
            P.reduce(st.ap[:, 0:4], on.ap.rearrange("p (h v) -> p h v", v=128), ALU.add, reads=[on], writes=[st])
            st2 = next_stat()
            P.act(st2.ap[:, 0:4], st.ap[:, 0:4], AF.Ln, reads=[st], writes=[st2], scale=1.0 / 128, bias=EPS)
            st3 = next_stat()
            P.act(st3.ap[:, 0:4], st2.ap[:, 0:4], AF.Exp, reads=[st2], writes=[st3], scale=-0.5)
            P.tt("vector", on.ap.rearrange("p (h v) -> p h v", v=128),
                 ops_.ap.rearrange("p (h v) -> p h v", v=128),
                 st3.ap[:, 0:4].unsqueeze(2).to_broadcast([128, 4, 128]), ALU.mult,
                 reads=[ops_, st3], writes=[on])
            P.tt("gpsimd", on.ap, on.ap, glan, ALU.mult, reads=[on, glan_b], writes=[on])
            obs = obss[t % 2]
            P.tt("vector", obs.ap, on.ap, sg_s.ap, ALU.mult, reads=[on, sg_s], writes=[obs])
            if debug and seq == 0:
                P.dma("sync", dbg["d_ob"][t * 128:(t + 1) * 128, :], obs.ap, "dbg_ob", reads=[obs])

        def gla_s3(t):
            obs = obss[t % 2]
            pbuf = pb[t % 2]
            pv = pbuf.ap.rearrange("p (a b) -> p a b", b=128)
            for j in range(4):
                P.transpose(pv[:, j, :], obs.ap[:, j * 128:(j + 1) * 128], identb.ap,
                            reads=[obs, identb], writes=[pbuf])
            P.copy("scalar", obT_t[:, :, t * 128:(t + 1) * 128], pv[:, 0:4, :], reads=[pbuf])

        prep(0)
        for t in range(NT):
            if t % 4 == 0:
                fm_block(t // 4)
                if t // 4 + 1 < 4:
                    prep(t // 4 + 1)
            gla_s1(t)
            if t >= 1:
                gla_s2(t - 1)
            if t >= 2:
                gla_s3(t - 2)
        gla_s2(NT - 1)
        gla_s3(NT - 2)
        gla_s3(NT - 1)
        if debug and seq == 0:
            P.barrier()
            P.dma("sync", dbg["d_qaT"], qaT, "dbg_qaT")

        P.barrier()
        ar.reset(AB_END)
        bias8 = ar.alloc([128, 2, 8, 128], BF16)
        bias8_b = Buf(bias8, "bias8")
        biasT = ar.alloc([128, 2, 8, 128], F32)
        biasT_b = Buf(biasT, "biasT")
        P.dma("sync", biasT.rearrange("p a b c -> p (a b c)"), c_d[:, C_BIAS:C_BIAS + 2048], "biasT", writes=[biasT_b])
        P.ts("gpsimd", bias8, biasT, 8.0, None, ALU.mult, reads=[biasT_b], writes=[bias8_b])
        sc = [Buf(ar.alloc([128, S], F32), "sc%d" % i) for i in range(4)]
        junkB = ar.alloc([128, S], BF16)
        msk = [Buf(ar.alloc([128, S], BF16), "msk%d" % i) for i in range(4)]
        mskT = [Buf(ar.alloc([128, NT, 128], BF16), "mskT%d" % i) for i in range(4)]
        rbf = [Buf(ar.alloc([128, 256], BF16), "rbf%d" % i) for i in range(3)]
        diagw = [Buf(ar.alloc([128, 8, 128], BF16), "diagw%d" % i) for i in range(2)]
        dmask_bf = Buf(ar.alloc([128, 128], BF16), "dmaskbf")
        P.copy("vector", dmask_bf.ap, dmask, reads=[cmin], writes=[dmask_bf])
        NPMAX = 5
        ebuf = [Buf(ar.alloc([128, 4, 128], BF16), "e%d" % i) for i in range(NPMAX)]
        pbuf_ = [Buf(ar.alloc([128, 4, 128], BF16), "p%d" % i) for i in range(NPMAX)]
        oasb = Buf(ar.alloc([128, 512], BF16), "oasb")
        bis_t = [ar.alloc([128, 8], F32) for i in range(2)]
        midb = [Buf(b_[:, 0:2], "mid") for b_ in bis_t]
        cntb = [[Buf(b_[:, 2:3], "cnt0"), Buf(b_[:, 3:4], "cnt1")] for b_ in bis_t]
        tbb = [Buf(b_[:, 4:6], "tb") for b_ in bis_t]
        thrb = [Buf(b_[:, 6:8], "thr") for b_ in bis_t]
        nrm = Buf(ar.alloc([128, 16], F32), "nrm")
        actb_t = ar.alloc([128, 8], F32)
        nmid = Buf(actb_t[:, 0:1], "nmid")
        ssum = Buf(actb_t[:, 1:2], "ssum")
        sgnb = Buf(actb_t[:, 2:3], "sgnb")
        thrA = Buf(actb_t[:, 3:4], "thrA")
        junkB2 = ar.alloc([128, S], BF16)
        assert ar.off <= OA_OFF, ("phase B arena", ar.off, OA_OFF)
        oaT = ar.alloc([128, 4, S], BF16, at=OA_OFF)
        r_ctr = [0]
        lgb = [pf[2], pf[3], pb[1], pf[0], pf[1]]
        lgv = [pf_t[2][:], pf_t[3][:], pb_t[1][:].bitcast(F32), pf_t[0][:], pf_t[1][:]]
        pbm = Buf(pb_t[0][:, 0:512], "pbm")
        pbo = pbm
        sps = Buf(pb_t[0][:, 512:1024].bitcast(F32), "sps")
        ops2 = [pf[4], pf[5]]
        ov = [b_.ap[:, 0:264].rearrange("p (h d) -> p h d", d=66) for b_ in ops2]

        def idx_group(g4):
            qts = [4 * g4 + k for k in range(4)]
            for k, qt in enumerate(qts):
                sk = (qt + 1) * 128
                scb = sc[k]
                qcs = slice(qt * 128, (qt + 1) * 128)
                dg = diagw[qt % 2]
                for h in range(8):
                    P.ts("vector", dg.ap[:, h, :], ident_f, wi[:, qt, h:h + 1], None, ALU.mult,
                         reads=[cmin], writes=[dg])
                for c0 in range(0, sk, 256):
                    w = min(256, sk - c0)
                    last_chunk = (c0 + w == sk)

                    def emit_d(h, c0=c0, w=w):
                        hp = slice((h % 3) * 32, (h % 3) * 32 + 32)
                        ps = pf[h % 2]
                        P.matmul(ps.ap[:, 0:w], qiT[hp, h // 3, qcs], kiT[hp, c0:c0 + w], True, True, writes=[ps])
                        rb_ = rbf[r_ctr[0] % 3]
                        r_ctr[0] += 1
                        P.act(rb_.ap[:, 0:w], ps.ap[:, 0:w], AF.Relu, reads=[ps], writes=[rb_])
                        return rb_

                    rbs = {0: emit_d(0)}
                    for h in range(8):
                        if h + 1 < 8:
                            rbs[h + 1] = emit_d(h + 1)
                        P.matmul(sps.ap[:, 0:w], dg.ap[:, h, :], rbs[h].ap[:, 0:w], h == 0,
                                 h == 7 and not last_chunk, reads=[dg, rbs[h]], writes=[sps])
                    if last_chunk:
                        P.matmul(sps.ap[:, w - 128:w], identb.ap, dmask_bf.ap, False, True,
                                 reads=[identb, dmask_bf], writes=[sps])
                    P.copy("scalar", scb.ap[:, c0:c0 + w], sps.ap[:, 0:w], reads=[sps], writes=[scb])
                if debug and seq == 0 and qt == 5:
                    P.dma("sync", dbg["d_sc"], scb.ap, "dbg_sc", reads=[scb])

        def bis_group(g4):
            qts = [4 * g4 + k for k in range(4)]
            if g4 == 0:
                pairs, act_k = [(), (2, 3)], None
            else:
                pairs, act_k = [(0, 1), (2, 3)], None
            act_pairs = [pi for pi in range(2) if len(pairs[pi])]
            last_step = R_BIS / (2.0 ** N_BIS)
            for pi in act_pairs:
                n_ = len(pairs[pi])
                P.memset("vector", midb[pi].ap, 0.0, writes=[midb[pi]])
                for it in range(1, N_BIS + 1):
                    step = R_BIS / (2.0 ** it)
                    for a, k in enumerate(pairs[pi]):
                        sk = (qts[k] + 1) * 128
                        P.ts("vector", junkB[:, 0:sk], sc[k].ap[:, 0:sk], midb[pi].ap[:, a:a + 1], None,
                             ALU.is_ge, ALU.add, reads=[sc[k], midb[pi]], writes=[cntb[pi][a]],
                             accum=cntb[pi][a].ap)
                    P.ts("vector", tbb[pi].ap[:, 0:n_], bis_t[pi][:, 2:2 + n_], 255.5, 2.0 * step, ALU.is_ge, ALU.mult,
                         reads=[cntb[pi][a] for a in range(n_)], writes=[tbb[pi]])
                    P.stt(midb[pi].ap[:, 0:n_], tbb[pi].ap[:, 0:n_], -step, midb[pi].ap[:, 0:n_], ALU.add, ALU.add,
                          reads=[tbb[pi], midb[pi]], writes=[midb[pi]])
                    yield
                P.ts("vector", thrb[pi].ap[:, 0:n_], midb[pi].ap[:, 0:n_], -last_step, None, ALU.add,
                     reads=[midb[pi]], writes=[thrb[pi]])
            thr_of = {}
            for pi in act_pairs:
                for a, k in enumerate(pairs[pi]):
                    thr_of[k] = (thrb[pi], thrb[pi].ap[:, a:a + 1])
            if act_k is not None:
                thr_of[act_k] = (thrA, thrA.ap)
            for k, qt in enumerate(qts):
                sk = (qt + 1) * 128
                if qt >= 2:
                    P.ts("vector", msk[k].ap[:, 0:sk], sc[k].ap[:, 0:sk], thr_of[k][1], None, ALU.is_ge,
                         reads=[sc[k], thr_of[k][0]], writes=[msk[k]])
                else:
                    P.ts("vector", msk[k].ap[:, 0:sk], sc[k].ap[:, 0:sk], -1.0e29, None, ALU.is_ge,
                         reads=[sc[k]], writes=[msk[k]])
            for k, qt in enumerate(qts):
                nkb = qt + 1
                mT = mskT[k]
                pv = pbm.ap.rearrange("p (a b) -> p a b", b=128)
                for c0 in range(0, nkb, 4):
                    n = min(4, nkb - c0)
                    for kk in range(n):
                        P.transpose(pv[:, kk, :], msk[k].ap[:, (c0 + kk) * 128:(c0 + kk + 1) * 128], identb.ap,
                                    reads=[msk[k], identb], writes=[pbm])
                    P.copy("scalar", mT.ap[:, c0:c0 + n, :], pv[:, 0:n, :], reads=[pbm], writes=[mT])

        def attn_group(g4):
            NPIPE = 5 if g4 == 3 else 3
            qts = [4 * g4 + k for k in range(4)]
            items = []
            for k, qt in enumerate(qts):
                near = [kb for kb in (qt - 1, qt) if kb >= 0]
                far = list(range(0, max(qt - 1, 0)))
                groups = [("far", far[c0:c0 + 4]) for c0 in range(0, len(far), 4)] + [("near", near)]
                for h in range(8):
                    for gi, (kind, kbs) in enumerate(groups):
                        items.append((k, qt, h, kind, kbs, gi == 0, gi == len(groups) - 1))

            def stage1(item, i):
                k, qt, h, kind, kbs, _, _ = item
                qcs = slice(qt * 128, (qt + 1) * 128)
                j, hp = h // 2, slice((h % 2) * 64, (h % 2) * 64 + 64)
                n = len(kbs)
                lps = lgb[i % NPIPE]
                lv = lgv[i % NPIPE].rearrange("p (a b) -> p a b", b=128)
                for kk, kb in enumerate(kbs):
                    if kind == "far":
                        P.matmul(lv[:, kk, :], kaT[hp, j, kb * 128:(kb + 1) * 128], qaT[hp, j, qcs], True, True,
                                 writes=[lps])
                    else:
                        which = 0 if kb == qt else 1
                        P.matmul(lv[:, kk, :], kaT[hp, j, kb * 128:(kb + 1) * 128], qaT[hp, j, qcs], True, False,
                                 writes=[lps])
                        P.matmul(lv[:, kk, :], identb.ap, bias8[:, which, h, :], False, True,
                                 reads=[identb, bias8_b], writes=[lps])
                eb = ebuf[i % NPIPE]
                pbf = pbuf_[i % NPIPE]
                if kind == "far":
                    P.act(eb.ap[:, 0:n, :], lv[:, 0:n, :], AF.Exp, reads=[lps, cmin], writes=[eb],
                          scale=0.125, bias=cfar[:, h:h + 1])
                else:
                    P.act(eb.ap[:, 0:n, :], lv[:, 0:n, :], AF.Exp, reads=[lps], writes=[eb], scale=0.125)
                meng = "vector" if (g4 == 3 and i % 2 == 1) else "gpsimd"
                P.tt(meng, pbf.ap[:, 0:n, :], eb.ap[:, 0:n, :], mskT[k].ap[:, kbs[0]:kbs[0] + n, :], ALU.mult,
                     reads=[eb, mskT[k]], writes=[pbf])

            def stage2(item, i):
                k, qt, h, kind, kbs, first_g, last_g = item
                qcs = slice(qt * 128, (qt + 1) * 128)
                n = len(kbs)
                pbf = pbuf_[i % NPIPE]
                for kk, kb in enumerate(kbs):
                    P.matmul(ov[h // 4][:, h % 4, 0:65], pbf.ap[:, kk, :], va[:, kb, h, 0:65],
                             first_g and kk == 0, last_g and kk == n - 1, reads=[pbf], writes=[ops2[h // 4]])
                if h == 7 and last_g:
                    for hh in range(2):
                        P.act(nrm.ap[:, hh * 4:(hh + 1) * 4].unsqueeze(2), ov[hh][:, :, 64:65], AF.Ln,
                              reads=[ops2[hh]], writes=[nrm])
                    P.act(nrm.ap[:, 8:16], nrm.ap[:, 0:8], AF.Exp, reads=[nrm], writes=[nrm], scale=-1.0)
                    for h2 in range(8):
                        P.act(oasb.ap[:, h2 * 64:(h2 + 1) * 64], ov[h2 // 4][:, h2 % 4, 0:64], AF.Copy,
                              reads=[ops2[h2 // 4], nrm], writes=[oasb], scale=nrm.ap[:, 8 + h2:9 + h2])
                    if debug and seq == 0:
                        P.dma("sync", dbg["d_oa"][qt * 128:(qt + 1) * 128, :], oasb.ap, "dbg_oa", reads=[oasb])
                    pv = pbo.ap.rearrange("p (a b) -> p a b", b=128)
                    for j2 in range(4):
                        P.transpose(pv[:, j2, :], oasb.ap[:, j2 * 128:(j2 + 1) * 128], identb.ap,
                                    reads=[oasb, identb], writes=[pbo])
                    P.copy("scalar", oaT[:, :, qcs], pv[:, 0:4, :], reads=[pbo])

            LA = NPIPE - 1
            for i in range(len(items) + LA):
                if i < len(items):
                    stage1(items[i], i)
                if i >= LA:
                    stage2(items[i - LA], i - LA)
                yield

        N_ITEMS = [48, 80, 112, 144]
        idx_group(0)
        for _ in bis_group(0):
            pass
        for g4 in range(4):
            if g4 + 1 < 4:
                idx_group(g4 + 1)
                ga = attn_group(g4)
                per = (N_ITEMS[g4] + 2 + 2 * N_BIS - 1) // (2 * N_BIS)
                for _ in bis_group(g4 + 1):
                    for _i in range(per):
                        next(ga, None)
                assert next(ga, "done") == "done", "attention items not fully emitted before masks"
            else:
                for _ in attn_group(g4):
                    pass

        P.barrier()
        ar.reset(0)
        h2T = ar.alloc([128, 8, S], BF16)
        wd = ar.alloc([128, NJ, D], BF16)
        wd_b = Buf(wd, "wd")
        NR = 3
        wgr = [Buf(ar.alloc([128, 8, 256], BF16), "wg%d" % i) for i in range(NR)]
        wur = [Buf(ar.alloc([128, 8, 256], BF16), "wu%d" % i) for i in range(NR)]
        gfin = ar.alloc([128, D], F32)
        gfin_b = Buf(gfin, "gfin")
        C2_START = ar.off
        wout = ar.alloc([128, 8, D], BF16)
        wout_b = Buf(wout, "wout")
        wgv = wg_d.rearrange("(kc p) n -> p kc n", p=128)
        wuv = wu_d.rearrange("(kc p) n -> p kc n", p=128)
        P.dma("gpsimd", wout, wout_d.rearrange("(kc p) n -> p kc n", p=128), "wout", writes=[wout_b])
        for q4 in range(2):
            P.dma("gpsimd", wd[:, q4 * 11:(q4 + 1) * 11, :],
                  wd_d[q4 * 1408:(q4 + 1) * 1408, :].rearrange("(j p) n -> p j n", p=128), "wd", writes=[wd_b])
        for r in range(NR):
            P.dma("gpsimd", wgr[r].ap, wgv[:, :, r * 256:(r + 1) * 256], "wg%d" % r, writes=[wgr[r]])
            P.dma("gpsimd", wur[r].ap, wuv[:, :, r * 256:(r + 1) * 256], "wu%d" % r, writes=[wur[r]])
        P.dma("sync", gfin, c_d[:, C_GFIN:C_GFIN + 1024], "gfin", writes=[gfin_b])
        xin = [Buf(ar.alloc([128, D], F32), "xinC%d" % i) for i in range(2)]
        x1s = [Buf(ar.alloc([128, D], F32), "x1s%d" % i) for i in range(2)]
        xs = [Buf(ar.alloc([128, D], BF16), "xsC%d" % i) for i in range(2)]
        assert ar.off <= OA_OFF
        x1d_b = Buf(x1_d, "x1d")
        for t in range(NT):
            tcs = slice(t * 128, (t + 1) * 128)
            xb, x1b, xsb = xin[t % 2], x1s[t % 2], xs[t % 2]
            P.dma("sync", xb.ap, x_d[seq, tcs, :], "xinC%d" % (t % 2), writes=[xb])
            for half in range(2):
                ps = pf[(2 * t + half) % 4]
                for kc in range(8):
                    lhs = oaT[:, kc, tcs] if kc < 4 else obT_t[:, kc - 4, tcs]
                    P.matmul(ps.ap, lhs, wout[:, kc, half * 512:(half + 1) * 512], kc == 0, kc == 7,
                             reads=[wout_b], writes=[ps])
                P.tt("vector", x1b.ap[:, half * 512:(half + 1) * 512], xb.ap[:, half * 512:(half + 1) * 512], ps.ap,
                     ALU.add, reads=[xb, ps], writes=[x1b])
            P.dma("sync", x1_d[tcs, :], x1b.ap, "x1st", reads=[x1b], writes=[x1d_b])
            if debug and seq == 0:
                P.dma("sync", dbg["d_x1"][tcs, :], x1b.ap, "dbg_x1", reads=[x1b])
            rms_to_T(x1b, x1b.ap, xsb, h2T[:, :, tcs], gffnT, pb[t % 2])

        P.barrier()
        ar.reset(C2_START)
        actT = ar.alloc([128, NJ, 1024], BF16)
        actT_b = Buf(actT, "actT")
        x1r = [Buf(ar.alloc([128, D], F32), "x1r%d" % i) for i in range(2)]
        ysb = [Buf(ar.alloc([128, D], F32), "ysb%d" % i) for i in range(2)]
        osb = [Buf(ar.alloc([128, D], F32), "osb%d" % i) for i in range(2)]
        sgc = [Buf(ar.alloc([128, 512], F32), "sgc%d" % i) for i in range(2)]
        junkC = ar.alloc([128, D], BF16)
        ring = [0]
        cctr = [0]
        for cb in range(2):
            for gj in range(NJ // 2):
                r = ring[0] % NR
                ring[0] += 1
                if ring[0] > NR:
                    P.dma("gpsimd", wgr[r].ap, wgv[:, :, gj * 256:(gj + 1) * 256], "wg%d" % r, writes=[wgr[r]])
                    P.dma("gpsimd", wur[r].ap, wuv[:, :, gj * 256:(gj + 1) * 256], "wu%d" % r, writes=[wur[r]])
                for jj in range(2):
                    j = 2 * gj + jj
                    for hb in range(2):
                        tok = slice(cb * 1024 + hb * 512, cb * 1024 + (hb + 1) * 512)
                        gps = pf[(cctr[0] % 2) * 2]
                        ups = pf[(cctr[0] % 2) * 2 + 1]
                        sgb = sgc[cctr[0] % 2]
                        cctr[0] += 1
                        for kc in range(8):
                            P.matmul(gps.ap, wgr[r].ap[:, kc, jj * 128:(jj + 1) * 128], h2T[:, kc, tok], kc == 0, kc == 7,
                                     reads=[wgr[r]], writes=[gps])
                        for kc in range(8):
                            P.matmul(ups.ap, wur[r].ap[:, kc, jj * 128:(jj + 1) * 128], h2T[:, kc, tok], kc == 0, kc == 7,
                                     reads=[wur[r]], writes=[ups])
                        P.act(sgb.ap, gps.ap, AF.Silu, reads=[gps], writes=[sgb])
                        P.tt("vector", actT[:, j, hb * 512:(hb + 1) * 512], sgb.ap, ups.ap, ALU.mult,
                             reads=[sgb, ups], writes=[actT_b])
            for ti in range(8):
                t = cb * 8 + ti
                tcs = slice(t * 128, (t + 1) * 128)
                xr, yb, ob_ = x1r[t % 2], ysb[t % 2], osb[t % 2]
                P.dma("sync", xr.ap, x1_d[tcs, :], "x1r%d" % (t % 2), reads=[x1d_b], writes=[xr])
                for half in range(2):
                    ps = pf[4 + half]
                    for j in range(NJ):
                        P.matmul(ps.ap, actT[:, j, ti * 128:(ti + 1) * 128], wd[:, j, half * 512:(half + 1) * 512],
                                 j == 0, j == NJ - 1, reads=[actT_b, wd_b], writes=[ps])
                    P.tt("vector", yb.ap[:, half * 512:(half + 1) * 512], xr.ap[:, half * 512:(half + 1) * 512], ps.ap,
                         ALU.add, reads=[xr, ps], writes=[yb])
                st = next_stat()
                P.act(junkC, yb.ap, AF.Square, reads=[yb], writes=[st], accum=st.ap[:, 0:1])
                P.act(st.ap[:, 1:2], st.ap[:, 0:1], AF.Ln, reads=[st], writes=[st], scale=1.0 / D, bias=EPS)
                P.act(st.ap[:, 2:3], st.ap[:, 1:2], AF.Exp, reads=[st], writes=[st], scale=-0.5)
                P.stt(ob_.ap, yb.ap, st.ap[:, 2:3], gfin, ALU.mult, ALU.mult, reads=[yb, st, gfin_b], writes=[ob_])
                P.dma("sync", y_d[seq, tcs, :], ob_.ap, "yout%d" % (t % 2), reads=[ob_])

    P.emit(final_waits=["yout0", "yout1"])
    return nc


_NC_CACHE = {}


def kernel(**inputs):
    x = np.ascontiguousarray(np.asarray(inputs["x"], np.float32))
    lay = _host_layout(inputs)
    if "nc" not in _NC_CACHE:
        _NC_CACHE["nc"] = build_nc()
    nc = _NC_CACHE["nc"]
    in_maps = []
    for c in range(8):
        m = {"x": np.ascontiguousarray(x[2 * c:2 * c + 2])}
        m.update(lay)
        in_maps.append(m)
    res = run_bass_kernel_spmd(nc, in_maps, core_ids=list(range(8)))
    out = np.concatenate([np.asarray(r["y"], np.float32).reshape(2, S, D) for r in res.results], axis=0)
    return out
```

```python
import math
from contextlib import ExitStack

import numpy as np
import concourse.bass as bass
import concourse.mybir as mybir
from concourse.bass_utils import run_bass_kernel_spmd

F32 = mybir.dt.float32
BF16 = mybir.dt.bfloat16
ALU = mybir.AluOpType
AF = mybir.ActivationFunctionType
AX = mybir.AxisListType

S = 2048
D = 1024
NT = S // 128
DFF = 2816
NJ = DFF // 128
NEG = -1.0e30
EPS = 1e-6

O_QA, O_KA, O_VA, O_QI, O_KI, O_WI, O_QB, O_KB, O_VB, O_GD, O_OG = (
    0, 512, 1024, 1536, 1792, 1824, 1832, 2088, 2344, 2856, 2872)

FM_TILES = ([("qa%d" % j, 128) for j in range(4)] + [("ka%d" % j, 128) for j in range(4)]
            + [("qi%d" % j, 96) for j in range(3)] + [("ki", 96)]
            + [("qb%d" % j, 128) for j in range(2)] + [("kb%d" % j, 128) for j in range(2)]
            + [("gd", 16)])
NFM = sum(m for _, m in FM_TILES)
NTM = 512 + 264 + 512 + 512

C_ID, C_U01, C_UNEG, C_LNEG, C_DMASK, C_GMIX, C_GFFN, C_CFAR = 0, 128, 256, 384, 512, 640, 648, 656
C_MIN = 672
C_GLAN = 672
C_GFIN = C_GLAN + 512
C_BIAS = C_GFIN + 1024
NCONST = C_BIAS + 2048

R_BIS = 128.0
N_BIS = 16


class Buf:
    __slots__ = ("ap", "name", "last_w", "readers")

    def __init__(self, ap, name=""):
        self.ap = ap
        self.name = name
        self.last_w = None
        self.readers = []


class Op:
    __slots__ = ("eng", "fn", "deps", "needs_inc", "semval", "is_dma", "dsem", "dval", "phase")

    def __init__(self, eng, fn):
        self.eng = eng
        self.fn = fn
        self.deps = []
        self.needs_inc = False
        self.semval = None
        self.is_dma = False
        self.dsem = None
        self.dval = None


class Prog:
    ENGS = ("tensor", "vector", "scalar", "gpsimd", "sync")

    def __init__(self, nc):
        self.nc = nc
        self.ops = {e: [] for e in self.ENGS}
        self.es = ExitStack()
        self.dma_counts = {}
        self.dma_last = {}
        self.phase = 0

    def sbuf(self, name, shape, dt):
        return self.es.enter_context(self.nc.sbuf_tensor("sb_" + name, list(shape), dt))

    def psum(self, name, shape, dt):
        return self.es.enter_context(self.nc.psum_tensor("ps_" + name, list(shape), dt))

    def _dep(self, op, src):
        if src is None or src is op:
            return
        if src.eng == "tensor" and op.eng == "tensor" and not src.is_dma and not op.is_dma:
            return
        op.deps.append(src)
        if not src.is_dma:
            src.needs_inc = True

    def op(self, eng, fn, reads=(), writes=(), dma_key=None, extra=()):
        o = Op(eng, fn)
        o.phase = self.phase
        if dma_key is not None:
            o.is_dma = True
            o.dsem = dma_key
            self.dma_counts[dma_key] = self.dma_counts.get(dma_key, 0) + 1
            o.dval = 16 * self.dma_counts[dma_key]
            self.dma_last[dma_key] = o
        for s in extra:
            self._dep(o, s)
        for b in reads:
            self._dep(o, b.last_w)
        for b in writes:
            lw = b.last_w
            if lw is not None and not (lw.eng == eng and not lw.is_dma and not o.is_dma):
                self._dep(o, lw)
            for r in b.readers:
                if r.eng == eng and not r.is_dma and not o.is_dma:
                    continue
                self._dep(o, r)
        for b in reads:
            b.readers.append(o)
        for b in writes:
            b.last_w = o
            b.readers = []
        self.ops[eng].append(o)
        return o

    def barrier(self):
        lasts = []
        for e in self.ENGS:
            for o in reversed(self.ops[e]):
                if not o.is_dma:
                    lasts.append(o)
                    break
        lasts += list(self.dma_last.values())
        for e in self.ENGS:
            self.op(e, lambda eng: eng.nop(), extra=[l for l in lasts])
        self.phase += 1

    def dma(self, eng, out_ap, in_ap, key, reads=(), writes=()):
        return self.op(eng, lambda e: e.dma_start(out=out_ap, in_=in_ap), reads, writes, dma_key=key)

    def matmul(self, out, lhsT, rhs, start, stop, reads=(), writes=()):
        return self.op("tensor", lambda e: e.matmul(out, lhsT=lhsT, rhs=rhs, start=start, stop=stop), reads, writes)

    def transpose(self, out, in_, ident, reads=(), writes=()):
        return self.op("tensor", lambda e: e.transpose(out, in_, ident), reads, writes)

    def act(self, out, in_, func, reads=(), writes=(), bias=None, scale=None, accum=None):
        kw = {}
        if bias is not None:
            kw["bias"] = bias
        if scale is not None:
            kw["scale"] = scale
        if accum is not None:
            kw["accum_out"] = accum
        return self.op("scalar", lambda e: e.activation(out=out, in_=in_, func=func, **kw), reads, writes)

    def tt(self, eng, out, in0, in1, op, reads=(), writes=()):
        return self.op(eng, lambda e: e.tensor_tensor(out=out, in0=in0, in1=in1, op=op), reads, writes)

    def ts(self, eng, out, in0, s1, s2, op0, op1=None, reads=(), writes=(), accum=None):
        kw = {}
        if op1 is not None:
            kw["op1"] = op1
        if accum is not None:
            kw["accum_out"] = accum
        return self.op(eng, lambda e: e.tensor_scalar(out=out, in0=in0, scalar1=s1, scalar2=s2, op0=op0, **kw),
                       reads, writes)

    def stt(self, out, in0, scalar, in1, op0, op1, reads=(), writes=()):
        return self.op("vector", lambda e: e.scalar_tensor_tensor(out=out, in0=in0, scalar=scalar, in1=in1,
                                                                  op0=op0, op1=op1), reads, writes)

    def copy(self, eng, out, in_, reads=(), writes=()):
        if eng == "scalar":
            return self.op(eng, lambda e: e.copy(out=out, in_=in_), reads, writes)
        return self.op(eng, lambda e: e.tensor_copy(out=out, in_=in_), reads, writes)

    def memset(self, eng, ap, val, writes=()):
        return self.op(eng, lambda e: e.memset(ap, val), (), writes)

    def recip(self, out, in_, reads=(), writes=()):
        return self.op("vector", lambda e: e.reciprocal(out=out, in_=in_), reads, writes)

    def reduce(self, out, in_, op, reads=(), writes=()):
        return self.op("vector", lambda e: e.tensor_reduce(out=out, in_=in_, axis=AX.X, op=op), reads, writes)

    def emit(self, final_waits=()):
        nc = self.nc
        es = self.es
        esem = {(e, ph): es.enter_context(nc.semaphore("s_%s_%d" % (e, ph)))
                for e in self.ENGS for ph in range(self.phase + 1)}
        dsem = {k: es.enter_context(nc.semaphore("d_" + str(k))) for k in self.dma_counts}
        for e in self.ENGS:
            c = {}
            for o in self.ops[e]:
                if o.is_dma:
                    continue
                if o.needs_inc:
                    c[o.phase] = c.get(o.phase, 0) + 1
                    o.semval = c[o.phase]
        block = es.enter_context(nc.Block())

        def run(ename):
            def body(eng):
                known = {}
                for o in self.ops[ename]:
                    for d in o.deps:
                        if d.is_dma:
                            key, val, sem = ("d", d.dsem), d.dval, dsem[d.dsem]
                        else:
                            key, val, sem = ("e", d.eng, d.phase), d.semval, esem[(d.eng, d.phase)]
                        if known.get(key, 0) >= val:
                            continue
                        known[key] = val
                        eng.wait_ge(sem, val)
                    ins = o.fn(eng)
                    if o.is_dma:
                        ins.then_inc(dsem[o.dsem], 16)
                    elif o.needs_inc:
                        ins.then_inc(esem[(ename, o.phase)], 1)
                if ename == "sync":
                    for k in final_waits:
                        eng.wait_ge(dsem[k], 16 * self.dma_counts[k])
            return body

        block.tensor(run("tensor"))
        block.vector(run("vector"))
        block.scalar(run("scalar"))
        block.gpsimd(run("gpsimd"))
        block.sync(run("sync"))
        es.close()


class Arena:
    def __init__(self, P, nf32):
        self.t = P.sbuf("arena", [128, nf32], F32)
        self.n = nf32
        self.off = 0

    def reset(self, to=0):
        self.off = to

    def alloc(self, shape, dt, at=None):
        per = 1
        for s in shape[1:]:
            per *= s
        nf = (per + 1) // 2 if dt == BF16 else per
        off = self.off if at is None else at
        assert off + nf <= self.n, ("arena overflow", off, nf, self.n)
        v = self.t[0:shape[0], off:off + nf]
        if dt == BF16:
            v = v.bitcast(BF16)
            if per % 2:
                v = v[:, 0:per]
        if len(shape) == 3:
            v = v.rearrange("p (a b) -> p a b", b=shape[2])
        elif len(shape) == 4:
            v = v.rearrange("p (a b c) -> p a b c", b=shape[2], c=shape[3])
        if at is None:
            self.off = off + nf
        return v


def _t5_bucket(rel):
    half, max_exact = 16, 8
    ret = np.where(rel > 0, half, 0)
    n = np.abs(rel)
    nf = np.maximum(n, 1).astype(np.float32)
    large = max_exact + (np.log(nf / np.float32(max_exact)) / np.float32(math.log(128 / max_exact))
                         * np.float32(half - max_exact)).astype(np.int32)
    large = np.minimum(large, half - 1)
    return ret + np.where(n < max_exact, n, large)


def _host_layout(inp):
    w_in = np.asarray(inp["w_in"], np.float32)[0]
    cols = []
    for j in range(4):
        cols.append(w_in[:, O_QA + j * 128:O_QA + (j + 1) * 128])
    for j in range(4):
        cols.append(w_in[:, O_KA + j * 128:O_KA + (j + 1) * 128])
    qi = w_in[:, O_QI:O_QI + 256]
    cols.append(qi[:, 0:96])
    cols.append(qi[:, 96:192])
    cols.append(np.concatenate([qi[:, 192:256], np.zeros((D, 32), np.float32)], axis=1))
    ki = w_in[:, O_KI:O_KI + 32]
    cols.append(np.concatenate([ki, ki, ki], axis=1))
    for j in range(2):
        cols.append(w_in[:, O_QB + j * 128:O_QB + (j + 1) * 128])
    for j in range(2):
        cols.append(w_in[:, O_KB + j * 128:O_KB + (j + 1) * 128])
    cols.append(w_in[:, O_GD:O_GD + 16])
    w_fm = np.ascontiguousarray(np.concatenate(cols, axis=1))
    assert w_fm.shape[1] == NFM
    w_tm = np.ascontiguousarray(np.concatenate([
        w_in[:, O_VA:O_VA + 512], w_in[:, O_KB:O_KB + 256], w_in[:, O_WI:O_WI + 8],
        w_in[:, O_VB:O_VB + 512], w_in[:, O_OG:O_OG + 512]], axis=1))
    assert w_tm.shape[1] == NTM

    c = np.zeros((128, NCONST), np.float32)
    ii = np.arange(128)
    c[:, C_ID:C_ID + 128] = np.eye(128, dtype=np.float32)
    u01 = (ii[:, None] <= ii[None, :]).astype(np.float32)
    c[:, C_U01:C_U01 + 128] = u01
    c[:, C_UNEG:C_UNEG + 128] = -u01 / 16.0
    c[:, C_LNEG:C_LNEG + 128] = -(ii[:, None] > ii[None, :]).astype(np.float32) / 16.0
    dm = np.zeros((128, 128), np.float32)
    dm[:64, 64:] = NEG
    c[:, C_DMASK:C_DMASK + 128] = dm
    c[:, C_GMIX:C_GMIX + 8] = np.asarray(inp["norm_mix"], np.float32)[0].reshape(8, 128).T
    c[:, C_GFFN:C_GFFN + 8] = np.asarray(inp["norm_ffn"], np.float32)[0].reshape(8, 128).T
    rb = np.asarray(inp["rel_bias"], np.float32)
    c[:, C_CFAR:C_CFAR + 8] = rb[15][None, :]
    c[:, C_GLAN:C_GLAN + 512] = np.asarray(inp["gla_norm"], np.float32)[0][None, :]
    c[:, C_GFIN:C_GFIN + 1024] = np.asarray(inp["norm_final"], np.float32)[None, :]
    bt = np.zeros((128, 2, 8, 128), np.float32)
    for which in range(2):
        rel = (ii[:, None] - 128 * which) - ii[None, :]
        bk = _t5_bucket(rel)
        bt[:, which] = rb[bk].transpose(0, 2, 1)
    c[:, C_BIAS:C_BIAS + 2048] = bt.reshape(128, 2048)
    wgu = np.concatenate([np.asarray(inp["w_gate_up"], np.float32)[0],
                          np.asarray(inp["b_gate"], np.float32)[0][None, :]], axis=0)
    return {
        "w_fm": w_fm, "w_tm": w_tm,
        "w_out": np.ascontiguousarray(np.asarray(inp["w_out"], np.float32)[0]),
        "w_g": np.ascontiguousarray(np.asarray(inp["w_ffn_gate"], np.float32)[0]),
        "w_u": np.ascontiguousarray(np.asarray(inp["w_ffn_up"], np.float32)[0]),
        "w_d": np.ascontiguousarray(np.asarray(inp["w_ffn_down"], np.float32)[0]),
        "consts": c, "wgu": np.ascontiguousarray(wgu),
    }


def build_nc(debug=False, nseq=2):
    nc = bass.Bass("TRN2", target_bir_lowering=False)
    x_d = nc.dram_tensor("x", [2, S, D], F32, kind="ExternalInput").ap()
    wfm_d = nc.dram_tensor("w_fm", [D, NFM], F32, kind="ExternalInput").ap()
    wtm_d = nc.dram_tensor("w_tm", [D, NTM], F32, kind="ExternalInput").ap()
    wout_d = nc.dram_tensor("w_out", [D, D], F32, kind="ExternalInput").ap()
    wg_d = nc.dram_tensor("w_g", [D, DFF], F32, kind="ExternalInput").ap()
    wu_d = nc.dram_tensor("w_u", [D, DFF], F32, kind="ExternalInput").ap()
    wd_d = nc.dram_tensor("w_d", [DFF, D], F32, kind="ExternalInput").ap()
    c_d = nc.dram_tensor("consts", [128, NCONST], F32, kind="ExternalInput").ap()
    wgu_d = nc.dram_tensor("wgu", [17, 256], F32, kind="ExternalInput").ap()
    y_d = nc.dram_tensor("y", [2, S, D], F32, kind="ExternalOutput").ap()
    x1_d = nc.dram_tensor("x1_scratch", [S, D], F32, kind="Internal").ap()
    dbg = {}
    if debug:
        for nm, shp in (("d_qaT", [128, 4, S]), ("d_oa", [S, 512]), ("d_ob", [S, 512]), ("d_sc", [128, S]),
                        ("d_x1", [S, D])):
            dbg[nm] = nc.dram_tensor(nm, shp, F32 if nm in ("d_sc", "d_x1") else BF16, kind="ExternalOutput").ap()

    P = Prog(nc)
    cmin_t = P.sbuf("cmin", [128, C_MIN], F32)
    cmin = Buf(cmin_t[:], "cmin")
    identb_t = P.sbuf("identb", [128, 128], BF16)
    identb = Buf(identb_t[:], "identb")
    wgu_t = P.sbuf("wgu", [17, 256], F32)
    wgu = Buf(wgu_t[:], "wgu")
    obT_t = P.sbuf("obT", [128, 4, S], BF16)
    stat_t = P.sbuf("stat", [128, 64], F32)
    ARENA_F = 47800
    ar = Arena(P, ARENA_F)
    OA_OFF = ARENA_F - 4096

    pf_t = [P.psum("pf%d" % i, [128, 512], F32) for i in range(6)]
    pb_t = [P.psum("pb%d" % i, [128, 1024], BF16) for i in range(2)]
    pf = [Buf(t[:], "pf") for t in pf_t]
    pb = [Buf(t[:], "pb") for t in pb_t]

    ident_f = cmin_t[:, C_ID:C_ID + 128]
    u01 = cmin_t[:, C_U01:C_U01 + 128]
    uneg = cmin_t[:, C_UNEG:C_UNEG + 128]
    lneg = cmin_t[:, C_LNEG:C_LNEG + 128]
    dmask = cmin_t[:, C_DMASK:C_DMASK + 128]
    gmixT = cmin_t[:, C_GMIX:C_GMIX + 8]
    gffnT = cmin_t[:, C_GFFN:C_GFFN + 8]
    cfar = cmin_t[:, C_CFAR:C_CFAR + 8]

    P.dma("sync", cmin.ap, c_d[:, 0:C_MIN], "cmin", writes=[cmin])
    P.dma("sync", wgu.ap, wgu_d, "wgu", writes=[wgu])
    P.copy("vector", identb.ap, ident_f, reads=[cmin], writes=[identb])

    stat_bufs = [Buf(stat_t[:, 4 * i:4 * i + 4], "stat%d" % i) for i in range(8)]
    stat_ctr = [0]

    def next_stat():
        b = stat_bufs[stat_ctr[0] % 8]
        stat_ctr[0] += 1
        return b

    evac_ctr = [0]

    def evac(out, in_, reads, writes=()):
        evac_ctr[0] += 1
        eng = "scalar" if evac_ctr[0] % 2 else "vector"
        return P.copy(eng, out, in_, reads=reads, writes=writes)

    def rms_to_T(src_buf, src_ap, xs_buf, dstT_ap, gT, pbuf):
        st = next_stat()
        P.act(xs_buf.ap, src_ap, AF.Square, reads=[src_buf], writes=[xs_buf, st], accum=st.ap[:, 0:1])
        P.act(st.ap[:, 1:2], st.ap[:, 0:1], AF.Ln, reads=[st], writes=[st], scale=1.0 / D, bias=EPS)
        P.act(st.ap[:, 2:3], st.ap[:, 1:2], AF.Exp, reads=[st], writes=[st], scale=-0.5)
        P.ts("vector", xs_buf.ap, src_ap, st.ap[:, 2:3], None, ALU.mult, reads=[src_buf, st], writes=[xs_buf])
        pv = pbuf.ap.rearrange("p (a b) -> p a b", b=128)
        for kc in range(8):
            P.transpose(pv[:, kc, :], xs_buf.ap[:, kc * 128:(kc + 1) * 128], identb.ap,
                        reads=[xs_buf, identb], writes=[pbuf])
        P.tt("vector", dstT_ap, pv, gT.unsqueeze(2).to_broadcast([128, 8, 128]), ALU.mult,
             reads=[pbuf, cmin], writes=[])
        return st

    for seq in range(nseq):
        P.barrier()
        ar.reset(0)
        qaT = ar.alloc([128, 4, S], BF16)
        kaT = ar.alloc([128, 4, S], BF16)
        va = ar.alloc([128, NT, 8, 66], BF16)
        qiT = ar.alloc([96, 3, S], BF16)
        kiT = ar.alloc([96, S], BF16)
        wi = ar.alloc([128, NT, 8], F32)
        AB_END = ar.off
        wfm = ar.alloc([128, 8, NFM], BF16)
        wtm = ar.alloc([128, 8, NTM], BF16)
        wfm_b1, wfm_b2, wtm_b = Buf(wfm, "wfm1"), Buf(wfm, "wfm2"), Buf(wtm, "wtm")
        glan = ar.alloc([128, 512], F32)
        glan_b = Buf(glan, "glan")
        hTs = [ar.alloc([128, 8, 512], BF16) for _ in range(2)]
        hT_bs = [Buf(hTs[i], "hT%d" % i) for i in range(2)]
        xin = [Buf(ar.alloc([128, D], F32), "xin%d" % i) for i in range(2)]
        xs = [Buf(ar.alloc([128, D], BF16), "xs%d" % i) for i in range(2)]
        qbT = Buf(ar.alloc([128, 2, 512], F32), "qbT")
        kbT = Buf(ar.alloc([128, 2, 512], F32), "kbT")
        gdT = Buf(ar.alloc([17, 512], F32), "gdT")
        kbtm = [Buf(ar.alloc([128, 256], F32), "kbtm") for _ in range(2)]
        vbtm = [Buf(ar.alloc([128, 512], BF16), "vbtm") for _ in range(2)]
        sg = [Buf(ar.alloc([128, 512], F32), "sg") for _ in range(2)]
        etmp = Buf(ar.alloc([128, 256], F32), "etmp")
        lsb = Buf(ar.alloc([128, 256], F32), "lsb")
        e1Ts = [Buf(ar.alloc([128, 2, 128], F32), "e1T") for _ in range(2)]
        e2T = Buf(ar.alloc([128, 2, 128], F32), "e2T")
        e3 = Buf(ar.alloc([128, 256], F32), "e3")
        qinTs = [Buf(ar.alloc([128, 2, 128], BF16), "qinT") for _ in range(2)]
        kinTs = [Buf(ar.alloc([128, 2, 128], BF16), "kinT") for _ in range(2)]
        kdecs = [Buf(ar.alloc([128, 256], BF16), "kdec") for _ in range(2)]
        attS = [Buf(ar.alloc([128, 128], BF16), "attS") for _ in range(2)]
        on = Buf(ar.alloc([128, 512], F32), "on")
        obss = [Buf(ar.alloc([128, 512], BF16), "obs%d" % i) for i in range(2)]
        stf = Buf(ar.alloc([128, 2, 128], F32), "stf")
        stb = Buf(ar.alloc([128, 2, 128], BF16), "stb")

        wfm_src = wfm_d.rearrange("(kc p) n -> p kc n", p=128)
        P.dma("gpsimd", wfm[:, :, 0:1024], wfm_src[:, :, 0:1024], "wfm1", writes=[wfm_b1])
        P.dma("gpsimd", wfm[:, :, 1024:NFM], wfm_src[:, :, 1024:NFM], "wfm2", writes=[wfm_b2])
        P.dma("gpsimd", wtm, wtm_d.rearrange("(kc p) n -> p kc n", p=128), "wtm", writes=[wtm_b])
        P.dma("sync", glan, c_d[:, C_GLAN:C_GLAN + 512], "glan", writes=[glan_b])
        P.memset("gpsimd", va[:, :, :, 64:66], 1.0)
        P.memset("gpsimd", gdT.ap, 1.0, writes=[gdT])
        P.memset("vector", stf.ap, 0.0, writes=[stf])
        P.memset("vector", stb.ap, 0.0, writes=[stb])

        fm_off = {}
        o = 0
        for nm, m in FM_TILES:
            fm_off[nm] = (o, m)
            o += m
        mm_ctr = [0]
        zps = Buf(pf_t[2][:, 0:256], "zps")
        rps = Buf(pf_t[2][:, 256:512], "rps")
        cps = Buf(pf_t[3][:, 0:256], "cps")
        ops_ = pf[4]
        aps2 = [Buf(pf_t[5][:, 0:128], "aps0"), Buf(pf_t[5][:, 128:256], "aps1")]
        kvp = Buf(pf_t[5][:, 256:512], "kvp")

        def prep(tb):
            hT, hT_b = hTs[tb % 2], hT_bs[tb % 2]
            for i in range(4):
                t = tb * 4 + i
                xb = xin[t % 2]
                P.dma("sync", xb.ap, x_d[seq, t * 128:(t + 1) * 128, :], "xin%d" % (t % 2), writes=[xb])
                st = next_stat()
                xsb = xs[t % 2]
                P.act(xsb.ap, xb.ap, AF.Square, reads=[xb], writes=[xsb, st], accum=st.ap[:, 0:1])
                P.act(st.ap[:, 1:2], st.ap[:, 0:1], AF.Ln, reads=[st], writes=[st], scale=1.0 / D, bias=EPS)
                P.act(st.ap[:, 2:3], st.ap[:, 1:2], AF.Exp, reads=[st], writes=[st], scale=-0.5)
                P.ts("vector", xsb.ap, xb.ap, st.ap[:, 2:3], None, ALU.mult, reads=[xb, st], writes=[xsb])
                pbuf = pb[t % 2]
                pv = pbuf.ap.rearrange("p (a b) -> p a b", b=128)
                for kc in range(8):
                    P.transpose(pv[:, kc, :], xsb.ap[:, kc * 128:(kc + 1) * 128], identb.ap,
                                reads=[xsb, identb], writes=[pbuf])
                P.tt("vector", hT[:, :, i * 128:(i + 1) * 128], pv,
                     gmixT.unsqueeze(2).to_broadcast([128, 8, 128]), ALU.mult,
                     reads=[pbuf, cmin], writes=[hT_b])

        def fm_block(tb):
            hT, hT_b = hTs[tb % 2], hT_bs[tb % 2]
            cs = slice(tb * 512, (tb + 1) * 512)
            for nm, m in FM_TILES:
                off, _ = fm_off[nm]
                ps = pf[mm_ctr[0] % 2]
                mm_ctr[0] += 1
                for kc in range(8):
                    P.matmul(ps.ap[0:m, :], wfm[:, kc, off:off + m], hT[:, kc, :], kc == 0, kc == 7,
                             reads=[wfm_b1 if off < 1024 else wfm_b2, hT_b], writes=[ps])
                if nm.startswith("qa"):
                    evac(qaT[:, int(nm[2]), cs], ps.ap, [ps])
                elif nm.startswith("ka"):
                    evac(kaT[:, int(nm[2]), cs], ps.ap, [ps])
                elif nm.startswith("qi"):
                    evac(qiT[:, int(nm[2]), cs], ps.ap[0:96, :], [ps])
                elif nm == "ki":
                    evac(kiT[:, cs], ps.ap[0:96, :], [ps])
                elif nm.startswith("qb"):
                    evac(qbT.ap[:, int(nm[2]), :], ps.ap, [ps], [qbT])
                elif nm.startswith("kb"):
                    evac(kbT.ap[:, int(nm[2]), :], ps.ap, [ps], [kbT])
                else:
                    evac(gdT.ap[0:16, :], ps.ap[0:16, :], [ps], [gdT])

        def gla_s1(t):
            tb, i = t // 4, t % 4
            hT, hT_b = hTs[tb % 2], hT_bs[tb % 2]
            tcs = slice(i * 128, (i + 1) * 128)
            kb_s, vb_s, sg_s = kbtm[t % 2], vbtm[t % 2], sg[t % 2]
            e1T, qinT, kinT, kdec = e1Ts[t % 2], qinTs[t % 2], kinTs[t % 2], kdecs[t % 2]
            P.matmul(zps.ap, gdT.ap[0:17, tcs], wgu.ap, True, True, reads=[gdT, wgu], writes=[zps])
            P.act(etmp.ap, zps.ap, AF.Exp, reads=[zps], writes=[etmp], scale=-1.0)
            P.act(lsb.ap, etmp.ap, AF.Ln, reads=[etmp], writes=[lsb], bias=1.0)
            for g, (goff, gn) in enumerate(((0, 512), (512, 264), (776, 512), (1288, 512))):
                ps = pf[mm_ctr[0] % 2]
                mm_ctr[0] += 1
                for kc in range(8):
                    P.matmul(ps.ap[:, 0:gn], hT[:, kc, tcs], wtm[:, kc, goff:goff + gn], kc == 0, kc == 7,
                             reads=[wtm_b, hT_b], writes=[ps])
                if g == 0:
                    evac(va[:, t, :, 0:64], ps.ap.rearrange("p (h d) -> p h d", d=64), [ps])
                elif g == 1:
                    P.copy("vector", kb_s.ap, ps.ap[:, 0:256], reads=[ps], writes=[kb_s])
                    P.copy("vector", wi[:, t, :], ps.ap[:, 256:264], reads=[ps])
                elif g == 2:
                    P.copy("scalar", vb_s.ap, ps.ap, reads=[ps], writes=[vb_s])
                else:
                    P.act(sg_s.ap, ps.ap, AF.Silu, reads=[ps], writes=[sg_s])
            cv = cps.ap.rearrange("p (a b) -> p a b", b=128)
            for pr in range(2):
                P.matmul(cv[:, pr, :], lsb.ap[:, pr * 128:(pr + 1) * 128], uneg, True, True,
                         reads=[lsb, cmin], writes=[cps])
            P.matmul(rps.ap, lneg, lsb.ap, True, True, reads=[lsb, cmin], writes=[rps])
            P.act(e1T.ap, cv, AF.Exp, reads=[cps], writes=[e1T])
            P.act(e2T.ap, cv, AF.Exp, reads=[cps], writes=[e2T], scale=-1.0)
            P.act(e3.ap, rps.ap, AF.Exp, reads=[rps], writes=[e3])
            P.stt(qinT.ap, qbT.ap[:, :, tcs], 0.125, e1T.ap, ALU.mult, ALU.mult,
                  reads=[qbT, e1T], writes=[qinT])
            P.tt("vector", kinT.ap, kbT.ap[:, :, tcs], e2T.ap, ALU.mult, reads=[kbT, e2T], writes=[kinT])
            P.tt("gpsimd", kdec.ap, kb_s.ap, e3.ap, ALU.mult, reads=[kb_s, e3], writes=[kdec])

        def gla_s2(t):
            vb_s, sg_s = vbtm[t % 2], sg[t % 2]
            e1T, qinT, kinT, kdec = e1Ts[t % 2], qinTs[t % 2], kinTs[t % 2], kdecs[t % 2]
            for pr in range(2):
                for hh in range(2):
                    h = 2 * pr + hh
                    hp = slice(hh * 64, hh * 64 + 64)
                    aps = aps2[hh]
                    P.matmul(aps.ap, kinT.ap[hp, pr, :], qinT.ap[hp, pr, :], True, True,
                             reads=[kinT, qinT], writes=[aps])
                    asb = attS[hh]
                    P.tt("vector", asb.ap, aps.ap, u01, ALU.mult, reads=[aps, cmin], writes=[asb])
                    P.matmul(ops_.ap[:, h * 128:(h + 1) * 128], asb.ap, vb_s.ap[:, h * 128:(h + 1) * 128],
                             True, False, reads=[asb, vb_s], writes=[ops_])
                    P.matmul(ops_.ap[:, h * 128:(h + 1) * 128], qinT.ap[hp, pr, :], stb.ap[hp, pr, :],
                             False, True, reads=[qinT, stb], writes=[ops_])
                P.matmul(kvp.ap, kdec.ap[:, pr * 128:(pr + 1) * 128],
                         vb_s.ap[:, pr * 256:(pr + 1) * 256], True, True, reads=[kdec, vb_s], writes=[kvp])
                for hh in range(2):
                    hp = slice(hh * 64, hh * 64 + 64)
                    P.stt(stf.ap[hp, pr, :], stf.ap[hp, pr, :], e1T.ap[hp, pr, 127:128],
                          kvp.ap[hp, hh * 128:(hh + 1) * 128], ALU.mult, ALU.add,
                          reads=[stf, e1T, kvp], writes=[stf])
            P.copy("gpsimd", stb.ap, stf.ap, reads=[stf], writes=[stb])
            st = next_stat()
            P.act(on.ap, ops_.ap, AF.Square, reads=[ops_], writes=[on])
            P.reduce(st.ap[:, 0:4], on.ap.rearrange("p (h v) -> p h v", v=128), ALU.add, reads=[on], writes=[st])
            st2 = next_stat()
            P.act(st2.ap[:, 0:4], st.ap[:, 0:4], AF.Ln, reads=[st], writes=[st2], scale=1.0 / 128, bias=EPS)
            st3 = next_stat()
            P.act(st3.ap[:, 0:4], st2.ap[:, 0:4], AF.Exp, reads=[st2], writes=[st3], scale=-0.5)
            P.tt("vector", on.ap.rearrange("p (h v) -> p h v", v=128),
                 ops_.ap.rearrange("p (h v) -> p h v", v=128),
                 st3.ap[:, 0:4].unsqueeze(2).to_broadcast([128, 4, 128]), ALU.mult,
                 reads=[ops_, st3], writes=[on])
            P.tt("gpsimd", on.ap, on.ap, glan, ALU.mult, reads=[on, glan_b], writes=[on])
            obs = obss[t % 2]
            P.tt("vector", obs.ap, on.ap, sg_s.ap, ALU.mult, reads=[on, sg_s], writes=[obs])
            if debug and seq == 0:
                P.dma("sync", dbg["d_ob"][t * 128:(t + 1) * 128, :], obs.ap, "dbg_ob", reads=[obs])

        def gla_s3(t):
            obs = obss[t % 2]
            pbuf = pb[t % 2]
            pv = pbuf.ap.rearrange("p (a b) -> p a b", b=128)
            for j in range(4):
                P.transpose(pv[:, j, :], obs.ap[:, j * 128:(j + 1) * 128], identb.ap,
                            reads=[obs, identb], writes=[pbuf])
            P.copy("scalar", obT_t[:, :, t * 128:(t + 1) * 128], pv[:, 0:4, :], reads=[pbuf])

        prep(0)
        for t in range(NT):
            if t % 4 == 0:
                fm_block(t // 4)
                if t // 4 + 1 < 4:
                    prep(t // 4 + 1)
            gla_s1(t)
            if t >= 1:
                gla_s2(t - 1)
            if t >= 2:
                gla_s3(t - 2)
        gla_s2(NT - 1)
        gla_s3(NT - 2)
        gla_s3(NT - 1)
        if debug and seq == 0:
            P.barrier()
            P.dma("sync", dbg["d_qaT"], qaT, "dbg_qaT")

        P.barrier()
        ar.reset(AB_END)
        bias8 = ar.alloc([128, 2, 8, 128], BF16)
        bias8_b = Buf(bias8, "bias8")
        biasT = ar.alloc([128, 2, 8, 128], F32)
        biasT_b = Buf(biasT, "biasT")
        P.dma("sync", biasT.rearrange("p a b c -> p (a b c)"), c_d[:, C_BIAS:C_BIAS + 2048], "biasT", writes=[biasT_b])
        P.ts("vector", bias8, biasT, 8.0, None, ALU.mult, reads=[biasT_b], writes=[bias8_b])
        sc = [Buf(ar.alloc([128, S], F32), "sc%d" % i) for i in range(4)]
        junkB = ar.alloc([128, S], BF16)
        msk = [Buf(ar.alloc([128, S], BF16), "msk%d" % i) for i in range(4)]
        mskT = [Buf(ar.alloc([128, NT, 128], BF16), "mskT%d" % i) for i in range(4)]
        rsb = [Buf(ar.alloc([128, 512], F32), "rsb%d" % i) for i in range(3)]
        NPMAX = 5
        ebuf = [Buf(ar.alloc([128, 4, 128], BF16), "e%d" % i) for i in range(NPMAX)]
        pbuf_ = [Buf(ar.alloc([128, 4, 128], BF16), "p%d" % i) for i in range(NPMAX)]
        oasb = Buf(ar.alloc([128, 512], BF16), "oasb")
        bis_t = [ar.alloc([128, 8], F32) for i in range(2)]
        midb = [Buf(b_[:, 0:2], "mid") for b_ in bis_t]
        cntb = [[Buf(b_[:, 2:3], "cnt0"), Buf(b_[:, 3:4], "cnt1")] for b_ in bis_t]
        tbb = [Buf(b_[:, 4:6], "tb") for b_ in bis_t]
        thrb = [Buf(b_[:, 6:8], "thr") for b_ in bis_t]
        nrm = Buf(ar.alloc([128, 16], F32), "nrm")
        actb_t = ar.alloc([128, 8], F32)
        nmid = Buf(actb_t[:, 0:1], "nmid")
        ssum = Buf(actb_t[:, 1:2], "ssum")
        sgnb = Buf(actb_t[:, 2:3], "sgnb")
        thrA = Buf(actb_t[:, 3:4], "thrA")
        junkB2 = ar.alloc([128, S], BF16)
        assert ar.off <= OA_OFF, ("phase B arena", ar.off, OA_OFF)
        oaT = ar.alloc([128, 4, S], BF16, at=OA_OFF)
        r_ctr = [0]
        lgb = [pf[2], pf[3], pb[1], pf[0], pf[1]]
        lgv = [pf_t[2][:], pf_t[3][:], pb_t[1][:].bitcast(F32), pf_t[0][:], pf_t[1][:]]
        pbm = Buf(pb_t[0][:, 0:512], "pbm")
        pbo = Buf(pb_t[0][:, 512:1024], "pbo")
        ops2 = [pf[4], pf[5]]
        ov = [b_.ap[:, 0:264].rearrange("p (h d) -> p h d", d=66) for b_ in ops2]

        def idx_group(g4):
            qts = [4 * g4 + k for k in range(4)]
            for k, qt in enumerate(qts):
                sk = (qt + 1) * 128
                scb = sc[k]
                qcs = slice(qt * 128, (qt + 1) * 128)
                for c0 in range(0, sk, 512):
                    w = min(512, sk - c0)
                    for h in range(8):
                        hp = slice((h % 3) * 32, (h % 3) * 32 + 32)
                        ps = pf[h % 2]
                        P.matmul(ps.ap[:, 0:w], qiT[hp, h // 3, qcs], kiT[hp, c0:c0 + w], True, True, writes=[ps])
                        rb_ = rsb[r_ctr[0] % 3]
                        r_ctr[0] += 1
                        P.act(rb_.ap[:, 0:w], ps.ap[:, 0:w], AF.Relu, reads=[ps], writes=[rb_])
                        if h == 0:
                            P.ts("vector", scb.ap[:, c0:c0 + w], rb_.ap[:, 0:w], wi[:, qt, 0:1], None, ALU.mult,
                                 reads=[rb_], writes=[scb])
                        else:
                            P.stt(scb.ap[:, c0:c0 + w], rb_.ap[:, 0:w], wi[:, qt, h:h + 1], scb.ap[:, c0:c0 + w],
                                  ALU.mult, ALU.add, reads=[rb_, scb], writes=[scb])
                P.tt("vector", scb.ap[:, sk - 128:sk], scb.ap[:, sk - 128:sk], dmask, ALU.add,
                     reads=[scb, cmin], writes=[scb])
                if debug and seq == 0 and qt == 5:
                    P.dma("sync", dbg["d_sc"], scb.ap, "dbg_sc", reads=[scb])

        def bis_group(g4):
            qts = [4 * g4 + k for k in range(4)]
            if g4 == 0:
                pairs, act_k = [(), (2, 3)], None
            else:
                pairs, act_k = [(0, 1), (2, 3)], None
            act_pairs = [pi for pi in range(2) if len(pairs[pi])]
            for pi in act_pairs:
                P.memset("vector", midb[pi].ap, 0.0, writes=[midb[pi]])
            if act_k is not None:
                P.memset("gpsimd", nmid.ap, 0.0, writes=[nmid])
                sk_a = (qts[act_k] + 1) * 128
            for it in range(1, N_BIS + 1):
                step = R_BIS / (2.0 ** it)
                for pi in act_pairs:
                    for a, k in enumerate(pairs[pi]):
                        sk = (qts[k] + 1) * 128
                        P.ts("vector", junkB[:, 0:sk], sc[k].ap[:, 0:sk], midb[pi].ap[:, a:a + 1], None,
                             ALU.is_ge, ALU.add, reads=[sc[k], midb[pi]], writes=[cntb[pi][a]],
                             accum=cntb[pi][a].ap)
                if act_k is not None:
                    P.act(junkB2[:, 0:sk_a], sc[act_k].ap[:, 0:sk_a], AF.Sign, reads=[sc[act_k], nmid],
                          writes=[ssum], bias=nmid.ap, accum=ssum.ap)
                    P.act(sgnb.ap, ssum.ap, AF.Sign, reads=[ssum], writes=[sgnb], bias=float(sk_a) - 511.5)
                    P.act(nmid.ap, sgnb.ap, AF.Identity, reads=[sgnb, nmid], writes=[nmid], scale=-step, bias=nmid.ap)
                for pi in act_pairs:
                    n_ = len(pairs[pi])
                    P.ts("vector", tbb[pi].ap[:, 0:n_], bis_t[pi][:, 2:2 + n_], 255.5, 2.0 * step, ALU.is_ge, ALU.mult,
                         reads=[cntb[pi][a] for a in range(n_)], writes=[tbb[pi]])
                for pi in act_pairs:
                    n_ = len(pairs[pi])
                    P.stt(midb[pi].ap[:, 0:n_], tbb[pi].ap[:, 0:n_], -step, midb[pi].ap[:, 0:n_], ALU.add, ALU.add,
                          reads=[tbb[pi], midb[pi]], writes=[midb[pi]])
                yield
            last_step = R_BIS / (2.0 ** N_BIS)
            for pi in act_pairs:
                n_ = len(pairs[pi])
                P.ts("vector", thrb[pi].ap[:, 0:n_], midb[pi].ap[:, 0:n_], -last_step, None, ALU.add,
                     reads=[midb[pi]], writes=[thrb[pi]])
            if act_k is not None:
                P.act(thrA.ap, nmid.ap, AF.Identity, reads=[nmid], writes=[thrA], scale=-1.0, bias=-last_step)
            thr_of = {}
            for pi in act_pairs:
                for a, k in enumerate(pairs[pi]):
                    thr_of[k] = (thrb[pi], thrb[pi].ap[:, a:a + 1])
            if act_k is not None:
                thr_of[act_k] = (thrA, thrA.ap)
            for k, qt in enumerate(qts):
                sk = (qt + 1) * 128
                if qt >= 2:
                    P.ts("vector", msk[k].ap[:, 0:sk], sc[k].ap[:, 0:sk], thr_of[k][1], None, ALU.is_ge,
                         reads=[sc[k], thr_of[k][0]], writes=[msk[k]])
                else:
                    P.ts("vector", msk[k].ap[:, 0:sk], sc[k].ap[:, 0:sk], -1.0e29, None, ALU.is_ge,
                         reads=[sc[k]], writes=[msk[k]])
            for k, qt in enumerate(qts):
                nkb = qt + 1
                mT = mskT[k]
                pv = pbm.ap.rearrange("p (a b) -> p a b", b=128)
                for c0 in range(0, nkb, 4):
                    n = min(4, nkb - c0)
                    for kk in range(n):
                        P.transpose(pv[:, kk, :], msk[k].ap[:, (c0 + kk) * 128:(c0 + kk + 1) * 128], identb.ap,
                                    reads=[msk[k], identb], writes=[pbm])
                    P.copy("scalar", mT.ap[:, c0:c0 + n, :], pv[:, 0:n, :], reads=[pbm], writes=[mT])

        def attn_group(g4):
            NPIPE = 5 if g4 == 3 else 3
            qts = [4 * g4 + k for k in range(4)]
            items = []
            for k, qt in enumerate(qts):
                near = [kb for kb in (qt - 1, qt) if kb >= 0]
                far = list(range(0, max(qt - 1, 0)))
                groups = [("far", far[c0:c0 + 4]) for c0 in range(0, len(far), 4)] + [("near", near)]
                for h in range(8):
                    for gi, (kind, kbs) in enumerate(groups):
                        items.append((k, qt, h, kind, kbs, gi == 0, gi == len(groups) - 1))

            def stage1(item, i):
                k, qt, h, kind, kbs, _, _ = item
                qcs = slice(qt * 128, (qt + 1) * 128)
                j, hp = h // 2, slice((h % 2) * 64, (h % 2) * 64 + 64)
                n = len(kbs)
                lps = lgb[i % NPIPE]
                lv = lgv[i % NPIPE].rearrange("p (a b) -> p a b", b=128)
                for kk, kb in enumerate(kbs):
                    if kind == "far":
                        P.matmul(lv[:, kk, :], kaT[hp, j, kb * 128:(kb + 1) * 128], qaT[hp, j, qcs], True, True,
                                 writes=[lps])
                    else:
                        which = 0 if kb == qt else 1
                        P.matmul(lv[:, kk, :], kaT[hp, j, kb * 128:(kb + 1) * 128], qaT[hp, j, qcs], True, False,
                                 writes=[lps])
                        P.matmul(lv[:, kk, :], identb.ap, bias8[:, which, h, :], False, True,
                                 reads=[identb, bias8_b], writes=[lps])
                eb = ebuf[i % NPIPE]
                pbf = pbuf_[i % NPIPE]
                if kind == "far":
                    P.act(eb.ap[:, 0:n, :], lv[:, 0:n, :], AF.Exp, reads=[lps, cmin], writes=[eb],
                          scale=0.125, bias=cfar[:, h:h + 1])
                else:
                    P.act(eb.ap[:, 0:n, :], lv[:, 0:n, :], AF.Exp, reads=[lps], writes=[eb], scale=0.125)
                meng = "vector" if (g4 == 3 and i % 2 == 1) else "gpsimd"
                P.tt(meng, pbf.ap[:, 0:n, :], eb.ap[:, 0:n, :], mskT[k].ap[:, kbs[0]:kbs[0] + n, :], ALU.mult,
                     reads=[eb, mskT[k]], writes=[pbf])

            def stage2(item, i):
                k, qt, h, kind, kbs, first_g, last_g = item
                qcs = slice(qt * 128, (qt + 1) * 128)
                n = len(kbs)
                pbf = pbuf_[i % NPIPE]
                for kk, kb in enumerate(kbs):
                    P.matmul(ov[h // 4][:, h % 4, 0:65], pbf.ap[:, kk, :], va[:, kb, h, 0:65],
                             first_g and kk == 0, last_g and kk == n - 1, reads=[pbf], writes=[ops2[h // 4]])
                if h == 7 and last_g:
                    for hh in range(2):
                        P.act(nrm.ap[:, hh * 4:(hh + 1) * 4].unsqueeze(2), ov[hh][:, :, 64:65], AF.Ln,
                              reads=[ops2[hh]], writes=[nrm])
                    P.act(nrm.ap[:, 8:16], nrm.ap[:, 0:8], AF.Exp, reads=[nrm], writes=[nrm], scale=-1.0)
                    for h2 in range(8):
                        P.act(oasb.ap[:, h2 * 64:(h2 + 1) * 64], ov[h2 // 4][:, h2 % 4, 0:64], AF.Copy,
                              reads=[ops2[h2 // 4], nrm], writes=[oasb], scale=nrm.ap[:, 8 + h2:9 + h2])
                    if debug and seq == 0:
                        P.dma("sync", dbg["d_oa"][qt * 128:(qt + 1) * 128, :], oasb.ap, "dbg_oa", reads=[oasb])
                    pv = pbo.ap.rearrange("p (a b) -> p a b", b=128)
                    for j2 in range(4):
                        P.transpose(pv[:, j2, :], oasb.ap[:, j2 * 128:(j2 + 1) * 128], identb.ap,
                                    reads=[oasb, identb], writes=[pbo])
                    P.copy("scalar", oaT[:, :, qcs], pv[:, 0:4, :], reads=[pbo])

            LA = NPIPE - 1
            for i in range(len(items) + LA):
                if i < len(items):
                    stage1(items[i], i)
                if i >= LA:
                    stage2(items[i - LA], i - LA)
                yield

        N_ITEMS = [48, 80, 112, 144]
        idx_group(0)
        for _ in bis_group(0):
            pass
        for g4 in range(4):
            if g4 + 1 < 4:
                idx_group(g4 + 1)
                ga = attn_group(g4)
                per = (N_ITEMS[g4] + 2 + N_BIS - 1) // N_BIS
                for _ in bis_group(g4 + 1):
                    for _i in range(per):
                        next(ga, None)
                assert next(ga, "done") == "done", "attention items not fully emitted before masks"
            else:
                for _ in attn_group(g4):
                    pass

        P.barrier()
        ar.reset(0)
        h2T = ar.alloc([128, 8, S], BF16)
        wd = ar.alloc([128, NJ, D], BF16)
        wd_b = Buf(wd, "wd")
        NR = 3
        wgr = [Buf(ar.alloc([128, 8, 256], BF16), "wg%d" % i) for i in range(NR)]
        wur = [Buf(ar.alloc([128, 8, 256], BF16), "wu%d" % i) for i in range(NR)]
        gfin = ar.alloc([128, D], F32)
        gfin_b = Buf(gfin, "gfin")
        C2_START = ar.off
        wout = ar.alloc([128, 8, D], BF16)
        wout_b = Buf(wout, "wout")
        wgv = wg_d.rearrange("(kc p) n -> p kc n", p=128)
        wuv = wu_d.rearrange("(kc p) n -> p kc n", p=128)
        P.dma("gpsimd", wout, wout_d.rearrange("(kc p) n -> p kc n", p=128), "wout", writes=[wout_b])
        for q4 in range(2):
            P.dma("gpsimd", wd[:, q4 * 11:(q4 + 1) * 11, :],
                  wd_d[q4 * 1408:(q4 + 1) * 1408, :].rearrange("(j p) n -> p j n", p=128), "wd", writes=[wd_b])
        for r in range(NR):
            P.dma("gpsimd", wgr[r].ap, wgv[:, :, r * 256:(r + 1) * 256], "wg%d" % r, writes=[wgr[r]])
            P.dma("gpsimd", wur[r].ap, wuv[:, :, r * 256:(r + 1) * 256], "wu%d" % r, writes=[wur[r]])
        P.dma("sync", gfin, c_d[:, C_GFIN:C_GFIN + 1024], "gfin", writes=[gfin_b])
        xin = [Buf(ar.alloc([128, D], F32), "xinC%d" % i) for i in range(2)]
        x1s = [Buf(ar.alloc([128, D], F32), "x1s%d" % i) for i in range(2)]
        xs = [Buf(ar.alloc([128, D], BF16), "xsC%d" % i) for i in range(2)]
        assert ar.off <= OA_OFF
        x1d_b = Buf(x1_d, "x1d")
        for t in range(NT):
            tcs = slice(t * 128, (t + 1) * 128)
            xb, x1b, xsb = xin[t % 2], x1s[t % 2], xs[t % 2]
            P.dma("sync", xb.ap, x_d[seq, tcs, :], "xinC%d" % (t % 2), writes=[xb])
            for half in range(2):
                ps = pf[(2 * t + half) % 4]
                for kc in range(8):
                    lhs = oaT[:, kc, tcs] if kc < 4 else obT_t[:, kc - 4, tcs]
                    P.matmul(ps.ap, lhs, wout[:, kc, half * 512:(half + 1) * 512], kc == 0, kc == 7,
                             reads=[wout_b], writes=[ps])
                P.tt("vector", x1b.ap[:, half * 512:(half + 1) * 512], xb.ap[:, half * 512:(half + 1) * 512], ps.ap,
                     ALU.add, reads=[xb, ps], writes=[x1b])
            P.dma("sync", x1_d[tcs, :], x1b.ap, "x1st", reads=[x1b], writes=[x1d_b])
            if debug and seq == 0:
                P.dma("sync", dbg["d_x1"][tcs, :], x1b.ap, "dbg_x1", reads=[x1b])
            rms_to_T(x1b, x1b.ap, xsb, h2T[:, :, tcs], gffnT, pb[t % 2])

        P.barrier()
        ar.reset(C2_START)
        actT = ar.alloc([128, NJ, 1024], BF16)
        actT_b = Buf(actT, "actT")
        x1r = [Buf(ar.alloc([128, D], F32), "x1r%d" % i) for i in range(2)]
        ysb = [Buf(ar.alloc([128, D], F32), "ysb%d" % i) for i in range(2)]
        osb = [Buf(ar.alloc([128, D], F32), "osb%d" % i) for i in range(2)]
        sgc = [Buf(ar.alloc([128, 512], F32), "sgc%d" % i) for i in range(2)]
        junkC = ar.alloc([128, D], BF16)
        ring = [0]
        cctr = [0]
        for cb in range(2):
            for gj in range(NJ // 2):
                r = ring[0] % NR
                ring[0] += 1
                if ring[0] > NR:
                    P.dma("gpsimd", wgr[r].ap, wgv[:, :, gj * 256:(gj + 1) * 256], "wg%d" % r, writes=[wgr[r]])
                    P.dma("gpsimd", wur[r].ap, wuv[:, :, gj * 256:(gj + 1) * 256], "wu%d" % r, writes=[wur[r]])
                for jj in range(2):
                    j = 2 * gj + jj
                    for hb in range(2):
                        tok = slice(cb * 1024 + hb * 512, cb * 1024 + (hb + 1) * 512)
                        gps = pf[(cctr[0] % 2) * 2]
                        ups = pf[(cctr[0] % 2) * 2 + 1]
                        sgb = sgc[cctr[0] % 2]
                        cctr[0] += 1
                        for kc in range(8):
                            P.matmul(gps.ap, wgr[r].ap[:, kc, jj * 128:(jj + 1) * 128], h2T[:, kc, tok], kc == 0, kc == 7,
                                     reads=[wgr[r]], writes=[gps])
                        for kc in range(8):
                            P.matmul(ups.ap, wur[r].ap[:, kc, jj * 128:(jj + 1) * 128], h2T[:, kc, tok], kc == 0, kc == 7,
                                     reads=[wur[r]], writes=[ups])
                        P.act(sgb.ap, gps.ap, AF.Silu, reads=[gps], writes=[sgb])
                        P.tt("vector", actT[:, j, hb * 512:(hb + 1) * 512], sgb.ap, ups.ap, ALU.mult,
                             reads=[sgb, ups], writes=[actT_b])
            for ti in range(8):
                t = cb * 8 + ti
                tcs = slice(t * 128, (t + 1) * 128)
                xr, yb, ob_ = x1r[t % 2], ysb[t % 2], osb[t % 2]
                P.dma("sync", xr.ap, x1_d[tcs, :], "x1r%d" % (t % 2), reads=[x1d_b], writes=[xr])
                for half in range(2):
                    ps = pf[4 + half]
                    for j in range(NJ):
                        P.matmul(ps.ap, actT[:, j, ti * 128:(ti + 1) * 128], wd[:, j, half * 512:(half + 1) * 512],
                                 j == 0, j == NJ - 1, reads=[actT_b, wd_b], writes=[ps])
                    P.tt("vector", yb.ap[:, half * 512:(half + 1) * 512], xr.ap[:, half * 512:(half + 1) * 512], ps.ap,
                         ALU.add, reads=[xr, ps], writes=[yb])
                st = next_stat()
                P.act(junkC, yb.ap, AF.Square, reads=[yb], writes=[st], accum=st.ap[:, 0:1])
                P.act(st.ap[:, 1:2], st.ap[:, 0:1], AF.Ln, reads=[st], writes=[st], scale=1.0 / D, bias=EPS)
                P.act(st.ap[:, 2:3], st.ap[:, 1:2], AF.Exp, reads=[st], writes=[st], scale=-0.5)
                P.stt(ob_.ap, yb.ap, st.ap[:, 2:3], gfin, ALU.mult, ALU.mult, reads=[yb, st, gfin_b], writes=[ob_])
                P.dma("sync", y_d[seq, tcs, :], ob_.ap, "yout%d" % (t % 2), reads=[ob_])

    P.emit(final_waits=["yout0", "yout1"])
    return nc


_NC_CACHE = {}


def kernel(**inputs):
    x = np.ascontiguousarray(np.asarray(inputs["x"], np.float32))
    lay = _host_layout(inputs)
    if "nc" not in _NC_CACHE:
        _NC_CACHE["nc"] = build_nc()
    nc = _NC_CACHE["nc"]
    in_maps = []
    for c in range(8):
        m = {"x": np.ascontiguousarray(x[2 * c:2 * c + 2])}
        m.update(lay)
        in_maps.append(m)
    res = run_bass_kernel_spmd(nc, in_maps, core_ids=list(range(8)))
    out = np.concatenate([np.asarray(r["y"], np.float32).reshape(2, S, D) for r in res.results], axis=0)
    return out
```

```python
import math
from contextlib import ExitStack

import numpy as np
import concourse.bass as bass
import concourse.mybir as mybir
from concourse.bass_utils import run_bass_kernel_spmd

F32 = mybir.dt.float32
BF16 = mybir.dt.bfloat16
ALU = mybir.AluOpType
AF = mybir.ActivationFunctionType
AX = mybir.AxisListType

S = 2048
D = 1024
NT = S // 128
DFF = 2816
NJ = DFF // 128
NEG = -1.0e30
EPS = 1e-6

O_QA, O_KA, O_VA, O_QI, O_KI, O_WI, O_QB, O_KB, O_VB, O_GD, O_OG = (
    0, 512, 1024, 1536, 1792, 1824, 1832, 2088, 2344, 2856, 2872)

FM_TILES = ([("qa%d" % j, 128) for j in range(4)] + [("ka%d" % j, 128) for j in range(4)]
            + [("qi%d" % j, 96) for j in range(3)] + [("ki", 96)]
            + [("qb%d" % j, 128) for j in range(2)] + [("kb%d" % j, 128) for j in range(2)]
            + [("gd", 16)])
NFM = sum(m for _, m in FM_TILES)
NTM = 512 + 264 + 512 + 512

C_ID, C_U01, C_UNEG, C_LNEG, C_DMASK, C_GMIX, C_GFFN, C_CFAR = 0, 128, 256, 384, 512, 640, 648, 656
C_MIN = 672
C_GLAN = 672
C_GFIN = C_GLAN + 512
C_BIAS = C_GFIN + 1024
NCONST = C_BIAS + 2048

R_BIS = 128.0
N_BIS = 16


class Buf:
    __slots__ = ("ap", "name", "last_w", "readers")

    def __init__(self, ap, name=""):
        self.ap = ap
        self.name = name
        self.last_w = None
        self.readers = []


class Op:
    __slots__ = ("eng", "fn", "deps", "needs_inc", "semval", "is_dma", "dsem", "dval", "phase")

    def __init__(self, eng, fn):
        self.eng = eng
        self.fn = fn
        self.deps = []
        self.needs_inc = False
        self.semval = None
        self.is_dma = False
        self.dsem = None
        self.dval = None


class Prog:
    ENGS = ("tensor", "vector", "scalar", "gpsimd", "sync")

    def __init__(self, nc):
        self.nc = nc
        self.ops = {e: [] for e in self.ENGS}
        self.es = ExitStack()
        self.dma_counts = {}
        self.dma_last = {}
        self.phase = 0

    def sbuf(self, name, shape, dt):
        return self.es.enter_context(self.nc.sbuf_tensor("sb_" + name, list(shape), dt))

    def psum(self, name, shape, dt):
        return self.es.enter_context(self.nc.psum_tensor("ps_" + name, list(shape), dt))

    def _dep(self, op, src):
        if src is None or src is op:
            return
        if src.eng == "tensor" and op.eng == "tensor" and not src.is_dma and not op.is_dma:
            return
        op.deps.append(src)
        if not src.is_dma:
            src.needs_inc = True

    def op(self, eng, fn, reads=(), writes=(), dma_key=None, extra=()):
        o = Op(eng, fn)
        o.phase = self.phase
        if dma_key is not None:
            o.is_dma = True
            o.dsem = dma_key
            self.dma_counts[dma_key] = self.dma_counts.get(dma_key, 0) + 1
            o.dval = 16 * self.dma_counts[dma_key]
            self.dma_last[dma_key] = o
        for s in extra:
            self._dep(o, s)
        for b in reads:
            self._dep(o, b.last_w)
        for b in writes:
            lw = b.last_w
            if lw is not None and not (lw.eng == eng and not lw.is_dma and not o.is_dma):
                self._dep(o, lw)
            for r in b.readers:
                if r.eng == eng and not r.is_dma and not o.is_dma:
                    continue
                self._dep(o, r)
        for b in reads:
            b.readers.append(o)
        for b in writes:
            b.last_w = o
            b.readers = []
        self.ops[eng].append(o)
        return o

    def barrier(self):
        lasts = []
        for e in self.ENGS:
            for o in reversed(self.ops[e]):
                if not o.is_dma:
                    lasts.append(o)
                    break
        lasts += list(self.dma_last.values())
        for e in self.ENGS:
            self.op(e, lambda eng: eng.nop(), extra=[l for l in lasts])
        self.phase += 1

    def dma(self, eng, out_ap, in_ap, key, reads=(), writes=()):
        return self.op(eng, lambda e: e.dma_start(out=out_ap, in_=in_ap), reads, writes, dma_key=key)

    def matmul(self, out, lhsT, rhs, start, stop, reads=(), writes=()):
        return self.op("tensor", lambda e: e.matmul(out, lhsT=lhsT, rhs=rhs, start=start, stop=stop), reads, writes)

    def transpose(self, out, in_, ident, reads=(), writes=()):
        return self.op("tensor", lambda e: e.transpose(out, in_, ident), reads, writes)

    def act(self, out, in_, func, reads=(), writes=(), bias=None, scale=None, accum=None):
        kw = {}
        if bias is not None:
            kw["bias"] = bias
        if scale is not None:
            kw["scale"] = scale
        if accum is not None:
            kw["accum_out"] = accum
        return self.op("scalar", lambda e: e.activation(out=out, in_=in_, func=func, **kw), reads, writes)

    def tt(self, eng, out, in0, in1, op, reads=(), writes=()):
        return self.op(eng, lambda e: e.tensor_tensor(out=out, in0=in0, in1=in1, op=op), reads, writes)

    def ts(self, eng, out, in0, s1, s2, op0, op1=None, reads=(), writes=(), accum=None):
        kw = {}
        if op1 is not None:
            kw["op1"] = op1
        if accum is not None:
            kw["accum_out"] = accum
        return self.op(eng, lambda e: e.tensor_scalar(out=out, in0=in0, scalar1=s1, scalar2=s2, op0=op0, **kw),
                       reads, writes)

    def stt(self, out, in0, scalar, in1, op0, op1, reads=(), writes=()):
        return self.op("vector", lambda e: e.scalar_tensor_tensor(out=out, in0=in0, scalar=scalar, in1=in1,
                                                                  op0=op0, op1=op1), reads, writes)

    def copy(self, eng, out, in_, reads=(), writes=()):
        if eng == "scalar":
            return self.op(eng, lambda e: e.copy(out=out, in_=in_), reads, writes)
        return self.op(eng, lambda e: e.tensor_copy(out=out, in_=in_), reads, writes)

    def memset(self, eng, ap, val, writes=()):
        return self.op(eng, lambda e: e.memset(ap, val), (), writes)

    def recip(self, out, in_, reads=(), writes=()):
        return self.op("vector", lambda e: e.reciprocal(out=out, in_=in_), reads, writes)

    def reduce(self, out, in_, op, reads=(), writes=()):
        return self.op("vector", lambda e: e.tensor_reduce(out=out, in_=in_, axis=AX.X, op=op), reads, writes)

    def emit(self, final_waits=()):
        nc = self.nc
        es = self.es
        esem = {(e, ph): es.enter_context(nc.semaphore("s_%s_%d" % (e, ph)))
                for e in self.ENGS for ph in range(self.phase + 1)}
        dsem = {k: es.enter_context(nc.semaphore("d_" + str(k))) for k in self.dma_counts}
        for e in self.ENGS:
            c = {}
            for o in self.ops[e]:
                if o.is_dma:
                    continue
                if o.needs_inc:
                    c[o.phase] = c.get(o.phase, 0) + 1
                    o.semval = c[o.phase]
        block = es.enter_context(nc.Block())

        def run(ename):
            def body(eng):
                known = {}
                for o in self.ops[ename]:
                    for d in o.deps:
                        if d.is_dma:
                            key, val, sem = ("d", d.dsem), d.dval, dsem[d.dsem]
                        else:
                            key, val, sem = ("e", d.eng, d.phase), d.semval, esem[(d.eng, d.phase)]
                        if known.get(key, 0) >= val:
                            continue
                        known[key] = val
                        eng.wait_ge(sem, val)
                    ins = o.fn(eng)
                    if o.is_dma:
                        ins.then_inc(dsem[o.dsem], 16)
                    elif o.needs_inc:
                        ins.then_inc(esem[(ename, o.phase)], 1)
                if ename == "sync":
                    for k in final_waits:
                        eng.wait_ge(dsem[k], 16 * self.dma_counts[k])
            return body

        block.tensor(run("tensor"))
        block.vector(run("vector"))
        block.scalar(run("scalar"))
        block.gpsimd(run("gpsimd"))
        block.sync(run("sync"))
        es.close()


class Arena:
    def __init__(self, P, nf32):
        self.t = P.sbuf("arena", [128, nf32], F32)
        self.n = nf32
        self.off = 0

    def reset(self, to=0):
        self.off = to

    def alloc(self, shape, dt, at=None):
        per = 1
        for s in shape[1:]:
            per *= s
        nf = (per + 1) // 2 if dt == BF16 else per
        off = self.off if at is None else at
        assert off + nf <= self.n, ("arena overflow", off, nf, self.n)
        v = self.t[0:shape[0], off:off + nf]
        if dt == BF16:
            v = v.bitcast(BF16)
            if per % 2:
                v = v[:, 0:per]
        if len(shape) == 3:
            v = v.rearrange("p (a b) -> p a b", b=shape[2])
        elif len(shape) == 4:
            v = v.rearrange("p (a b c) -> p a b c", b=shape[2], c=shape[3])
        if at is None:
            self.off = off + nf
        return v


def _t5_bucket(rel):
    half, max_exact = 16, 8
    ret = np.where(rel > 0, half, 0)
    n = np.abs(rel)
    nf = np.maximum(n, 1).astype(np.float32)
    large = max_exact + (np.log(nf / np.float32(max_exact)) / np.float32(math.log(128 / max_exact))
                         * np.float32(half - max_exact)).astype(np.int32)
    large = np.minimum(large, half - 1)
    return ret + np.where(n < max_exact, n, large)


def _host_layout(inp):
    w_in = np.asarray(inp["w_in"], np.float32)[0]
    cols = []
    for j in range(4):
        cols.append(w_in[:, O_QA + j * 128:O_QA + (j + 1) * 128])
    for j in range(4):
        cols.append(w_in[:, O_KA + j * 128:O_KA + (j + 1) * 128])
    qi = w_in[:, O_QI:O_QI + 256]
    cols.append(qi[:, 0:96])
    cols.append(qi[:, 96:192])
    cols.append(np.concatenate([qi[:, 192:256], np.zeros((D, 32), np.float32)], axis=1))
    ki = w_in[:, O_KI:O_KI + 32]
    cols.append(np.concatenate([ki, ki, ki], axis=1))
    for j in range(2):
        cols.append(w_in[:, O_QB + j * 128:O_QB + (j + 1) * 128])
    for j in range(2):
        cols.append(w_in[:, O_KB + j * 128:O_KB + (j + 1) * 128])
    cols.append(w_in[:, O_GD:O_GD + 16])
    w_fm = np.ascontiguousarray(np.concatenate(cols, axis=1))
    assert w_fm.shape[1] == NFM
    w_tm = np.ascontiguousarray(np.concatenate([
        w_in[:, O_VA:O_VA + 512], w_in[:, O_KB:O_KB + 256], w_in[:, O_WI:O_WI + 8],
        w_in[:, O_VB:O_VB + 512], w_in[:, O_OG:O_OG + 512]], axis=1))
    assert w_tm.shape[1] == NTM

    c = np.zeros((128, NCONST), np.float32)
    ii = np.arange(128)
    c[:, C_ID:C_ID + 128] = np.eye(128, dtype=np.float32)
    u01 = (ii[:, None] <= ii[None, :]).astype(np.float32)
    c[:, C_U01:C_U01 + 128] = u01
    c[:, C_UNEG:C_UNEG + 128] = -u01 / 16.0
    c[:, C_LNEG:C_LNEG + 128] = -(ii[:, None] > ii[None, :]).astype(np.float32) / 16.0
    dm = np.zeros((128, 128), np.float32)
    dm[:64, 64:] = NEG
    c[:, C_DMASK:C_DMASK + 128] = dm
    c[:, C_GMIX:C_GMIX + 8] = np.asarray(inp["norm_mix"], np.float32)[0].reshape(8, 128).T
    c[:, C_GFFN:C_GFFN + 8] = np.asarray(inp["norm_ffn"], np.float32)[0].reshape(8, 128).T
    rb = np.asarray(inp["rel_bias"], np.float32)
    c[:, C_CFAR:C_CFAR + 8] = rb[15][None, :]
    c[:, C_GLAN:C_GLAN + 512] = np.asarray(inp["gla_norm"], np.float32)[0][None, :]
    c[:, C_GFIN:C_GFIN + 1024] = np.asarray(inp["norm_final"], np.float32)[None, :]
    bt = np.zeros((128, 2, 8, 128), np.float32)
    for which in range(2):
        rel = (ii[:, None] - 128 * which) - ii[None, :]
        bk = _t5_bucket(rel)
        bt[:, which] = rb[bk].transpose(0, 2, 1)
    c[:, C_BIAS:C_BIAS + 2048] = bt.reshape(128, 2048)
    wgu = np.concatenate([np.asarray(inp["w_gate_up"], np.float32)[0],
                          np.asarray(inp["b_gate"], np.float32)[0][None, :]], axis=0)
    return {
        "w_fm": w_fm, "w_tm": w_tm,
        "w_out": np.ascontiguousarray(np.asarray(inp["w_out"], np.float32)[0]),
        "w_g": np.ascontiguousarray(np.asarray(inp["w_ffn_gate"], np.float32)[0]),
        "w_u": np.ascontiguousarray(np.asarray(inp["w_ffn_up"], np.float32)[0]),
        "w_d": np.ascontiguousarray(np.asarray(inp["w_ffn_down"], np.float32)[0]),
        "consts": c, "wgu": np.ascontiguousarray(wgu),
    }


def build_nc(debug=False, nseq=2):
    nc = bass.Bass("TRN2", target_bir_lowering=False)
    x_d = nc.dram_tensor("x", [2, S, D], F32, kind="ExternalInput").ap()
    wfm_d = nc.dram_tensor("w_fm", [D, NFM], F32, kind="ExternalInput").ap()
    wtm_d = nc.dram_tensor("w_tm", [D, NTM], F32, kind="ExternalInput").ap()
    wout_d = nc.dram_tensor("w_out", [D, D], F32, kind="ExternalInput").ap()
    wg_d = nc.dram_tensor("w_g", [D, DFF], F32, kind="ExternalInput").ap()
    wu_d = nc.dram_tensor("w_u", [D, DFF], F32, kind="ExternalInput").ap()
    wd_d = nc.dram_tensor("w_d", [DFF, D], F32, kind="ExternalInput").ap()
    c_d = nc.dram_tensor("consts", [128, NCONST], F32, kind="ExternalInput").ap()
    wgu_d = nc.dram_tensor("wgu", [17, 256], F32, kind="ExternalInput").ap()
    y_d = nc.dram_tensor("y", [2, S, D], F32, kind="ExternalOutput").ap()
    x1_d = nc.dram_tensor("x1_scratch", [S, D], F32, kind="Internal").ap()
    dbg = {}
    if debug:
        for nm, shp in (("d_qaT", [128, 4, S]), ("d_oa", [S, 512]), ("d_ob", [S, 512]), ("d_sc", [128, S]),
                        ("d_x1", [S, D])):
            dbg[nm] = nc.dram_tensor(nm, shp, F32 if nm in ("d_sc", "d_x1") else BF16, kind="ExternalOutput").ap()

    P = Prog(nc)
    cmin_t = P.sbuf("cmin", [128, C_MIN], F32)
    cmin = Buf(cmin_t[:], "cmin")
    identb_t = P.sbuf("identb", [128, 128], BF16)
    identb = Buf(identb_t[:], "identb")
    wgu_t = P.sbuf("wgu", [17, 256], F32)
    wgu = Buf(wgu_t[:], "wgu")
    obT_t = P.sbuf("obT", [128, 4, S], BF16)
    stat_t = P.sbuf("stat", [128, 64], F32)
    ARENA_F = 47800
    ar = Arena(P, ARENA_F)
    OA_OFF = ARENA_F - 4096

    pf_t = [P.psum("pf%d" % i, [128, 512], F32) for i in range(6)]
    pb_t = [P.psum("pb%d" % i, [128, 1024], BF16) for i in range(2)]
    pf = [Buf(t[:], "pf") for t in pf_t]
    pb = [Buf(t[:], "pb") for t in pb_t]

    ident_f = cmin_t[:, C_ID:C_ID + 128]
    u01 = cmin_t[:, C_U01:C_U01 + 128]
    uneg = cmin_t[:, C_UNEG:C_UNEG + 128]
    lneg = cmin_t[:, C_LNEG:C_LNEG + 128]
    dmask = cmin_t[:, C_DMASK:C_DMASK + 128]
    gmixT = cmin_t[:, C_GMIX:C_GMIX + 8]
    gffnT = cmin_t[:, C_GFFN:C_GFFN + 8]
    cfar = cmin_t[:, C_CFAR:C_CFAR + 8]

    P.dma("sync", cmin.ap, c_d[:, 0:C_MIN], "cmin", writes=[cmin])
    P.dma("sync", wgu.ap, wgu_d, "wgu", writes=[wgu])
    P.copy("vector", identb.ap, ident_f, reads=[cmin], writes=[identb])

    stat_bufs = [Buf(stat_t[:, 4 * i:4 * i + 4], "stat%d" % i) for i in range(8)]
    stat_ctr = [0]

    def next_stat():
        b = stat_bufs[stat_ctr[0] % 8]
        stat_ctr[0] += 1
        return b

    evac_ctr = [0]

    def evac(out, in_, reads, writes=()):
        evac_ctr[0] += 1
        eng = "scalar" if evac_ctr[0] % 2 else "vector"
        return P.copy(eng, out, in_, reads=reads, writes=writes)

    def rms_scale(src_buf, src_ap, xs_buf):
        st = next_stat()
        P.act(xs_buf.ap, src_ap, AF.Square, reads=[src_buf], writes=[xs_buf, st], accum=st.ap[:, 0:1])
        P.act(st.ap[:, 1:2], st.ap[:, 0:1], AF.Ln, reads=[st], writes=[st], scale=1.0 / D, bias=EPS)
        P.act(st.ap[:, 2:3], st.ap[:, 1:2], AF.Exp, reads=[st], writes=[st], scale=-0.5)
        P.ts("vector", xs_buf.ap, src_ap, st.ap[:, 2:3], None, ALU.mult, reads=[src_buf, st], writes=[xs_buf])

    def xs_to_T(xs_buf, dstT_ap, gT, pbuf):
        pv = pbuf.ap.rearrange("p (a b) -> p a b", b=128)
        for kc in range(8):
            P.transpose(pv[:, kc, :], xs_buf.ap[:, kc * 128:(kc + 1) * 128], identb.ap,
                        reads=[xs_buf, identb], writes=[pbuf])
        P.tt("vector", dstT_ap, pv, gT.unsqueeze(2).to_broadcast([128, 8, 128]), ALU.mult,
             reads=[pbuf, cmin], writes=[])

    for seq in range(nseq):
        P.barrier()
        ar.reset(0)
        qaT = ar.alloc([128, 4, S], BF16)
        kaT = ar.alloc([128, 4, S], BF16)
        va = ar.alloc([128, NT, 8, 66], BF16)
        qiT = ar.alloc([96, 3, S], BF16)
        kiT = ar.alloc([96, S], BF16)
        wi = ar.alloc([128, NT, 8], F32)
        AB_END = ar.off
        wfm = ar.alloc([128, 8, NFM], BF16)
        wtm = ar.alloc([128, 8, NTM], BF16)
        wfm_b1, wfm_b2, wtm_b = Buf(wfm, "wfm1"), Buf(wfm, "wfm2"), Buf(wtm, "wtm")
        glan = ar.alloc([128, 512], F32)
        glan_b = Buf(glan, "glan")
        hTs = [ar.alloc([128, 8, 512], BF16) for _ in range(2)]
        hT_bs = [Buf(hTs[i], "hT%d" % i) for i in range(2)]
        xin = [Buf(ar.alloc([128, D], F32), "xin%d" % i) for i in range(2)]
        xs = [Buf(ar.alloc([128, D], BF16), "xs%d" % i) for i in range(2)]
        qbT = Buf(ar.alloc([128, 2, 512], F32), "qbT")
        kbT = Buf(ar.alloc([128, 2, 512], F32), "kbT")
        gdT = Buf(ar.alloc([17, 512], F32), "gdT")
        kbtm = [Buf(ar.alloc([128, 256], F32), "kbtm") for _ in range(2)]
        vbtm = [Buf(ar.alloc([128, 512], BF16), "vbtm") for _ in range(2)]
        sg = [Buf(ar.alloc([128, 512], F32), "sg") for _ in range(2)]
        etmp = Buf(ar.alloc([128, 256], F32), "etmp")
        lsb = Buf(ar.alloc([128, 256], F32), "lsb")
        e1Ts = [Buf(ar.alloc([128, 2, 128], F32), "e1T") for _ in range(2)]
        e2T = Buf(ar.alloc([128, 2, 128], F32), "e2T")
        e3 = Buf(ar.alloc([128, 256], F32), "e3")
        qinTs = [Buf(ar.alloc([128, 2, 128], BF16), "qinT") for _ in range(2)]
        kinTs = [Buf(ar.alloc([128, 2, 128], BF16), "kinT") for _ in range(2)]
        kdecs = [Buf(ar.alloc([128, 256], BF16), "kdec") for _ in range(2)]
        attS = [Buf(ar.alloc([128, 128], BF16), "attS") for _ in range(2)]
        on = Buf(ar.alloc([128, 512], F32), "on")
        obss = [Buf(ar.alloc([128, 512], BF16), "obs%d" % i) for i in range(2)]
        stf = Buf(ar.alloc([128, 2, 128], F32), "stf")
        stb = Buf(ar.alloc([128, 2, 128], BF16), "stb")

        wfm_src = wfm_d.rearrange("(kc p) n -> p kc n", p=128)
        P.dma("gpsimd", wfm[:, :, 0:1024], wfm_src[:, :, 0:1024], "wfm1", writes=[wfm_b1])
        P.dma("gpsimd", wfm[:, :, 1024:NFM], wfm_src[:, :, 1024:NFM], "wfm2", writes=[wfm_b2])
        P.dma("gpsimd", wtm, wtm_d.rearrange("(kc p) n -> p kc n", p=128), "wtm", writes=[wtm_b])
        P.dma("sync", glan, c_d[:, C_GLAN:C_GLAN + 512], "glan", writes=[glan_b])
        P.memset("gpsimd", va[:, :, :, 64:66], 1.0)
        P.memset("gpsimd", gdT.ap, 1.0, writes=[gdT])
        P.memset("vector", stf.ap, 0.0, writes=[stf])
        P.memset("vector", stb.ap, 0.0, writes=[stb])

        fm_off = {}
        o = 0
        for nm, m in FM_TILES:
            fm_off[nm] = (o, m)
            o += m
        mm_ctr = [0]
        zps = Buf(pf_t[2][:, 0:256], "zps")
        rps = Buf(pf_t[2][:, 256:512], "rps")
        cps = Buf(pf_t[3][:, 0:256], "cps")
        ops_ = pf[4]
        aps2 = [Buf(pf_t[5][:, 0:128], "aps0"), Buf(pf_t[5][:, 128:256], "aps1")]
        kvp = Buf(pf_t[5][:, 256:512], "kvp")

        def prep(tb):
            hT, hT_b = hTs[tb % 2], hT_bs[tb % 2]
            for i in range(4):
                t = tb * 4 + i
                xb = xin[t % 2]
                P.dma("sync", xb.ap, x_d[seq, t * 128:(t + 1) * 128, :], "xin%d" % (t % 2), writes=[xb])
                st = next_stat()
                xsb = xs[t % 2]
                P.act(xsb.ap, xb.ap, AF.Square, reads=[xb], writes=[xsb, st], accum=st.ap[:, 0:1])
                P.act(st.ap[:, 1:2], st.ap[:, 0:1], AF.Ln, reads=[st], writes=[st], scale=1.0 / D, bias=EPS)
                P.act(st.ap[:, 2:3], st.ap[:, 1:2], AF.Exp, reads=[st], writes=[st], scale=-0.5)
                P.ts("vector", xsb.ap, xb.ap, st.ap[:, 2:3], None, ALU.mult, reads=[xb, st], writes=[xsb])
                pbuf = pb[t % 2]
                pv = pbuf.ap.rearrange("p (a b) -> p a b", b=128)
                for kc in range(8):
                    P.transpose(pv[:, kc, :], xsb.ap[:, kc * 128:(kc + 1) * 128], identb.ap,
                                reads=[xsb, identb], writes=[pbuf])
                P.tt("vector", hT[:, :, i * 128:(i + 1) * 128], pv,
                     gmixT.unsqueeze(2).to_broadcast([128, 8, 128]), ALU.mult,
                     reads=[pbuf, cmin], writes=[hT_b])

        def fm_block(tb):
            hT, hT_b = hTs[tb % 2], hT_bs[tb % 2]
            cs = slice(tb * 512, (tb + 1) * 512)
            for nm, m in FM_TILES:
                off, _ = fm_off[nm]
                ps = pf[mm_ctr[0] % 2]
                mm_ctr[0] += 1
                for kc in range(8):
                    P.matmul(ps.ap[0:m, :], wfm[:, kc, off:off + m], hT[:, kc, :], kc == 0, kc == 7,
                             reads=[wfm_b1 if off < 1024 else wfm_b2, hT_b], writes=[ps])
                if nm.startswith("qa"):
                    evac(qaT[:, int(nm[2]), cs], ps.ap, [ps])
                elif nm.startswith("ka"):
                    evac(kaT[:, int(nm[2]), cs], ps.ap, [ps])
                elif nm.startswith("qi"):
                    evac(qiT[:, int(nm[2]), cs], ps.ap[0:96, :], [ps])
                elif nm == "ki":
                    evac(kiT[:, cs], ps.ap[0:96, :], [ps])
                elif nm.startswith("qb"):
                    evac(qbT.ap[:, int(nm[2]), :], ps.ap, [ps], [qbT])
                elif nm.startswith("kb"):
                    evac(kbT.ap[:, int(nm[2]), :], ps.ap, [ps], [kbT])
                else:
                    evac(gdT.ap[0:16, :], ps.ap[0:16, :], [ps], [gdT])

        def gla_s1(t):
            tb, i = t // 4, t % 4
            hT, hT_b = hTs[tb % 2], hT_bs[tb % 2]
            tcs = slice(i * 128, (i + 1) * 128)
            kb_s, vb_s, sg_s = kbtm[t % 2], vbtm[t % 2], sg[t % 2]
            e1T, qinT, kinT, kdec = e1Ts[t % 2], qinTs[t % 2], kinTs[t % 2], kdecs[t % 2]
            P.matmul(zps.ap, gdT.ap[0:17, tcs], wgu.ap, True, True, reads=[gdT, wgu], writes=[zps])
            P.act(etmp.ap, zps.ap, AF.Exp, reads=[zps], writes=[etmp], scale=-1.0)
            P.act(lsb.ap, etmp.ap, AF.Ln, reads=[etmp], writes=[lsb], bias=1.0)
            for g, (goff, gn) in enumerate(((0, 512), (512, 264), (776, 512), (1288, 512))):
                ps = pf[mm_ctr[0] % 2]
                mm_ctr[0] += 1
                for kc in range(8):
                    P.matmul(ps.ap[:, 0:gn], hT[:, kc, tcs], wtm[:, kc, goff:goff + gn], kc == 0, kc == 7,
                             reads=[wtm_b, hT_b], writes=[ps])
                if g == 0:
                    evac(va[:, t, :, 0:64], ps.ap.rearrange("p (h d) -> p h d", d=64), [ps])
                elif g == 1:
                    P.copy("vector", kb_s.ap, ps.ap[:, 0:256], reads=[ps], writes=[kb_s])
                    P.copy("vector", wi[:, t, :], ps.ap[:, 256:264], reads=[ps])
                elif g == 2:
                    P.copy("scalar", vb_s.ap, ps.ap, reads=[ps], writes=[vb_s])
                else:
                    P.act(sg_s.ap, ps.ap, AF.Silu, reads=[ps], writes=[sg_s])
            cv = cps.ap.rearrange("p (a b) -> p a b", b=128)
            for pr in range(2):
                P.matmul(cv[:, pr, :], lsb.ap[:, pr * 128:(pr + 1) * 128], uneg, True, True,
                         reads=[lsb, cmin], writes=[cps])
            P.matmul(rps.ap, lneg, lsb.ap, True, True, reads=[lsb, cmin], writes=[rps])
            P.act(e1T.ap, cv, AF.Exp, reads=[cps], writes=[e1T])
            P.act(e2T.ap, cv, AF.Exp, reads=[cps], writes=[e2T], scale=-1.0)
            P.act(e3.ap, rps.ap, AF.Exp, reads=[rps], writes=[e3])
            P.stt(qinT.ap, qbT.ap[:, :, tcs], 0.125, e1T.ap, ALU.mult, ALU.mult,
                  reads=[qbT, e1T], writes=[qinT])
            P.tt("vector", kinT.ap, kbT.ap[:, :, tcs], e2T.ap, ALU.mult, reads=[kbT, e2T], writes=[kinT])
            P.tt("gpsimd", kdec.ap, kb_s.ap, e3.ap, ALU.mult, reads=[kb_s, e3], writes=[kdec])

        def gla_s2(t):
            vb_s, sg_s = vbtm[t % 2], sg[t % 2]
            e1T, qinT, kinT, kdec = e1Ts[t % 2], qinTs[t % 2], kinTs[t % 2], kdecs[t % 2]
            for pr in range(2):
                for hh in range(2):
                    h = 2 * pr + hh
                    hp = slice(hh * 64, hh * 64 + 64)
                    aps = aps2[hh]
                    P.matmul(aps.ap, kinT.ap[hp, pr, :], qinT.ap[hp, pr, :], True, True,
                             reads=[kinT, qinT], writes=[aps])
                    asb = attS[hh]
                    P.tt("vector", asb.ap, aps.ap, u01, ALU.mult, reads=[aps, cmin], writes=[asb])
                    P.matmul(ops_.ap[:, h * 128:(h + 1) * 128], asb.ap, vb_s.ap[:, h * 128:(h + 1) * 128],
                             True, False, reads=[asb, vb_s], writes=[ops_])
                    P.matmul(ops_.ap[:, h * 128:(h + 1) * 128], qinT.ap[hp, pr, :], stb.ap[hp, pr, :],
                             False, True, reads=[qinT, stb], writes=[ops_])
                P.matmul(kvp.ap, kdec.ap[:, pr * 128:(pr + 1) * 128],
                         vb_s.ap[:, pr * 256:(pr + 1) * 256], True, True, reads=[kdec, vb_s], writes=[kvp])
                for hh in range(2):
                    hp = slice(hh * 64, hh * 64 + 64)
                    P.stt(stf.ap[hp, pr, :], stf.ap[hp, pr, :], e1T.ap[hp, pr, 127:128],
                          kvp.ap[hp, hh * 128:(hh + 1) * 128], ALU.mult, ALU.add,
                          reads=[stf, e1T, kvp], writes=[stf])
            P.copy("gpsimd", stb.ap, stf.ap, reads=[stf], writes=[stb])
            st = next_stat()
            P.act(on.ap, ops_.ap, AF.Square, reads=[ops_], writes=[on])
            P.reduce(st.ap[:, 0:4], on.ap.rearrange("p (h v) -> p h v", v=128), ALU.add, reads=[on], writes=[st])
            st2 = next_stat()
            P.act(st2.ap[:, 0:4], st.ap[:, 0:4], AF.Ln, reads=[st], writes=[st2], scale=1.0 / 128, bias=EPS)
            st3 = next_stat()
            P.act(st3.ap[:, 0:4], st2.ap[:, 0:4], AF.Exp, reads=[st2], writes=[st3], scale=-0.5)
            P.tt("vector", on.ap.rearrange("p (h v) -> p h v", v=128),
                 ops_.ap.rearrange("p (h v) -> p h v", v=128),
                 st3.ap[:, 0:4].unsqueeze(2).to_broadcast([128, 4, 128]), ALU.mult,
                 reads=[ops_, st3], writes=[on])
            P.tt("gpsimd", on.ap, on.ap, glan, ALU.mult, reads=[on, glan_b], writes=[on])
            obs = obss[t % 2]
            P.tt("vector", obs.ap, on.ap, sg_s.ap, ALU.mult, reads=[on, sg_s], writes=[obs])
            if debug and seq == 0:
                P.dma("sync", dbg["d_ob"][t * 128:(t + 1) * 128, :], obs.ap, "dbg_ob", reads=[obs])

        def gla_s3(t):
            obs = obss[t % 2]
            pbuf = pb[t % 2]
            pv = pbuf.ap.rearrange("p (a b) -> p a b", b=128)
            for j in range(4):
                P.transpose(pv[:, j, :], obs.ap[:, j * 128:(j + 1) * 128], identb.ap,
                            reads=[obs, identb], writes=[pbuf])
            P.copy("scalar", obT_t[:, :, t * 128:(t + 1) * 128], pv[:, 0:4, :], reads=[pbuf])

        prep(0)
        for t in range(NT):
            if t % 4 == 0:
                fm_block(t // 4)
                if t // 4 + 1 < 4:
                    prep(t // 4 + 1)
            gla_s1(t)
            if t >= 1:
                gla_s2(t - 1)
            if t >= 2:
                gla_s3(t - 2)
        gla_s2(NT - 1)
        gla_s3(NT - 2)
        gla_s3(NT - 1)
        if debug and seq == 0:
            P.barrier()
            P.dma("sync", dbg["d_qaT"], qaT, "dbg_qaT")

        P.barrier()
        ar.reset(AB_END)
        bias8 = ar.alloc([128, 2, 8, 128], BF16)
        bias8_b = Buf(bias8, "bias8")
        biasT = ar.alloc([128, 2, 8, 128], F32)
        biasT_b = Buf(biasT, "biasT")
        P.dma("sync", biasT.rearrange("p a b c -> p (a b c)"), c_d[:, C_BIAS:C_BIAS + 2048], "biasT", writes=[biasT_b])
        P.ts("vector", bias8, biasT, 8.0, None, ALU.mult, reads=[biasT_b], writes=[bias8_b])
        sc = [Buf(ar.alloc([128, S], F32), "sc%d" % i) for i in range(4)]
        junkB = ar.alloc([128, S], BF16)
        msk = [Buf(ar.alloc([128, S], BF16), "msk%d" % i) for i in range(4)]
        mskT = [Buf(ar.alloc([128, NT, 128], BF16), "mskT%d" % i) for i in range(4)]
        rsb = [Buf(ar.alloc([128, 512], F32), "rsb%d" % i) for i in range(3)]
        NPMAX = 5
        ebuf = [Buf(ar.alloc([128, 4, 128], BF16), "e%d" % i) for i in range(NPMAX)]
        pbuf_ = [Buf(ar.alloc([128, 4, 128], BF16), "p%d" % i) for i in range(NPMAX)]
        oasb = Buf(ar.alloc([128, 512], BF16), "oasb")
        bis_t = [ar.alloc([128, 8], F32) for i in range(2)]
        midb = [Buf(b_[:, 0:2], "mid") for b_ in bis_t]
        cntb = [[Buf(b_[:, 2:3], "cnt0"), Buf(b_[:, 3:4], "cnt1")] for b_ in bis_t]
        tbb = [Buf(b_[:, 4:6], "tb") for b_ in bis_t]
        thrb = [Buf(b_[:, 6:8], "thr") for b_ in bis_t]
        nrm = Buf(ar.alloc([128, 16], F32), "nrm")
        actb_t = ar.alloc([128, 8], F32)
        nmid = Buf(actb_t[:, 0:1], "nmid")
        ssum = Buf(actb_t[:, 1:2], "ssum")
        sgnb = Buf(actb_t[:, 2:3], "sgnb")
        thrA = Buf(actb_t[:, 3:4], "thrA")
        junkB2 = ar.alloc([128, S], BF16)
        assert ar.off <= OA_OFF, ("phase B arena", ar.off, OA_OFF)
        oaT = ar.alloc([128, 4, S], BF16, at=OA_OFF)
        r_ctr = [0]
        lgb = [pf[2], pf[3], pb[1], pf[0], pf[1]]
        lgv = [pf_t[2][:], pf_t[3][:], pb_t[1][:].bitcast(F32), pf_t[0][:], pf_t[1][:]]
        pbm = Buf(pb_t[0][:, 0:512], "pbm")
        pbo = Buf(pb_t[0][:, 512:1024], "pbo")
        ops2 = [pf[4], pf[5]]
        ov = [b_.ap[:, 0:264].rearrange("p (h d) -> p h d", d=66) for b_ in ops2]

        def idx_group(g4):
            qts = [4 * g4 + k for k in range(4)]
            for k, qt in enumerate(qts):
                sk = (qt + 1) * 128
                scb = sc[k]
                qcs = slice(qt * 128, (qt + 1) * 128)
                for c0 in range(0, sk, 512):
                    w = min(512, sk - c0)
                    for h in range(8):
                        hp = slice((h % 3) * 32, (h % 3) * 32 + 32)
                        ps = pf[h % 2]
                        P.matmul(ps.ap[:, 0:w], qiT[hp, h // 3, qcs], kiT[hp, c0:c0 + w], True, True, writes=[ps])
                        rb_ = rsb[r_ctr[0] % 3]
                        r_ctr[0] += 1
                        P.act(rb_.ap[:, 0:w], ps.ap[:, 0:w], AF.Relu, reads=[ps], writes=[rb_])
                        if h == 0:
                            P.ts("vector", scb.ap[:, c0:c0 + w], rb_.ap[:, 0:w], wi[:, qt, 0:1], None, ALU.mult,
                                 reads=[rb_], writes=[scb])
                        else:
                            P.stt(scb.ap[:, c0:c0 + w], rb_.ap[:, 0:w], wi[:, qt, h:h + 1], scb.ap[:, c0:c0 + w],
                                  ALU.mult, ALU.add, reads=[rb_, scb], writes=[scb])
                P.tt("vector", scb.ap[:, sk - 128:sk], scb.ap[:, sk - 128:sk], dmask, ALU.add,
                     reads=[scb, cmin], writes=[scb])
                if debug and seq == 0 and qt == 5:
                    P.dma("sync", dbg["d_sc"], scb.ap, "dbg_sc", reads=[scb])

        def bis_group(g4):
            qts = [4 * g4 + k for k in range(4)]
            if g4 == 0:
                pairs, act_k = [(), (2, 3)], None
            else:
                pairs, act_k = [(0, 1), (2, 3)], None
            act_pairs = [pi for pi in range(2) if len(pairs[pi])]
            for pi in act_pairs:
                P.memset("vector", midb[pi].ap, 0.0, writes=[midb[pi]])
            if act_k is not None:
                P.memset("gpsimd", nmid.ap, 0.0, writes=[nmid])
                sk_a = (qts[act_k] + 1) * 128
            for it in range(1, N_BIS + 1):
                step = R_BIS / (2.0 ** it)
                for pi in act_pairs:
                    for a, k in enumerate(pairs[pi]):
                        sk = (qts[k] + 1) * 128
                        P.ts("vector", junkB[:, 0:sk], sc[k].ap[:, 0:sk], midb[pi].ap[:, a:a + 1], None,
                             ALU.is_ge, ALU.add, reads=[sc[k], midb[pi]], writes=[cntb[pi][a]],
                             accum=cntb[pi][a].ap)
                if act_k is not None:
                    P.act(junkB2[:, 0:sk_a], sc[act_k].ap[:, 0:sk_a], AF.Sign, reads=[sc[act_k], nmid],
                          writes=[ssum], bias=nmid.ap, accum=ssum.ap)
                    P.act(sgnb.ap, ssum.ap, AF.Sign, reads=[ssum], writes=[sgnb], bias=float(sk_a) - 511.5)
                    P.act(nmid.ap, sgnb.ap, AF.Identity, reads=[sgnb, nmid], writes=[nmid], scale=-step, bias=nmid.ap)
                for pi in act_pairs:
                    n_ = len(pairs[pi])
                    P.ts("vector", tbb[pi].ap[:, 0:n_], bis_t[pi][:, 2:2 + n_], 255.5, 2.0 * step, ALU.is_ge, ALU.mult,
                         reads=[cntb[pi][a] for a in range(n_)], writes=[tbb[pi]])
                for pi in act_pairs:
                    n_ = len(pairs[pi])
                    P.stt(midb[pi].ap[:, 0:n_], tbb[pi].ap[:, 0:n_], -step, midb[pi].ap[:, 0:n_], ALU.add, ALU.add,
                          reads=[tbb[pi], midb[pi]], writes=[midb[pi]])
                yield
            last_step = R_BIS / (2.0 ** N_BIS)
            for pi in act_pairs:
                n_ = len(pairs[pi])
                P.ts("vector", thrb[pi].ap[:, 0:n_], midb[pi].ap[:, 0:n_], -last_step, None, ALU.add,
                     reads=[midb[pi]], writes=[thrb[pi]])
            if act_k is not None:
                P.act(thrA.ap, nmid.ap, AF.Identity, reads=[nmid], writes=[thrA], scale=-1.0, bias=-last_step)
            thr_of = {}
            for pi in act_pairs:
                for a, k in enumerate(pairs[pi]):
                    thr_of[k] = (thrb[pi], thrb[pi].ap[:, a:a + 1])
            if act_k is not None:
                thr_of[act_k] = (thrA, thrA.ap)
            for k, qt in enumerate(qts):
                sk = (qt + 1) * 128
                if qt >= 2:
                    P.ts("vector", msk[k].ap[:, 0:sk], sc[k].ap[:, 0:sk], thr_of[k][1], None, ALU.is_ge,
                         reads=[sc[k], thr_of[k][0]], writes=[msk[k]])
                else:
                    P.ts("vector", msk[k].ap[:, 0:sk], sc[k].ap[:, 0:sk], -1.0e29, None, ALU.is_ge,
                         reads=[sc[k]], writes=[msk[k]])
            for k, qt in enumerate(qts):
                nkb = qt + 1
                mT = mskT[k]
                pv = pbm.ap.rearrange("p (a b) -> p a b", b=128)
                for c0 in range(0, nkb, 4):
                    n = min(4, nkb - c0)
                    for kk in range(n):
                        P.transpose(pv[:, kk, :], msk[k].ap[:, (c0 + kk) * 128:(c0 + kk + 1) * 128], identb.ap,
                                    reads=[msk[k], identb], writes=[pbm])
                    P.copy("scalar", mT.ap[:, c0:c0 + n, :], pv[:, 0:n, :], reads=[pbm], writes=[mT])

        def attn_group(g4):
            NPIPE = 5 if g4 == 3 else 3
            qts = [4 * g4 + k for k in range(4)]
            items = []
            for k, qt in enumerate(qts):
                near = [kb for kb in (qt - 1, qt) if kb >= 0]
                far = list(range(0, max(qt - 1, 0)))
                groups = [("far", far[c0:c0 + 4]) for c0 in range(0, len(far), 4)] + [("near", near)]
                for h in range(8):
                    for gi, (kind, kbs) in enumerate(groups):
                        items.append((k, qt, h, kind, kbs, gi == 0, gi == len(groups) - 1))

            def stage1(item, i):
                k, qt, h, kind, kbs, _, _ = item
                qcs = slice(qt * 128, (qt + 1) * 128)
                j, hp = h // 2, slice((h % 2) * 64, (h % 2) * 64 + 64)
                n = len(kbs)
                lps = lgb[i % NPIPE]
                lv = lgv[i % NPIPE].rearrange("p (a b) -> p a b", b=128)
                for kk, kb in enumerate(kbs):
                    if kind == "far":
                        P.matmul(lv[:, kk, :], kaT[hp, j, kb * 128:(kb + 1) * 128], qaT[hp, j, qcs], True, True,
                                 writes=[lps])
                    else:
                        which = 0 if kb == qt else 1
                        P.matmul(lv[:, kk, :], kaT[hp, j, kb * 128:(kb + 1) * 128], qaT[hp, j, qcs], True, False,
                                 writes=[lps])
                        P.matmul(lv[:, kk, :], identb.ap, bias8[:, which, h, :], False, True,
                                 reads=[identb, bias8_b], writes=[lps])
                eb = ebuf[i % NPIPE]
                pbf = pbuf_[i % NPIPE]
                if kind == "far":
                    P.act(eb.ap[:, 0:n, :], lv[:, 0:n, :], AF.Exp, reads=[lps, cmin], writes=[eb],
                          scale=0.125, bias=cfar[:, h:h + 1])
                else:
                    P.act(eb.ap[:, 0:n, :], lv[:, 0:n, :], AF.Exp, reads=[lps], writes=[eb], scale=0.125)
                meng = "vector" if (g4 == 3 and i % 2 == 1) else "gpsimd"
                P.tt(meng, pbf.ap[:, 0:n, :], eb.ap[:, 0:n, :], mskT[k].ap[:, kbs[0]:kbs[0] + n, :], ALU.mult,
                     reads=[eb, mskT[k]], writes=[pbf])

            def stage2(item, i):
                k, qt, h, kind, kbs, first_g, last_g = item
                qcs = slice(qt * 128, (qt + 1) * 128)
                n = len(kbs)
                pbf = pbuf_[i % NPIPE]
                for kk, kb in enumerate(kbs):
                    P.matmul(ov[h // 4][:, h % 4, 0:65], pbf.ap[:, kk, :], va[:, kb, h, 0:65],
                             first_g and kk == 0, last_g and kk == n - 1, reads=[pbf], writes=[ops2[h // 4]])
                if h == 7 and last_g:
                    for hh in range(2):
                        P.act(nrm.ap[:, hh * 4:(hh + 1) * 4].unsqueeze(2), ov[hh][:, :, 64:65], AF.Ln,
                              reads=[ops2[hh]], writes=[nrm])
                    P.act(nrm.ap[:, 8:16], nrm.ap[:, 0:8], AF.Exp, reads=[nrm], writes=[nrm], scale=-1.0)
                    for h2 in range(8):
                        P.act(oasb.ap[:, h2 * 64:(h2 + 1) * 64], ov[h2 // 4][:, h2 % 4, 0:64], AF.Copy,
                              reads=[ops2[h2 // 4], nrm], writes=[oasb], scale=nrm.ap[:, 8 + h2:9 + h2])
                    if debug and seq == 0:
                        P.dma("sync", dbg["d_oa"][qt * 128:(qt + 1) * 128, :], oasb.ap, "dbg_oa", reads=[oasb])
                    pv = pbo.ap.rearrange("p (a b) -> p a b", b=128)
                    for j2 in range(4):
                        P.transpose(pv[:, j2, :], oasb.ap[:, j2 * 128:(j2 + 1) * 128], identb.ap,
                                    reads=[oasb, identb], writes=[pbo])
                    P.copy("scalar", oaT[:, :, qcs], pv[:, 0:4, :], reads=[pbo])

            LA = NPIPE - 1
            for i in range(len(items) + LA):
                if i < len(items):
                    stage1(items[i], i)
                if i >= LA:
                    stage2(items[i - LA], i - LA)
                yield

        N_ITEMS = [48, 80, 112, 144]
        idx_group(0)
        for _ in bis_group(0):
            pass
        for g4 in range(4):
            if g4 + 1 < 4:
                idx_group(g4 + 1)
                ga = attn_group(g4)
                per = (N_ITEMS[g4] + 2 + N_BIS - 1) // N_BIS
                for _ in bis_group(g4 + 1):
                    for _i in range(per):
                        next(ga, None)
                assert next(ga, "done") == "done", "attention items not fully emitted before masks"
            else:
                for _ in attn_group(g4):
                    pass

        P.barrier()
        ar.reset(0)
        h2T = ar.alloc([128, 8, S], BF16)
        wd = ar.alloc([128, NJ, D], BF16)
        wd_b = Buf(wd, "wd")
        NR = 3
        wgr = [Buf(ar.alloc([128, 8, 256], BF16), "wg%d" % i) for i in range(NR)]
        wur = [Buf(ar.alloc([128, 8, 256], BF16), "wu%d" % i) for i in range(NR)]
        gfin = ar.alloc([128, D], F32)
        gfin_b = Buf(gfin, "gfin")
        C2_START = ar.off
        wout = ar.alloc([128, 8, D], BF16)
        wout_b = Buf(wout, "wout")
        wgv = wg_d.rearrange("(kc p) n -> p kc n", p=128)
        wuv = wu_d.rearrange("(kc p) n -> p kc n", p=128)
        P.dma("gpsimd", wout, wout_d.rearrange("(kc p) n -> p kc n", p=128), "wout", writes=[wout_b])
        for q4 in range(2):
            P.dma("gpsimd", wd[:, q4 * 11:(q4 + 1) * 11, :],
                  wd_d[q4 * 1408:(q4 + 1) * 1408, :].rearrange("(j p) n -> p j n", p=128), "wd", writes=[wd_b])
        for r in range(NR):
            P.dma("gpsimd", wgr[r].ap, wgv[:, :, r * 256:(r + 1) * 256], "wg%d" % r, writes=[wgr[r]])
            P.dma("gpsimd", wur[r].ap, wuv[:, :, r * 256:(r + 1) * 256], "wu%d" % r, writes=[wur[r]])
        P.dma("sync", gfin, c_d[:, C_GFIN:C_GFIN + 1024], "gfin", writes=[gfin_b])
        xin = [Buf(ar.alloc([128, D], F32), "xinC%d" % i) for i in range(2)]
        x1s = [Buf(ar.alloc([128, D], F32), "x1s%d" % i) for i in range(2)]
        xs = [Buf(ar.alloc([128, D], BF16), "xsC%d" % i) for i in range(2)]
        assert ar.off <= OA_OFF
        x1d_b = Buf(x1_d, "x1d")
        for t in range(NT):
            tcs = slice(t * 128, (t + 1) * 128)
            xb, x1b, xsb = xin[t % 2], x1s[t % 2], xs[t % 2]
            P.dma("sync", xb.ap, x_d[seq, tcs, :], "xinC%d" % (t % 2), writes=[xb])
            for half in range(2):
                ps = pf[(2 * t + half) % 4]
                for kc in range(8):
                    lhs = oaT[:, kc, tcs] if kc < 4 else obT_t[:, kc - 4, tcs]
                    P.matmul(ps.ap, lhs, wout[:, kc, half * 512:(half + 1) * 512], kc == 0, kc == 7,
                             reads=[wout_b], writes=[ps])
                P.tt("vector", x1b.ap[:, half * 512:(half + 1) * 512], xb.ap[:, half * 512:(half + 1) * 512], ps.ap,
                     ALU.add, reads=[xb, ps], writes=[x1b])
            P.dma("sync", x1_d[tcs, :], x1b.ap, "x1st", reads=[x1b], writes=[x1d_b])
            if debug and seq == 0:
                P.dma("sync", dbg["d_x1"][tcs, :], x1b.ap, "dbg_x1", reads=[x1b])
            rms_scale(x1b, x1b.ap, xsb)
            if t >= 1:
                xs_to_T(xs[(t - 1) % 2], h2T[:, :, (t - 1) * 128:t * 128], gffnT, pb[(t - 1) % 2])
        xs_to_T(xs[(NT - 1) % 2], h2T[:, :, (NT - 1) * 128:NT * 128], gffnT, pb[(NT - 1) % 2])

        P.barrier()
        ar.reset(C2_START)
        actT = ar.alloc([128, NJ, 1024], BF16)
        actT_b = Buf(actT, "actT")
        x1r = [Buf(ar.alloc([128, D], F32), "x1r%d" % i) for i in range(2)]
        ysb = [Buf(ar.alloc([128, D], F32), "ysb%d" % i) for i in range(2)]
        osb = [Buf(ar.alloc([128, D], F32), "osb%d" % i) for i in range(2)]
        sgc = [Buf(ar.alloc([128, 512], F32), "sgc%d" % i) for i in range(2)]
        junkC = ar.alloc([128, D], BF16)
        ring = [0]
        cctr = [0]
        for cb in range(2):
            for gj in range(NJ // 2):
                r = ring[0] % NR
                ring[0] += 1
                if ring[0] > NR:
                    P.dma("gpsimd", wgr[r].ap, wgv[:, :, gj * 256:(gj + 1) * 256], "wg%d" % r, writes=[wgr[r]])
                    P.dma("gpsimd", wur[r].ap, wuv[:, :, gj * 256:(gj + 1) * 256], "wu%d" % r, writes=[wur[r]])
                for jj in range(2):
                    j = 2 * gj + jj
                    for hb in range(2):
                        tok = slice(cb * 1024 + hb * 512, cb * 1024 + (hb + 1) * 512)
                        gps = pf[(cctr[0] % 2) * 2]
                        ups = pf[(cctr[0] % 2) * 2 + 1]
                        sgb = sgc[cctr[0] % 2]
                        cctr[0] += 1
                        for kc in range(8):
                            P.matmul(gps.ap, wgr[r].ap[:, kc, jj * 128:(jj + 1) * 128], h2T[:, kc, tok], kc == 0, kc == 7,
                                     reads=[wgr[r]], writes=[gps])
                        for kc in range(8):
                            P.matmul(ups.ap, wur[r].ap[:, kc, jj * 128:(jj + 1) * 128], h2T[:, kc, tok], kc == 0, kc == 7,
                                     reads=[wur[r]], writes=[ups])
                        P.act(sgb.ap, gps.ap, AF.Silu, reads=[gps], writes=[sgb])
                        P.tt("vector", actT[:, j, hb * 512:(hb + 1) * 512], sgb.ap, ups.ap, ALU.mult,
                             reads=[sgb, ups], writes=[actT_b])
            for ti in range(8):
                t = cb * 8 + ti
                tcs = slice(t * 128, (t + 1) * 128)
                xr, yb, ob_ = x1r[t % 2], ysb[t % 2], osb[t % 2]
                P.dma("sync", xr.ap, x1_d[tcs, :], "x1r%d" % (t % 2), reads=[x1d_b], writes=[xr])
                for half in range(2):
                    ps = pf[4 + half]
                    for j in range(NJ):
                        P.matmul(ps.ap, actT[:, j, ti * 128:(ti + 1) * 128], wd[:, j, half * 512:(half + 1) * 512],
                                 j == 0, j == NJ - 1, reads=[actT_b, wd_b], writes=[ps])
                    P.tt("vector", yb.ap[:, half * 512:(half + 1) * 512], xr.ap[:, half * 512:(half + 1) * 512], ps.ap,
                         ALU.add, reads=[xr, ps], writes=[yb])
                st = next_stat()
                P.act(junkC, yb.ap, AF.Square, reads=[yb], writes=[st], accum=st.ap[:, 0:1])
                P.act(st.ap[:, 1:2], st.ap[:, 0:1], AF.Ln, reads=[st], writes=[st], scale=1.0 / D, bias=EPS)
                P.act(st.ap[:, 2:3], st.ap[:, 1:2], AF.Exp, reads=[st], writes=[st], scale=-0.5)
                P.stt(ob_.ap, yb.ap, st.ap[:, 2:3], gfin, ALU.mult, ALU.mult, reads=[yb, st, gfin_b], writes=[ob_])
                P.dma("sync", y_d[seq, tcs, :], ob_.ap, "yout%d" % (t % 2), reads=[ob_])

    P.emit(final_waits=["yout0", "yout1"])
    return nc


_NC_CACHE = {}


def kernel(**inputs):
    x = np.ascontiguousarray(np.asarray(inputs["x"], np.float32))
    lay = _host_layout(inputs)
    if "nc" not in _NC_CACHE:
        _NC_CACHE["nc"] = build_nc()
    nc = _NC_CACHE["nc"]
    in_maps = []
    for c in range(8):
        m = {"x": np.ascontiguousarray(x[2 * c:2 * c + 2])}
        m.update(lay)
        in_maps.append(m)
    res = run_bass_kernel_spmd(nc, in_maps, core_ids=list(range(8)))
    out = np.concatenate([np.asarray(r["y"], np.float32).reshape(2, S, D) for r in res.results], axis=0)
    return out
```
